# Optimizing a Trainium2 kernel written in Bass

```python
import math
import jax, jax.numpy as jnp
from jax import lax
import numpy as np

D_MODEL = 1024
BATCH = 4
SEQ = 8192
DEPTH = 1
DEC_BATCH = 128
DEC_SEQ = 1
PAST_LEN = 16384
PAGE_SIZE = 128

HEAD_DIM = 64
N_HEADS = D_MODEL // HEAD_DIM
SWA_Q_HEADS = N_HEADS // 4
SWA_KV_HEADS = SWA_Q_HEADS // 2
SWA_GROUP = SWA_Q_HEADS // SWA_KV_HEADS
SWA_WINDOW = 128
DIL_CONFIGS = ((128, 1), (512, 4), (2048, 16))
N_DIL = len(DIL_CONFIGS)
DIL_HEADS = (N_HEADS - SWA_Q_HEADS) // N_DIL
SWA_Q_WIDTH = SWA_Q_HEADS * HEAD_DIM
SWA_KV_WIDTH = SWA_KV_HEADS * HEAD_DIM
DIL_WIDTH = N_DIL * DIL_HEADS * HEAD_DIM
QKV_SPLITS = (SWA_Q_WIDTH, SWA_Q_WIDTH + SWA_KV_WIDTH, SWA_Q_WIDTH + 2 * SWA_KV_WIDTH,
              SWA_Q_WIDTH + 2 * SWA_KV_WIDTH + DIL_WIDTH,
              SWA_Q_WIDTH + 2 * SWA_KV_WIDTH + 2 * DIL_WIDTH)
QKV_WIDTH = SWA_Q_WIDTH + 2 * SWA_KV_WIDTH + 3 * DIL_WIDTH
MIX_OUT = (SWA_Q_HEADS + DIL_HEADS) * HEAD_DIM
NUM_BUCKETS = 32
MAX_EXACT = NUM_BUCKETS // 2
MAX_DISTANCE = 2048
N_EXPERTS = 32
TOP_K = 4
D_FF = D_MODEL
SWIGLU_ALPHA = 1.702
SWIGLU_LIMIT = 7.0
MOE_BLOCK = 128
BAND_BLOCK = 128
LN_EPS = 1e-5
DEEPNORM_ALPHA = (2 * DEPTH) ** 0.25
DEEPNORM_BETA = (8 * DEPTH) ** -0.25

kernel_name = 'hybrid_swa_dilated_moe_step'


def layer_norm(x, g, b):
    xf = x.astype(jnp.float32)
    mu = xf.mean(-1, keepdims=True)
    var = jnp.square(xf - mu).mean(-1, keepdims=True)
    return ((xf - mu) * lax.rsqrt(var + LN_EPS) * g + b).astype(x.dtype)


def t5_bucket(dist):
    n = jnp.maximum(dist, 0)
    ratio = jnp.log(jnp.maximum(n, MAX_EXACT).astype(jnp.float32) / MAX_EXACT) / math.log(MAX_DISTANCE / MAX_EXACT)
    large = MAX_EXACT + (ratio * (NUM_BUCKETS - MAX_EXACT)).astype(jnp.int32)
    return jnp.where(n < MAX_EXACT, n, jnp.minimum(large, NUM_BUCKETS - 1))


def attend(q, k, v, dist, valid, bias_table, sink=None):
    kvh, grp, hd = q.shape[-3:]
    s = jnp.einsum('...qhgd,...shd->...hgqs', q, k).astype(jnp.float32) * (hd ** -0.5)
    bias = bias_table[t5_bucket(dist)].astype(jnp.float32)
    bias = bias.reshape(bias.shape[:-1] + (kvh, grp))
    bias = jnp.moveaxis(bias, (-2, -1), (-4, -3))
    s = jnp.where(valid[..., None, None, :, :], s + bias, -jnp.inf)
    m = s.max(axis=-1, keepdims=True)
    if sink is not None:
        sk = sink.astype(jnp.float32)[:, :, None, None]
        m = jnp.maximum(m, sk)
    e = jnp.exp(s - m)
    denom = e.sum(axis=-1, keepdims=True)
    if sink is not None:
        denom = denom + jnp.exp(sk - m)
    p = (e / denom).astype(v.dtype)
    out = jnp.einsum('...hgqs,...shd->...qhgd', p, v)
    lse = jnp.moveaxis((m + jnp.log(denom))[..., 0], -1, -3)
    return out, lse


def banded_attention(q, k, v, max_dist, dilation, bias_table, sink=None):
    n, L = q.shape[:2]
    nb = -(-L // BAND_BLOCK)
    lp = nb * BAND_BLOCK
    def pad_seq(x):
        return jnp.pad(x, [(0, 0), (0, lp - L)] + [(0, 0)] * (x.ndim - 2))
    def with_prev(x):
        xb = pad_seq(x).reshape((n, nb, BAND_BLOCK) + x.shape[2:])
        prev = jnp.pad(xb[:, :-1], [(0, 0), (1, 0)] + [(0, 0)] * (xb.ndim - 2))
        return jnp.concatenate([prev, xb], axis=2)
    qb = pad_seq(q).reshape((n, nb, BAND_BLOCK) + q.shape[2:])
    kk, vv = with_prev(k), with_prev(v)
    i = jnp.arange(BAND_BLOCK)[:, None]
    j = jnp.arange(2 * BAND_BLOCK)[None, :]
    dist = BAND_BLOCK + i - j
    kpos = (jnp.arange(nb)[:, None, None] - 1) * BAND_BLOCK + j
    valid = (dist >= 0) & (dist <= max_dist) & (kpos >= 0)
    out, lse = attend(qb, kk, vv, dist * dilation, valid, bias_table, sink)
    out = out.reshape((n, lp) + out.shape[3:])[:, :L]
    lse = lse.reshape((n, lp) + lse.shape[3:])[:, :L]
    return out, lse


def to_strided(x, d):
    n, s = x.shape[:2]
    x = jnp.swapaxes(x.reshape((n, s // d, d) + x.shape[2:]), 1, 2)
    return x.reshape((n * d, s // d) + x.shape[3:])


def from_strided(x, d, n):
    nd, l = x.shape[:2]
    x = jnp.swapaxes(x.reshape((n, d, l) + x.shape[2:]), 1, 2)
    return x.reshape((n, l * d) + x.shape[3:])


def gathered_attention(q, kv_cat, n_off, dilation, bias_table, sink=None):
    t = q.shape[1]
    l_past = kv_cat.shape[1] - t
    off = jnp.arange(n_off)
    idx = l_past + jnp.arange(t)[:, None] - off[None, :] * dilation
    valid = idx >= 0
    kvg = kv_cat[:, jnp.maximum(idx, 0)]
    out, lse = attend(q[:, :, None], kvg[..., 0, :, :], kvg[..., 1, :, :],
                      (off * dilation)[None, :], valid[:, None, :], bias_table, sink)
    return out[:, :, 0], lse[:, :, 0]


def project_qkv(h, w_in, b_in):
    n, t = h.shape[:2]
    p = h @ w_in + b_in
    qa, ka, va, qd, kd, vd = jnp.split(p, list(QKV_SPLITS), axis=-1)
    qa = qa.reshape(n, t, SWA_KV_HEADS, SWA_GROUP, HEAD_DIM)
    kva = jnp.stack([ka.reshape(n, t, SWA_KV_HEADS, HEAD_DIM),
                     va.reshape(n, t, SWA_KV_HEADS, HEAD_DIM)], axis=2)
    qd = qd.reshape(n, t, N_DIL, DIL_HEADS, 1, HEAD_DIM)
    kvd = jnp.stack([kd.reshape(n, t, N_DIL, DIL_HEADS, HEAD_DIM),
                     vd.reshape(n, t, N_DIL, DIL_HEADS, HEAD_DIM)], axis=3)
    return qa, kva, qd, kvd


def dil_table(table, g):
    return table[:, SWA_Q_HEADS + g * DIL_HEADS: SWA_Q_HEADS + (g + 1) * DIL_HEADS]


def merge_project(oa, outs, lses, w_o, b_o):
    n, t = oa.shape[:2]
    o = jnp.stack(outs)[..., 0, :]
    lse = jnp.stack(lses)[..., 0]
    wts = jax.nn.softmax(lse, axis=0).astype(o.dtype)
    od = jnp.einsum('gnth,gnthd->nthd', wts, o)
    cat = jnp.concatenate([oa.reshape(n, t, -1), od.reshape(n, t, -1)], axis=-1)
    return cat @ w_o + b_o


def mix_prompt(h, w_in, b_in, sinks, table, w_o, b_o):
    n, s = h.shape[:2]
    qa, kva, qd, kvd = project_qkv(h, w_in, b_in)
    oa, _ = banded_attention(qa, kva[:, :, 0], kva[:, :, 1], SWA_WINDOW - 1, 1,
                             table[:, :SWA_Q_HEADS], sinks)
    states = [kva[:, -min(SWA_WINDOW, s):]]
    outs, lses = [], []
    for g, (win, dil) in enumerate(DIL_CONFIGS):
        o, l = banded_attention(to_strided(qd[:, :, g], dil), to_strided(kvd[:, :, g, 0], dil),
                                to_strided(kvd[:, :, g, 1], dil), win // dil, dil, dil_table(table, g))
        outs.append(from_strided(o, dil, n))
        lses.append(from_strided(l, dil, n))
        states.append(kvd[:, -min(win, s):, g])
    return merge_project(oa, outs, lses, w_o, b_o), states


def mix_sample(h, caches, w_in, b_in, sinks, table, w_o, b_o):
    qa, kva, qd, kvd = project_qkv(h, w_in, b_in)
    kv_cat = jnp.concatenate([caches[0], kva], axis=1)
    oa, _ = gathered_attention(qa, kv_cat, SWA_WINDOW, 1, table[:, :SWA_Q_HEADS], sinks)
    states = [kv_cat[:, -min(SWA_WINDOW, kv_cat.shape[1]):]]
    outs, lses = [], []
    for g, (win, dil) in enumerate(DIL_CONFIGS):
        kv_cat = jnp.concatenate([caches[g + 1], kvd[:, :, g]], axis=1)
        o, l = gathered_attention(qd[:, :, g], kv_cat, win // dil + 1, dil, dil_table(table, g))
        outs.append(o)
        lses.append(l)
        states.append(kv_cat[:, -min(win, kv_cat.shape[1]):])
    return merge_project(oa, outs, lses, w_o, b_o), states


def moe_tokens(x2d, w_router, b_router, w_up, b_up, w_down, b_down):
    t = x2d.shape[0]
    logits = (x2d @ w_router + b_router).astype(jnp.float32)
    top_val, top_idx = lax.top_k(logits, TOP_K)
    gates = jax.nn.softmax(top_val, axis=-1).astype(x2d.dtype)
    flat_e = top_idx.reshape(-1)
    flat_tok = jnp.repeat(jnp.arange(t, dtype=jnp.int32), TOP_K)
    flat_g = gates.reshape(-1)
    order = jnp.argsort(flat_e)
    se = flat_e[order]
    counts = jnp.bincount(flat_e, length=N_EXPERTS)
    padded = (counts + MOE_BLOCK - 1) // MOE_BLOCK * MOE_BLOCK
    start = jnp.cumsum(counts) - counts
    pend = jnp.cumsum(padded)
    pstart = pend - padded
    dest = pstart[se] + jnp.arange(t * TOP_K) - start[se]
    n_blocks = -(-(t * TOP_K) // MOE_BLOCK) + N_EXPERTS
    rows = n_blocks * MOE_BLOCK
    row_tok = jnp.full((rows,), t, jnp.int32).at[dest].set(flat_tok[order])
    row_gate = jnp.zeros((rows,), x2d.dtype).at[dest].set(flat_g[order])
    block_e = jnp.minimum(jnp.searchsorted(pend, jnp.arange(n_blocks) * MOE_BLOCK, side='right'),
                          N_EXPERTS - 1)
    def expert_block(args):
        e, toks, g = args
        xb = x2d[jnp.minimum(toks, t - 1)]
        hb = xb @ w_up[e] + b_up[e]
        glu = jnp.minimum(hb[:, :D_FF], SWIGLU_LIMIT)
        lin = jnp.clip(hb[:, D_FF:], -SWIGLU_LIMIT, SWIGLU_LIMIT)
        a = glu * jax.nn.sigmoid(SWIGLU_ALPHA * glu) * (lin + 1)
        return (a @ w_down[e] + b_down[e]) * g[:, None]
    y = lax.map(expert_block, (block_e, row_tok.reshape(n_blocks, MOE_BLOCK),
                               row_gate.reshape(n_blocks, MOE_BLOCK)))
    return jax.ops.segment_sum(y.reshape(rows, -1), row_tok, num_segments=t + 1)[:t]


def moe_ffn(x, w_router, b_router, w_up, b_up, w_down, b_down):
    n, t, d = x.shape
    return moe_tokens(x.reshape(n * t, d), w_router, b_router, w_up, b_up, w_down, b_down).reshape(n, t, d)


def setup_inputs(seed: int = 0) -> dict:
    key = jax.random.key(seed)
    ks = jax.random.split(key, 24)
    f32 = jnp.float32
    def nrm(k, shape, scale):
        return jax.random.normal(k, shape, f32) * scale
    def kv_shape(win, heads):
        return (DEPTH, DEC_BATCH, min(win, PAST_LEN), 2, heads, HEAD_DIM)
    return {
        'x_prompt': nrm(ks[0], (BATCH, SEQ, D_MODEL), 1.0),
        'x_sample': nrm(ks[1], (DEC_BATCH, DEC_SEQ, D_MODEL), 1.0),
        'cache_swa_kv': nrm(ks[2], kv_shape(SWA_WINDOW, SWA_KV_HEADS), 1.0),
        'cache_dil1_kv': nrm(ks[3], kv_shape(DIL_CONFIGS[0][0], DIL_HEADS), 1.0),
        'cache_dil2_kv': nrm(ks[4], kv_shape(DIL_CONFIGS[1][0], DIL_HEADS), 1.0),
        'cache_dil3_kv': nrm(ks[5], kv_shape(DIL_CONFIGS[2][0], DIL_HEADS), 1.0),
        'rel_bias_table': nrm(ks[6], (NUM_BUCKETS, N_HEADS), 0.5),
        'w_in': nrm(ks[7], (DEPTH, D_MODEL, QKV_WIDTH), D_MODEL ** -0.5),
        'b_in': nrm(ks[8], (DEPTH, QKV_WIDTH), 0.02),
        'attn_sinks': nrm(ks[9], (DEPTH, SWA_Q_HEADS), 0.5),
        'w_o': nrm(ks[10], (DEPTH, MIX_OUT, D_MODEL), MIX_OUT ** -0.5 * DEEPNORM_BETA),
        'b_o': nrm(ks[11], (DEPTH, D_MODEL), 0.02),
        'ln1_g': 1.0 + nrm(ks[12], (DEPTH, D_MODEL), 0.02),
        'ln1_b': nrm(ks[13], (DEPTH, D_MODEL), 0.02),
        'w_router': nrm(ks[14], (DEPTH, D_MODEL, N_EXPERTS), D_MODEL ** -0.5),
        'b_router': nrm(ks[15], (DEPTH, N_EXPERTS), 0.01),
        'w_up': nrm(ks[16], (DEPTH, N_EXPERTS, D_MODEL, 2 * D_FF), D_MODEL ** -0.5),
        'b_up': nrm(ks[17], (DEPTH, N_EXPERTS, 2 * D_FF), 0.02),
        'w_down': nrm(ks[18], (DEPTH, N_EXPERTS, D_FF, D_MODEL), D_FF ** -0.5 * DEEPNORM_BETA),
        'b_down': nrm(ks[19], (DEPTH, N_EXPERTS, D_MODEL), 0.02),
        'ln2_g': 1.0 + nrm(ks[20], (DEPTH, D_MODEL), 0.02),
        'ln2_b': nrm(ks[21], (DEPTH, D_MODEL), 0.02),
    }


def reference(x_prompt, x_sample, cache_swa_kv, cache_dil1_kv, cache_dil2_kv, cache_dil3_kv,
              rel_bias_table, w_in, b_in, attn_sinks, w_o, b_o, ln1_g, ln1_b,
              w_router, b_router, w_up, b_up, w_down, b_down, ln2_g, ln2_b):
    xp, xs = x_prompt, x_sample
    states_p = [[] for _ in range(1 + N_DIL)]
    states_s = [[] for _ in range(1 + N_DIL)]
    for l in range(DEPTH):
        sinks = attn_sinks[l].reshape(SWA_KV_HEADS, SWA_GROUP)
        mp, sp = mix_prompt(xp, w_in[l], b_in[l], sinks, rel_bias_table, w_o[l], b_o[l])
        caches = [cache_swa_kv[l], cache_dil1_kv[l], cache_dil2_kv[l], cache_dil3_kv[l]]
        ms, ss = mix_sample(xs, caches, w_in[l], b_in[l], sinks, rel_bias_table, w_o[l], b_o[l])
        xp = layer_norm(DEEPNORM_ALPHA * xp + mp, ln1_g[l], ln1_b[l])
        xs = layer_norm(DEEPNORM_ALPHA * xs + ms, ln1_g[l], ln1_b[l])
        xp = layer_norm(DEEPNORM_ALPHA * xp + moe_ffn(xp, w_router[l], b_router[l], w_up[l], b_up[l],
                                                       w_down[l], b_down[l]), ln2_g[l], ln2_b[l])
        xs = layer_norm(DEEPNORM_ALPHA * xs + moe_ffn(xs, w_router[l], b_router[l], w_up[l], b_up[l],
                                                       w_down[l], b_down[l]), ln2_g[l], ln2_b[l])
        for i in range(1 + N_DIL):
            states_p[i].append(sp[i])
            states_s[i].append(ss[i])
    swa_kv_prompt = jnp.stack(states_p[0])
    dil1_kv_prompt = jnp.stack(states_p[1])
    dil2_kv_prompt = jnp.stack(states_p[2])
    dil3_kv_prompt = jnp.stack(states_p[3])
    swa_kv_sample = jnp.stack(states_s[0])
    dil1_kv_sample = jnp.stack(states_s[1])
    dil2_kv_sample = jnp.stack(states_s[2])
    dil3_kv_sample = jnp.stack(states_s[3])
    return (xp, xs, swa_kv_prompt, dil1_kv_prompt, dil2_kv_prompt, dil3_kv_prompt,
            swa_kv_sample, dil1_kv_sample, dil2_kv_sample, dil3_kv_sample)
```

```python
import os
import numpy as np
import concourse.bass as bass
import concourse.mybir as mybir
from concourse.bass_utils import run_bass_kernel_spmd

F32 = mybir.dt.float32
BF16 = mybir.dt.bfloat16
I32 = mybir.dt.int32
U8 = mybir.dt.uint8
ALU = mybir.AluOpType
AF = mybir.ActivationFunctionType
AX = mybir.AxisListType

NCORES = 8
D = 1024
QKV = 2816
NE = 32
CAP = 640
UNIT = 2048
HALO = 2048
WIN = HALO + UNIT
NUNIT = 2
SOWN = UNIT * NUNIT
NSMP = 16
NT = SOWN // 128 + 1
ALPHA = float(2.0 ** 0.25)
EPS = 1e-5
BIG = 1.0e6
XGROWS = NE * CAP
FW = 512
ARENA = 186 * 1024
STAGE = int(os.environ.get("K_STAGE", "99"))
NE_DECL = NE if STAGE >= 5 else 1

GROUPS = [
    (1, 127, 128), (1, 128, 128), (4, 128, 512), (16, 128, 2048)]


def t5_bucket_np(n):
    n = np.maximum(np.asarray(n, np.int64), 0)
    ratio = np.log(np.maximum(n, 16).astype(np.float32) / np.float32(16)) / np.float32(np.log(2048 / 16))
    large = 16 + (ratio.astype(np.float32) * np.float32(16)).astype(np.int32)
    return np.where(n < 16, n, np.minimum(large, 31)).astype(np.int64)


def host_consts():
    c128 = np.zeros((128, 3 * 128 + 32 + 2 + 16), np.float32)
    c128[:, 0:128] = np.eye(128, dtype=np.float32)
    c128[:, 128:256] = np.triu(np.ones((128, 128), np.float32), 1)
    c128[:, 256:384] = 1.0
    c128[:, 384:416] = (np.arange(NE, dtype=np.float32) * CAP)[None, :]
    c128[:NSMP, 416] = 1.0
    c128[NSMP:, 417] = BIG
    for p in range(128):
        c128[p, 418 + p // 8] = 1.0
    oh = np.zeros((32, 4, FW), np.float32)
    valid = np.zeros((4, 4, FW), np.float32)
    for gi, (d, maxd, L) in enumerate(GROUPS):
        for u in range(383):
            dist = u - 127
            if 0 <= dist <= maxd:
                oh[t5_bucket_np(dist * d), gi, u] = 1.0
                valid[:, gi, u] = 1.0
        for j in range(129):
            o = 128 - j
            if o <= maxd:
                oh[t5_bucket_np(o * d), gi, 383 + j] = 1.0
                valid[:, gi, 383 + j] = 1.0
    rep = np.zeros((16, 128), np.float32)
    for p in range(128):
        rep[p // 8, p] = 1.0
    return c128, oh.reshape(32, 4 * FW), valid.reshape(4, 4 * FW), rep


class Lazy:
    def __init__(self, f):
        self.f = f


class _Rec:
    def __init__(self):
        self.call = None

    def __getattr__(self, name):
        def m(*args, **kwargs):
            assert self.call is None
            self.call = (name, args, kwargs)
            return self
        return m


def _bind(fn):
    r = _Rec()
    fn(r)
    name, args, kwargs = r.call

    def run(eng):
        kw = {k: (v.f() if isinstance(v, Lazy) else v) for k, v in kwargs.items()}
        return getattr(eng, name)(*args, **kw)
    return run


class Sched:
    ENG = ("pe", "act", "dve", "pool", "sp")

    def __init__(self, nc, esems, dsems, bgsems=()):
        self.nc = nc
        self.bgsem = list(bgsems)
        self.bgcnt = [0] * len(self.bgsem)
        self.bgnext = 0
        self.bgnext_pool = 0
        self.q = {e: [] for e in self.ENG}
        self.cnt = {e: 0 for e in self.ENG}
        self.esem = esems
        self.dsem = dsems
        self.dcnt = [0] * len(dsems)
        self.dnext = 0
        self.dnext_pool = 0
        self.seen = {e: {} for e in self.ENG}
        self.lastw = {}
        self.readers = {}
        self.pending_pe = False

    def _need(self, e, tok):
        k, v = tok
        if k == e and e == "pe":
            return
        if self.seen[e].get(k, 0) >= v:
            return
        self.seen[e][k] = v
        if isinstance(k, str):
            sem = self.esem[k]
        elif k[0] == "bg":
            sem = self.bgsem[k[1]]
        else:
            sem = self.dsem[k[1]]
        self.q[e].append(lambda eng, s=sem, vv=v: eng.wait_ge(s, vv))

    def _deps(self, e, reads, writes):
        toks = []
        for k in reads:
            if k in self.lastw:
                toks.append(self.lastw[k])
        for k in writes:
            if k in self.lastw:
                toks.append(self.lastw[k])
            toks.extend(self.readers.get(k, {}).values())
        for t in toks:
            self._need(e, t)

    def _record(self, tok, reads, writes):
        for k in reads:
            self.readers.setdefault(k, {})[tok[0]] = tok
        for k in writes:
            self.lastw[k] = tok
            self.readers[k] = {}

    def op(self, e, fn, reads=(), writes=(), signal=True):
        fn = _bind(fn)
        self._deps(e, reads, writes)
        tok = (e, self.cnt[e] + 1)
        if signal:
            self.cnt[e] += 1
            sem = self.esem[e]
            self.q[e].append(lambda eng, f=fn, s=sem: f(eng).then_inc(s, 1))
        else:
            assert e == "pe"
            self.q[e].append(lambda eng, f=fn: f(eng))
        self._record(tok, reads, writes)

    def dma(self, e, fn, reads=(), writes=()):
        fn = _bind(fn)
        self._deps(e, reads, writes)
        npool = 16
        if e == "pool":
            idx = self.dnext_pool
            self.dnext_pool = (self.dnext_pool + 1) % npool
        else:
            idx = npool + self.dnext
            self.dnext = (self.dnext + 1) % (len(self.dsem) - npool)
        if self.dcnt[idx] > 0:
            self._need(e, (("dma", idx), self.dcnt[idx] * 16))
        self.dcnt[idx] += 1
        tok = (("dma", idx), self.dcnt[idx] * 16)
        sem = self.dsem[idx]
        self.q[e].append(lambda eng, f=fn, s=sem: f(eng).then_inc(s, 16))
        self._record(tok, reads, writes)

    def dma_bg(self, e, fn, reads=()):
        fn = _bind(fn)
        self._deps(e, reads, ())
        half = len(self.bgsem) // 2
        if e == "pool":
            idx = half + self.bgnext_pool
            self.bgnext_pool = (self.bgnext_pool + 1) % half
        else:
            idx = self.bgnext
            self.bgnext = (self.bgnext + 1) % half
        self.bgcnt[idx] += 1
        sem = self.bgsem[idx]
        self.q[e].append(lambda eng, f=fn, s=sem: f(eng).then_inc(s, 16))

    def wait_bg(self, engines=None):
        for e in (engines or self.ENG):
            for i in range(len(self.bgsem)):
                if self.bgcnt[i] > 0:
                    self._need(e, (("bg", i), self.bgcnt[i] * 16))

    def barrier(self):
        for e in self.ENG:
            for o in ("pe", "act", "dve", "pool"):
                if o != e and self.cnt[o] > 0:
                    self._need(e, (o, self.cnt[o]))
            for i in range(len(self.dsem)):
                if self.dcnt[i] > 0:
                    self._need(e, (("dma", i), self.dcnt[i] * 16))
        self.lastw = {}
        self.readers = {}

    def replay(self, block):
        q = self.q

        @block.tensor
        def _(eng):
            for t in q["pe"]:
                t(eng)

        @block.scalar
        def _(eng):
            for t in q["act"]:
                t(eng)

        @block.vector
        def _(eng):
            for t in q["dve"]:
                t(eng)

        @block.gpsimd
        def _(eng):
            for t in q["pool"]:
                t(eng)

        @block.sync
        def _(eng):
            for t in q["sp"]:
                t(eng)


class Arena:
    def __init__(self, t, nbytes):
        self.t = t
        self.n = nbytes
        self.off = 0

    def alloc(self, shape, dt, nbytes_el):
        free = int(np.prod(shape[1:]))
        nb = free * nbytes_el
        nb = (nb + 63) // 64 * 64
        assert self.off + nb <= self.n, ("arena overflow", self.off, nb, self.n)
        v = self.t[:, self.off:self.off + free * nbytes_el].bitcast(dt)
        self.off += nb
        if len(shape) == 3:
            v = v.rearrange("p (a b) -> p a b", a=shape[1])
        elif len(shape) == 4:
            v = v.rearrange("p (a b c) -> p a b c", a=shape[1], b=shape[2])
        return v

    def f32(self, *shape):
        return self.alloc((128,) + shape, F32, 4)

    def bf(self, *shape):
        return self.alloc((128,) + shape, BF16, 2)

    def i32(self, *shape):
        return self.alloc((128,) + shape, I32, 4)


def build_program():
    nc = bass.Bass("TRN2", target_bir_lowering=False)

    def din(name, shape, dt=F32):
        return nc.dram_tensor(name, list(shape), dt, kind="ExternalInput")

    def dout(name, shape, dt=F32):
        return nc.dram_tensor(name, list(shape), dt, kind="ExternalOutput")

    def dint(name, shape, dt=F32):
        return nc.dram_tensor(name, list(shape), dt, kind="Internal")

    xw = din("xw", [HALO + SOWN, D])
    flag_d = din("flag", [128, NUNIT])
    xs_d = din("xs", [NSMP, D])
    cache_d = [din("c_swa", [NSMP, 128, 256]), din("c_d1", [NSMP, 128, 512]),
               din("c_d2", [NSMP, 512, 512]), din("c_d3", [NSMP, 2048, 512])]
    table_d = din("table", [32, 16])
    w_in_d = din("w_in", [D, QKV])
    b_in_d = din("b_in", [QKV])
    sinks_d = din("sinks", [4])
    w_o_d = din("w_o", [512, D])
    b_o_d = din("b_o", [D])
    ln1g_d = din("ln1_g", [D]); ln1b_d = din("ln1_b", [D])
    ln2g_d = din("ln2_g", [D]); ln2b_d = din("ln2_b", [D])
    w_r_d = din("w_r", [D, NE]); b_r_d = din("b_r", [NE])
    w_up_d = din("w_up", [NE_DECL, D, 2 * D]); b_up_d = din("b_up", [NE * 16, 128])
    w_dn_d = din("w_dn", [NE_DECL, D, D]); b_dn_d = din("b_dn", [NE, D])
    c128_d = din("c128", [128, 434]); coh_d = din("coh", [32, 4 * FW])
    cval_d = din("cval", [4, 4 * FW]); crep_d = din("crep", [16, 128])

    yp_d = dout("yp", [SOWN, D]); ys_d = dout("ys", [NSMP, D])
    ps_d = [dout("ps_swa", [128, 2, 2, 64]), dout("ps_d1", [128, 2, 4, 64]),
            dout("ps_d2", [512, 2, 4, 64]), dout("ps_d3", [2048, 2, 4, 64])]
    ss_d = [dout("ss_swa", [NSMP, 128, 256]), dout("ss_d1", [NSMP, 128, 512]),
            dout("ss_d2", [NSMP, 512, 512]), dout("ss_d3", [NSMP, 2048, 512])]

    X1 = dint("X1", [NT * 128, D])
    XG = dint("XG", [XGROWS, D], BF16)
    YS = dint("YS", [XGROWS + 1, D])
    FD = dint("FD", [16, FW])
    FDR = dint("FDR", [16, 128, FW])
    FDS = dint("FDS", [16, 16, 128])

    import contextlib
    with contextlib.ExitStack() as es:
        arena_t = es.enter_context(nc.sbuf_tensor("arena", [128, ARENA], U8))
        psb = [es.enter_context(nc.psum_tensor("psb%d" % i, [128, 512], F32)) for i in range(8)]
        esems = {e: es.enter_context(nc.semaphore("s_" + e)) for e in ("pe", "act", "dve", "pool")}
        dsems = [es.enter_context(nc.semaphore("d%d" % i)) for i in range(56)]
        bgsems = [es.enter_context(nc.semaphore("g%d" % i)) for i in range(8)]
        es.enter_context(nc.allow_non_contiguous_dma(reason="small strided constant loads"))
        S = Sched(nc, esems, dsems, bgsems)
        A = Arena(arena_t, ARENA)
        emit(nc, S, A, psb, locals())
        block = es.enter_context(nc.Block())
        S.replay(block)
    return nc


def emit(nc, S, A, psb, T):
    xw, flag_d, xs_d, cache_d, table_d = T["xw"], T["flag_d"], T["xs_d"], T["cache_d"], T["table_d"]
    w_in_d, b_in_d, sinks_d, w_o_d, b_o_d = T["w_in_d"], T["b_in_d"], T["sinks_d"], T["w_o_d"], T["b_o_d"]
    X1, XG, YS, FD = T["X1"], T["XG"], T["YS"], T["FD"]
    FDR, FDS = T["FDR"], T["FDS"]
    ps_d, ss_d = T["ps_d"], T["ss_d"]

    def PS(i):
        return psb[i][:, :]

    def PSB(i):
        return psb[i][:, :].bitcast(BF16)

    def bc_row(dram_ap_1d, n):
        return dram_ap_1d.unsqueeze(0).broadcast_to([128, n])

    c128 = A.f32(434)
    ident_f = c128[:, 0:128]
    ones_f = c128[:, 256:384]
    ecap = c128[:, 384:416]
    rowvalid = c128[:, 416:417]
    rowbig = c128[:, 417:418]
    repT = c128[:, 418:434]
    cbf = A.bf(384)
    ident_b = cbf[:, 0:128]
    triu_b = cbf[:, 128:256]
    ones_b = cbf[:, 256:384]
    crep = A.f32(128)
    flag = A.f32(NUNIT)
    bqk = A.f32(22)
    bkdup = A.f32(2)
    expsink = A.f32(4)
    G_all = A.f32(NT, NE)
    g4_all = A.f32(NT, 4)
    dsc_all = A.i32(NT, 4)
    dga_all = A.i32(NT, 4)
    cnt = A.f32(NE)
    wr = A.f32(8, NE)
    brb = A.f32(NE)
    zero_bf = A.bf(2048)
    persist_mark = A.off

    REGS = T["_regs"] = {}

    def _mkregs(eng):
        REGS["sc"] = eng.alloc_register("bc_sc")
        eng.reg_mov(REGS["sc"], XGROWS - 1)
        REGS["ga"] = eng.alloc_register("bc_ga")
        eng.reg_mov(REGS["ga"], XGROWS)
    S.q["pool"].append(_mkregs)
    S.dma("sp", lambda e: e.dma_start(out=c128, in_=T["c128_d"].ap()), writes=["c128"])
    S.dma("sp", lambda e: e.dma_start(out=crep[0:16, :], in_=T["crep_d"].ap()), writes=["crep"])
    S.dma("sp", lambda e: e.dma_start(out=flag, in_=flag_d.ap()), writes=["flag"])
    S.dma("sp", lambda e: e.dma_start(out=bqk, in_=b_in_d.ap().rearrange("(j p) -> p j", p=128)), writes=["bqk"])
    for p in range(2):
        for hh in range(2):
            S.dma("sp", lambda e, p=p, hh=hh: e.dma_start(
                out=bkdup[hh * 64:(hh + 1) * 64, p:p + 1],
                in_=b_in_d.ap()[256 + p * 64:256 + (p + 1) * 64].unsqueeze(1)), writes=["bkdup"])
    S.dma("sp", lambda e: e.dma_start(out=expsink, in_=bc_row(sinks_d.ap(), 4)), writes=["expsink"])
    S.dma("sp", lambda e: e.dma_start(out=wr, in_=T["w_r_d"].ap().rearrange("(k p) n -> p k n", p=128)), writes=["wr"])
    S.dma("sp", lambda e: e.dma_start(out=brb, in_=bc_row(T["b_r_d"].ap(), NE)), writes=["brb"])
    S.op("act", lambda e: e.activation(out=expsink, in_=expsink, func=AF.Exp), reads=["expsink"], writes=["expsink"])
    S.op("dve", lambda e: e.tensor_copy(out=cbf, in_=c128[:, 0:384]), reads=["c128"], writes=["cbf"])
    S.op("pool", lambda e: e.memset(cnt, 0.0), writes=["cnt"])
    S.op("pool", lambda e: e.memset(zero_bf, 0.0), writes=["zero_bf"])

    xg_v = XG.ap().rearrange("(a p r) d -> a p (r d)", p=128, r=2)
    for a in range(XGROWS // 256):
        S.dma_bg("pool", lambda e, a=a: e.dma_start(out=xg_v[a], in_=zero_bf), reads=["zero_bf"])
    S.dma_bg("pool", lambda e: e.dma_start(out=YS.ap()[XGROWS:XGROWS + 1, :].bitcast(BF16),
                                         in_=zero_bf[0:1, :]), reads=["zero_bf"])

    for gi, (d, maxd, L) in enumerate(GROUPS):
        nch = max(1, L // 256)
        rows = (L - 1)
        per = (rows + nch - 1) // nch
        for ch in range(nch):
            r0, r1 = ch * per, min(rows, (ch + 1) * per)
            if r0 >= r1:
                continue
            S.dma_bg("sp", lambda e, gi=gi, r0=r0, r1=r1: e.dma_start(
                out=ss_d[gi].ap()[:, r0:r1, :].rearrange("b r c -> b (r c)"),
                in_=cache_d[gi].ap()[:, r0 + 1:r1 + 1, :].rearrange("b r c -> b (r c)")))

    m0 = A.off
    tab = A.f32(16)
    coh = A.f32(4 * FW)
    cval = A.f32(4 * FW)
    ft = A.f32(4, FW)
    S.dma("sp", lambda e: e.dma_start(out=tab[0:32, :], in_=table_d.ap()), writes=["tab"])
    S.dma("sp", lambda e: e.dma_start(out=coh[0:32, :], in_=T["coh_d"].ap()), writes=["coh"])
    S.dma("sp", lambda e: e.dma_start(out=cval[0:4, :], in_=T["cval_d"].ap()), writes=["cval"])
    for gi in range(4):
        S.op("pe", lambda e, gi=gi: e.matmul(PS(gi)[0:4, :], lhsT=tab[0:32, gi * 4:gi * 4 + 4],
                                             rhs=coh[0:32, gi * FW:(gi + 1) * FW], start=True, stop=True),
             reads=["tab", "coh"], writes=[("ps", gi)])
        S.op("act", lambda e, gi=gi: e.activation(out=ft[0:4, gi, :], in_=PS(gi)[0:4, :], func=AF.Exp),
             reads=[("ps", gi)], writes=[("ft", gi)])
        S.op("dve", lambda e, gi=gi: e.tensor_tensor(out=ft[0:4, gi, :], in0=ft[0:4, gi, :],
                                                      in1=cval[0:4, gi * FW:(gi + 1) * FW], op=ALU.mult),
             reads=[("ft", gi), "cval"], writes=[("ft", gi)])
        S.dma("sp", lambda e, gi=gi: e.dma_start(out=FD.ap()[gi * 4:gi * 4 + 4, :], in_=ft[0:4, gi, :]),
              reads=[("ft", gi)], writes=[("FDw", gi)])
        S.dma("sp", lambda e, gi=gi: e.dma_start(out=FDR.ap()[gi * 4:gi * 4 + 4, :, :],
                                                 in_=ft[0:4, gi, :].unsqueeze(1).broadcast_to([4, 128, FW])),
              reads=[("ft", gi)], writes=[("FDRw", gi)])
        S.dma("sp", lambda e, gi=gi: e.dma_start(out=FDS.ap()[gi * 4:gi * 4 + 4, :, :],
                                                 in_=ft[0:4, gi, 383:383 + 128].unsqueeze(1).broadcast_to([4, 16, 128])),
              reads=[("ft", gi)], writes=[("FDSw", gi)])
    S.barrier()
    A.off = m0
    if STAGE == 1:
        S.barrier()
        S.wait_bg()
        return

    sample_phase(nc, S, A, PS, T, dict(c128=c128, ident_f=ident_f, ident_b=ident_b, repT=repT, crep=crep,
                                        expsink=expsink, bc_row=bc_row, PSB=PSB))
    cat_s = T["_cat_s"]
    xs_t = T["_xs_t"]
    S.barrier()
    if STAGE == 2:
        S.barrier()
        S.wait_bg()
        return

    jobs = []
    for p in range(2):
        jobs.append(dict(gi=0, d=1, maxd=127, qcol=p * 128, kcol=256 + p * 64, vcol=384 + p * 64, dup=True,
                         tcol=[2 * p, 2 * p + 1], slot=[2 * p, 2 * p + 1], first=True, last=True,
                         sink=[2 * p, 2 * p + 1], bq=p, bk=None, st_h=p, pair=p))
    for pr in range(2):
        for g in range(3):
            base = (g * 4 + 2 * pr) * 64
            jobs.append(dict(gi=g + 1, d=GROUPS[g + 1][0], maxd=128, qcol=512 + base, kcol=1280 + base,
                             vcol=2048 + base, dup=False, tcol=[4 + g * 4 + 2 * pr, 4 + g * 4 + 2 * pr + 1],
                             slot=[4 + 2 * pr, 4 + 2 * pr + 1], first=(g == 0), last=(g == 2), sink=None,
                             bq=(512 + base) // 128, bk=(1280 + base) // 128, st_h=2 * pr, pair=pr))

    pm = A.off
    xT = A.bf(8, WIN)
    catT = A.bf(8, UNIT)
    xst = A.f32(D)
    xbf = A.bf(D)
    sfull = [A.f32(256), A.f32(256)]
    wq = A.bf(8, 128)
    wkv = A.bf(8, 256)
    jm = A.off

    tile_ctx = dict(cnt=cnt, G_all=G_all, g4_all=g4_all, dsc_all=dsc_all, dga_all=dga_all, wr=wr, brb=brb,
                    ecap=ecap, rowvalid=rowvalid, rowbig=rowbig, ident_f=ident_f, ident_b=ident_b,
                    triu_b=triu_b, ones_b=ones_b, bc_row=bc_row, PSB=PSB)

    for u in range(NUNIT):
        for t in range(WIN // 128):
            S.dma("act", lambda e, t=t, u=u: e.dma_start(out=xst, in_=xw.ap()[u * UNIT + t * 128:u * UNIT + (t + 1) * 128, :]),
                  writes=["xst"])
            S.op("act", lambda e: e.activation(out=xbf, in_=xst, func=AF.Copy), reads=["xst"], writes=["xbf"])
            pb = 6 + (t % 2)
            for kc in range(8):
                S.op("pe", lambda e, kc=kc, pb=pb: e.transpose(PSB(pb)[:, kc * 128:(kc + 1) * 128],
                                                              xbf[:, kc * 128:(kc + 1) * 128], ident_b),
                     reads=["xbf", "cbf"], writes=[("ps", pb)], signal=(kc == 7))
            S.op("dve", lambda e, t=t, pb=pb: e.tensor_copy(
                out=xT[:, :, t * 128:(t + 1) * 128], in_=PSB(pb).rearrange("p (a b) -> p a b", a=8)),
                reads=[("ps", pb)], writes=[("xT", t)])
        xT_keys = [("xT", t) for t in range(WIN // 128)]

        for ji, jb in enumerate(jobs):
            A.off = jm
            d, gi = jb["d"], jb["gi"]
            halo_len = 128 * d
            T0 = HALO - halo_len
            nbo = UNIT // (128 * d)
            nb = nbo + 1
            QT = A.bf(UNIT)
            KT = A.bf(WIN)
            Vaug = A.bf(32, 2, 65)
            acc = A.f32(2, UNIT)
            EB = A.f32(2, 256)
            EBf = A.f32(2, 256)
            Et = [A.bf(512), A.bf(512)]
            Pt = [A.bf(512), A.bf(512)]
            kvb = A.f32(256)
            K = lambda name: (name, 0)
            S.dma("pool", lambda e, jb=jb: e.dma_start(
                out=wq, in_=w_in_d.ap()[:, jb["qcol"]:jb["qcol"] + 128].rearrange("(k p) n -> p k n", p=128)),
                writes=["wq"])
            if jb["dup"]:
                for hh in range(2):
                    S.dma("pool", lambda e, jb=jb, hh=hh: e.dma_start(
                        out=wkv[:, :, hh * 64:(hh + 1) * 64],
                        in_=w_in_d.ap()[:, jb["kcol"]:jb["kcol"] + 64].rearrange("(k p) n -> p k n", p=128)),
                        writes=["wkv"])
                    S.dma("pool", lambda e, jb=jb, hh=hh: e.dma_start(
                        out=wkv[:, :, 128 + hh * 64:128 + (hh + 1) * 64],
                        in_=w_in_d.ap()[:, jb["vcol"]:jb["vcol"] + 64].rearrange("(k p) n -> p k n", p=128)),
                        writes=["wkv"])
                    S.dma("sp", lambda e, jb=jb, hh=hh: e.dma_start(
                        out=kvb[:, hh * 64:(hh + 1) * 64], in_=bc_row(b_in_d.ap()[jb["kcol"]:jb["kcol"] + 64], 64)),
                        writes=["kvb"])
                    S.dma("sp", lambda e, jb=jb, hh=hh: e.dma_start(
                        out=kvb[:, 128 + hh * 64:128 + (hh + 1) * 64],
                        in_=bc_row(b_in_d.ap()[jb["vcol"]:jb["vcol"] + 64], 64)), writes=["kvb"])
            else:
                S.dma("pool", lambda e, jb=jb: e.dma_start(
                    out=wkv[:, :, 0:128],
                    in_=w_in_d.ap()[:, jb["kcol"]:jb["kcol"] + 128].rearrange("(k p) n -> p k n", p=128)),
                    writes=["wkv"])
                S.dma("pool", lambda e, jb=jb: e.dma_start(
                    out=wkv[:, :, 128:256],
                    in_=w_in_d.ap()[:, jb["vcol"]:jb["vcol"] + 128].rearrange("(k p) n -> p k n", p=128)),
                    writes=["wkv"])
                S.dma("sp", lambda e, jb=jb: e.dma_start(
                    out=kvb[:, 0:128], in_=bc_row(b_in_d.ap()[jb["kcol"]:jb["kcol"] + 128], 128)), writes=["kvb"])
                S.dma("sp", lambda e, jb=jb: e.dma_start(
                    out=kvb[:, 128:256], in_=bc_row(b_in_d.ap()[jb["vcol"]:jb["vcol"] + 128], 128)), writes=["kvb"])
            for hh in range(2):
                c = jb["tcol"][hh]
                S.dma("act", lambda e, c=c, hh=hh: e.dma_start(
                    out=EB[:, hh, 0:128], in_=bass.AP(FDR, c * 128 * FW + 255, [[FW - 1, 128], [1, 128]])),
                    reads=["FD"], writes=["EB"])
                S.dma("act", lambda e, c=c, hh=hh: e.dma_start(
                    out=EB[:, hh, 128:256], in_=bass.AP(FDR, c * 128 * FW + 127, [[FW - 1, 128], [1, 128]])),
                    reads=["FD"], writes=["EB"])
            S.op("pool", lambda e, u=u: e.tensor_scalar(out=EBf[:, :, 0:128], in0=EB[:, :, 0:128],
                                                         scalar1=flag[:, u:u + 1], scalar2=None, op0=ALU.mult),
                 reads=["EB", "flag"], writes=["EBf"])
            S.op("pool", lambda e: e.tensor_copy(out=EBf[:, :, 128:256], in_=EB[:, :, 128:256]),
                 reads=["EB"], writes=["EBf"])
            S.op("pool", lambda e: e.memset(Vaug[:, :, :, 64:65], 1.0), writes=["Vones"])
            bqc = bqk[:, jb["bq"]:jb["bq"] + 1]
            for c4 in range(UNIT // 512):
                pb = c4 % 2
                for kc in range(8):
                    S.op("pe", lambda e, kc=kc, pb=pb, c4=c4: e.matmul(
                        PS(pb), lhsT=wq[:, kc, :], rhs=xT[:, kc, HALO + c4 * 512:HALO + (c4 + 1) * 512],
                        start=(kc == 0), stop=(kc == 7)),
                        reads=["wq"] + xT_keys[16 + c4 * 4:16 + c4 * 4 + 4], writes=[("ps", pb)], signal=(kc == 7))
                S.op("dve", lambda e, pb=pb, c4=c4, bqc=bqc: e.tensor_scalar(
                    out=QT[:, c4 * 512:(c4 + 1) * 512], in0=PS(pb), scalar1=bqc, scalar2=0.125,
                    op0=ALU.add, op1=ALU.mult), reads=[("ps", pb), "bqk"], writes=["QT"])
            bkc = bkdup[:, jb["pair"]:jb["pair"] + 1] if jb["dup"] else bqk[:, jb["bk"]:jb["bk"] + 1]
            pos = T0
            ci = 0
            while pos < WIN:
                n = min(512, WIN - pos)
                pb = ci % 2
                for kc in range(8):
                    S.op("pe", lambda e, kc=kc, pb=pb, pos=pos, n=n: e.matmul(
                        PS(pb)[:, 0:n], lhsT=wkv[:, kc, 0:128], rhs=xT[:, kc, pos:pos + n],
                        start=(kc == 0), stop=(kc == 7)),
                        reads=["wkv"] + xT_keys[pos // 128:(pos + n + 127) // 128], writes=[("ps", pb)],
                        signal=(kc == 7))
                S.op("act", lambda e, pb=pb, pos=pos, n=n, bkc=bkc: e.activation(
                    out=KT[:, pos:pos + n], in_=PS(pb)[:, 0:n], func=AF.Identity, bias=bkc, scale=1.0),
                    reads=[("ps", pb), "bqk", "bkdup"], writes=["KT"])
                pos += n
                ci += 1
            for r in range(d):
                for ib in range(nb):
                    blk = r * nb + ib
                    pb = 2 + (blk % 2)
                    start = T0 + r + d * 128 * ib
                    for kc in range(8):
                        S.op("pe", lambda e, kc=kc, pb=pb, start=start, d=d: e.matmul(
                            PS(pb)[:, 0:256], lhsT=xT[:, kc, start:start + 127 * d + 1:d], rhs=wkv[:, kc, :],
                            start=(kc == 0), stop=(kc == 7)),
                            reads=["wkv"] + xT_keys[start // 128:(start + 128 * d + 127) // 128],
                            writes=[("ps", pb)], signal=(kc == 7))
                    st_ib = nbo
                    is_state = (u == NUNIT - 1 and ib == st_ib and not os.environ.get("K_NOSTATE"))
                    if not is_state:
                        S.op("dve", lambda e, pb=pb, blk=blk: e.tensor_tensor(
                            out=Vaug[:, blk, :, 0:64], in0=PS(pb)[:, 128:256].rearrange("p (a b) -> p a b", a=2),
                            in1=kvb[:, 128:256].rearrange("p (a b) -> p a b", a=2), op=ALU.add),
                            reads=[("ps", pb), "kvb"], writes=[("V", blk)])
                    else:
                        sf = sfull[blk % 2]
                        S.op("dve", lambda e, pb=pb, sf=sf: e.tensor_tensor(out=sf, in0=PS(pb)[:, 0:256], in1=kvb, op=ALU.add),
                             reads=[("ps", pb), "kvb"], writes=[("sf", blk % 2)])
                        S.op("pool", lambda e, sf=sf, blk=blk: e.tensor_copy(
                            out=Vaug[:, blk, :, 0:64], in_=sf[:, 128:256].rearrange("p (a b) -> p a b", a=2)),
                            reads=[("sf", blk % 2)], writes=[("V", blk)])
                        for kv in range(2):
                            if jb["dup"]:
                                dst = ps_d[0].ap()[:, kv, jb["st_h"], :]
                                src = sf[:, kv * 128:kv * 128 + 64]
                            else:
                                dst = ps_d[gi].ap()[r::d, kv, jb["st_h"]:jb["st_h"] + 2, :].rearrange("p b c -> p (b c)")
                                src = sf[:, kv * 128:(kv + 1) * 128]
                            S.dma("pool", lambda e, dst=dst, src=src: e.dma_start(out=dst, in_=src),
                                  reads=[("sf", blk % 2)], writes=[("psd", gi, jb["pair"], r, kv)])
            qbs = [(r, ib) for r in range(d) for ib in range(1, nb)]
            for hh in range(2):
                hp = slice(hh * 64, (hh + 1) * 64)
                for bi in range(0, len(qbs), 2):
                    bsel = (bi // 2) % 2
                    stb = 4 + bsel
                    ob = 6 + bsel
                    ST = PS(stb).rearrange("p (a b) -> p a b", a=2)
                    for qi in range(2):
                        r, ib = qbs[bi + qi]
                        qs = r + d * 128 * (ib - 1)
                        qap = QT[hp, qs:qs + 127 * d + 1:d]
                        for side in range(2):
                            ks = T0 + r + d * 128 * (ib - 1 + side)
                            S.op("pe", lambda e, ST=ST, qi=qi, side=side, ks=ks, qap=qap, hp=hp, d=d: e.matmul(
                                ST[:, qi, side * 128:(side + 1) * 128], lhsT=KT[hp, ks:ks + 127 * d + 1:d], rhs=qap,
                                start=True, stop=True),
                                reads=["QT", "KT"], writes=[("ps", stb)], signal=(qi == 1 and side == 1))
                    E = Et[bsel]
                    S.op("act", lambda e, E=E, stb=stb: e.activation(out=E, in_=PS(stb), func=AF.Exp),
                         reads=[("ps", stb)], writes=[("E", bsel)])
                    Pq = Pt[bsel].rearrange("p (a b) -> p a b", a=2)
                    Ev = E.rearrange("p (a b) -> p a b", a=2)
                    for qi in range(2):
                        r, ib = qbs[bi + qi]
                        ebt = EBf if ib == 1 else EB
                        S.op("pool" if qi == 0 else "dve", lambda e, Pq=Pq, Ev=Ev, qi=qi, ebt=ebt, hh=hh: e.tensor_tensor(
                            out=Pq[:, qi, :], in0=Ev[:, qi, :], in1=ebt[:, hh, :], op=ALU.mult),
                            reads=[("E", bsel), "EB", "EBf"], writes=[("P", bsel, qi)])
                    OT = PS(ob).rearrange("p (a b) -> p a b", a=4)
                    for qi in range(2):
                        r, ib = qbs[bi + qi]
                        for side in range(2):
                            blk = r * nb + ib - 1 + side
                            S.op("pe", lambda e, OT=OT, qi=qi, side=side, blk=blk, Pq=Pq, hh=hh: e.matmul(
                                OT[0:65, qi, :], lhsT=Vaug[:, blk, hh, :], rhs=Pq[:, qi, side * 128:(side + 1) * 128],
                                start=(side == 0), stop=(side == 1)),
                                reads=[("V", blk), "Vones", ("P", bsel, qi)], writes=[("ps", ob)],
                                signal=(qi == 1 and side == 1))
                    for qi in range(2):
                        r, ib = qbs[bi + qi]
                        qs = r + d * 128 * (ib - 1)
                        dst = acc[0:65, hh, qs:qs + 127 * d + 1:d]
                        if jb["first"]:
                            S.op("act", lambda e, dst=dst, OT=OT, qi=qi: e.activation(out=dst, in_=OT[0:65, qi, :], func=AF.Identity),
                                 reads=[("ps", ob)], writes=[("acc", hh)])
                        else:
                            S.op("dve", lambda e, dst=dst, OT=OT, qi=qi: e.tensor_tensor(
                                out=dst, in0=OT[0:65, qi, :], in1=dst, op=ALU.add),
                                reads=[("ps", ob)], writes=[("acc", hh)])
            if jb["last"]:
                for hh in range(2):
                    slot = jb["slot"][hh]
                    den = acc[64:65, hh, :]
                    if jb["sink"] is not None:
                        si = jb["sink"][hh]
                        S.op("dve", lambda e, den=den, si=si: e.tensor_scalar(
                            out=den, in0=den, scalar1=expsink[64:65, si:si + 1], scalar2=None, op0=ALU.add),
                            reads=[("acc", hh), "expsink"], writes=[("acc", hh)])
                    S.op("dve", lambda e, den=den: e.reciprocal(out=den, in_=den), reads=[("acc", hh)], writes=[("acc", hh)])
                    for c4 in range(UNIT // 512):
                        pb = c4 % 2
                        S.op("pe", lambda e, pb=pb, c4=c4, hh=hh: e.matmul(
                            PS(pb)[0:64, :], lhsT=ones_f[64:65, 0:64], rhs=acc[64:65, hh, c4 * 512:(c4 + 1) * 512],
                            start=True, stop=True), reads=[("acc", hh), "c128"], writes=[("ps", pb)])
                        S.op("dve", lambda e, pb=pb, c4=c4, hh=hh, slot=slot: e.tensor_tensor(
                            out=catT[0:64, slot, c4 * 512:(c4 + 1) * 512], in0=acc[0:64, hh, c4 * 512:(c4 + 1) * 512],
                            in1=PS(pb)[0:64, :], op=ALU.mult), reads=[("ps", pb), ("acc", hh)], writes=[("catT", slot)])
        if STAGE == 3:
            S.barrier()
            S.wait_bg()
            return
        S.barrier()
        S.wait_bg()
        A.off = jm
        if STAGE == 32 and u == 1:
            return
        post_tiles(nc, S, A, PS, T, tile_ctx, catT, u, list(range(UNIT // 128)), xst)
        S.barrier()
        if STAGE == 31 or (STAGE == 33 and u == 1):
            S.wait_bg()
            return
    A.off = jm
    post_tiles(nc, S, A, PS, T, tile_ctx, None, None, [None], xst, sample=(cat_s, xs_t, catT))
    S.barrier()
    A.off = persist_mark
    if STAGE == 4:
        S.barrier()
        S.wait_bg()
        return

    moe_phase(nc, S, A, PS, PSB, T, ident_b)
    S.barrier()
    A.off = persist_mark
    if STAGE == 5:
        S.barrier()
        S.wait_bg()
        return
    combine_phase(nc, S, A, PS, T, tile_ctx)
    S.barrier()
    S.wait_bg()


def layer_norm_tile(S, A_tiles, z, g_b, b_b, out, key_in, key_out):
    junk, st = A_tiles["junk"], A_tiles["st"]
    S.op("act", lambda e: e.activation(out=junk, in_=z, func=AF.Identity, accum_out=st[:, 0:1]),
         reads=[key_in], writes=["ln_junk", "ln_st0"])
    S.op("act", lambda e: e.activation(out=junk, in_=z, func=AF.Square, accum_out=st[:, 1:2]),
         reads=[key_in], writes=["ln_junk", "ln_st1"])
    S.op("dve", lambda e: e.tensor_scalar(out=st[:, 2:3], in0=st[:, 0:1], scalar1=1.0 / D, scalar2=None, op0=ALU.mult),
         reads=["ln_st0"], writes=["ln_st2"])
    S.op("dve", lambda e: e.tensor_tensor(out=st[:, 3:4], in0=st[:, 2:3], in1=st[:, 2:3], op=ALU.mult),
         reads=["ln_st2"], writes=["ln_st3"])
    S.op("dve", lambda e: e.scalar_tensor_tensor(out=st[:, 4:5], in0=st[:, 1:2], scalar=1.0 / D, in1=st[:, 3:4],
                                                  op0=ALU.mult, op1=ALU.subtract),
         reads=["ln_st1", "ln_st3"], writes=["ln_st4"])
    S.op("dve", lambda e: e.tensor_scalar(out=st[:, 4:5], in0=st[:, 4:5], scalar1=EPS, scalar2=None, op0=ALU.add),
         reads=["ln_st4"], writes=["ln_st4"])
    S.op("act", lambda e: e.activation(out=st[:, 5:6], in_=st[:, 4:5], func=AF.Ln), reads=["ln_st4"], writes=["ln_st5"])
    S.op("act", lambda e: e.activation(out=st[:, 5:6], in_=st[:, 5:6], func=AF.Exp, scale=-0.5), reads=["ln_st5"], writes=["ln_st5"])
    S.op("dve", lambda e: e.scalar_tensor_tensor(out=st[:, 6:7], in0=st[:, 2:3], scalar=-1.0, in1=st[:, 5:6],
                                                  op0=ALU.mult, op1=ALU.mult),
         reads=["ln_st2", "ln_st5"], writes=["ln_st6"])
    S.op("act", lambda e: e.activation(out=junk, in_=z, func=AF.Identity, bias=st[:, 6:7], scale=st[:, 5:6]),
         reads=[key_in, "ln_st5", "ln_st6"], writes=["ln_junk"])
    S.op("pool", lambda e: e.tensor_tensor(out=junk, in0=junk, in1=g_b, op=ALU.mult),
         reads=["ln_junk", "lnconst"], writes=["ln_junk"])
    S.op("pool", lambda e: e.tensor_tensor(out=out, in0=junk, in1=b_b, op=ALU.add),
         reads=["ln_junk", "lnconst"], writes=[key_out])


def post_tiles(nc, S, A, PS, T, C, catT, u, tiles, xst, sample=None):
    bc_row = C["bc_row"]
    X1, XG = T["X1"], T["XG"]
    w_o = A.bf(8, D)
    bo_b = A.f32(D)
    g_b = A.f32(D)
    b_b = A.f32(D)
    z = A.f32(D)
    junk = A.f32(D)
    x1 = A.f32(D)
    x1bf = A.bf(D)
    x1T = A.f32(8, 128)
    st = A.f32(8)
    lg = A.f32(NE)
    m8 = A.f32(8)
    mask = A.f32(NE)
    mask_b = A.bf(NE)
    ex = A.f32(NE)
    ssum = A.f32(2)
    posc = A.f32(NE)
    big = A.f32(NE)
    dst_f = A.f32(8)
    scr = A.f32(NE)
    S.dma("pool", lambda e: e.dma_start(out=w_o[0:64, :, :], in_=T["w_o_d"].ap().rearrange("(s p) n -> p s n", p=64)),
          writes=["w_o"])
    S.dma("sp", lambda e: e.dma_start(out=bo_b, in_=bc_row(T["b_o_d"].ap(), D)), writes=["lnconst"])
    S.dma("sp", lambda e: e.dma_start(out=g_b, in_=bc_row(T["ln1g_d"].ap(), D)), writes=["lnconst"])
    S.dma("sp", lambda e: e.dma_start(out=b_b, in_=bc_row(T["ln1b_d"].ap(), D)), writes=["lnconst"])
    if sample is not None:
        cat_s, xs_t, catT_buf = sample
        catT = catT_buf
        for slot in range(8):
            S.op("pe", lambda e, slot=slot: e.transpose(PS(4 + slot // 4)[0:64, (slot % 4) * 128:(slot % 4) * 128 + 128],
                                                        cat_s[:, slot, :], C["ident_f"]),
                 reads=["cat_s", "c128"], writes=[("ps", 4 + slot // 4)], signal=(slot % 4 == 3))
        for half in range(2):
            S.op("dve", lambda e, half=half: e.tensor_copy(
                out=catT[0:64, half * 4:half * 4 + 4, 0:128], in_=PS(4 + half)[0:64, :].rearrange("p (a b) -> p a b", a=4)),
                reads=[("ps", 4 + half)], writes=[("catT", "s")])
    for tl in tiles:
        if sample is None:
            ti = u * (UNIT // 128) + tl
            tok0 = tl * 128
            S.dma("sp", lambda e, ti=ti: e.dma_start(out=xst, in_=T["xw"].ap()[HALO + ti * 128:HALO + (ti + 1) * 128, :]),
                  writes=["xst"])
            xin = xst
        else:
            ti = NT - 1
            tok0 = 0
            xin = sample[1]
        for half in range(2):
            for slot in range(8):
                S.op("pe", lambda e, half=half, slot=slot, tok0=tok0: e.matmul(
                    PS(half), lhsT=catT[0:64, slot, tok0:tok0 + 128], rhs=w_o[0:64, slot, half * 512:(half + 1) * 512],
                    start=(slot == 0), stop=(slot == 7)),
                    reads=["w_o"] + [("catT", s) for s in list(range(8)) + ["s"]], writes=[("ps", half)], signal=(slot == 7))
        S.op("pool", lambda e, xin=xin: e.tensor_scalar(out=z, in0=xin, scalar1=ALPHA, scalar2=None, op0=ALU.mult),
             reads=["xst", "xs_t"], writes=["z"])
        S.op("pool", lambda e: e.tensor_tensor(out=z, in0=z, in1=bo_b, op=ALU.add), reads=["z", "lnconst"], writes=["z"])
        for half in range(2):
            S.op("dve", lambda e, half=half: e.tensor_tensor(out=z[:, half * 512:(half + 1) * 512],
                                                              in0=z[:, half * 512:(half + 1) * 512], in1=PS(half), op=ALU.add),
                 reads=["z", ("ps", half)], writes=["z"])
        layer_norm_tile(S, dict(junk=junk, st=st), z, g_b, b_b, x1, "z", "x1")
        S.dma("pool", lambda e, ti=ti: e.dma_start(out=X1.ap()[ti * 128:(ti + 1) * 128, :], in_=x1), reads=["x1"], writes=[("X1", ti)])
        S.op("act", lambda e: e.activation(out=x1bf, in_=x1, func=AF.Copy), reads=["x1"], writes=["x1bf"])
        for kc in range(8):
            S.op("pe", lambda e, kc=kc: e.transpose(PS(2 + kc // 4)[:, (kc % 4) * 128:(kc % 4) * 128 + 128],
                                                    x1[:, kc * 128:(kc + 1) * 128], C["ident_f"]),
                 reads=["x1", "c128"], writes=[("ps", 2 + kc // 4)], signal=(kc % 4 == 3))
        for half in range(2):
            S.op("act", lambda e, half=half: e.activation(
                out=x1T[:, half * 4:half * 4 + 4, :], in_=PS(2 + half).rearrange("p (a b) -> p a b", a=4), func=AF.Identity),
                reads=[("ps", 2 + half)], writes=[("x1T", half)])
        for kc in range(8):
            S.op("pe", lambda e, kc=kc: e.matmul(PS(4)[:, 0:NE], lhsT=x1T[:, kc, :], rhs=C["wr"][:, kc, :],
                                                 start=(kc == 0), stop=(kc == 7)),
                 reads=[("x1T", kc // 4), "wr"], writes=[("ps", 4)], signal=(kc == 7))
        S.op("dve", lambda e: e.tensor_tensor(out=lg, in0=PS(4)[:, 0:NE], in1=C["brb"], op=ALU.add),
             reads=[("ps", 4), "brb"], writes=["lg"])
        S.op("dve", lambda e: e.max(out=m8, in_=lg), reads=["lg"], writes=["m8"])
        S.op("dve", lambda e: e.tensor_scalar(out=mask, in0=lg, scalar1=m8[:, 3:4], scalar2=None, op0=ALU.is_ge),
             reads=["lg", "m8"], writes=["mask"])
        if sample is not None:
            S.op("dve", lambda e: e.tensor_scalar(out=mask, in0=mask, scalar1=C["rowvalid"], scalar2=None, op0=ALU.mult),
                 reads=["mask", "c128"], writes=["mask"])
        S.op("dve", lambda e: e.tensor_scalar(out=ssum[:, 0:1], in0=m8[:, 0:1], scalar1=-1.0, scalar2=None, op0=ALU.mult),
             reads=["m8"], writes=["nmax"])
        S.op("act", lambda e: e.activation(out=ex, in_=lg, func=AF.Exp, bias=ssum[:, 0:1], scale=1.0),
             reads=["lg", "nmax"], writes=["ex"])
        S.op("dve", lambda e: e.tensor_tensor(out=ex, in0=ex, in1=mask, op=ALU.mult), reads=["ex", "mask"], writes=["ex"])
        S.op("dve", lambda e: e.reduce_sum(out=ssum[:, 1:2], in_=ex, axis=AX.X), reads=["ex"], writes=["esum"])
        if sample is not None:
            S.op("dve", lambda e: e.tensor_tensor(out=ssum[:, 1:2], in0=ssum[:, 1:2], in1=C["rowbig"], op=ALU.add),
                 reads=["esum", "c128"], writes=["esum"])
        S.op("dve", lambda e: e.reciprocal(out=ssum[:, 1:2], in_=ssum[:, 1:2]), reads=["esum"], writes=["esum"])
        Gt = C["G_all"][:, ti, :]
        S.op("dve", lambda e, Gt=Gt: e.tensor_scalar(out=Gt, in0=ex, scalar1=ssum[:, 1:2], scalar2=None, op0=ALU.mult),
             reads=["ex", "esum"], writes=["G"])
        S.op("act", lambda e: e.activation(out=mask_b, in_=mask, func=AF.Copy), reads=["mask"], writes=["mask_b"])
        S.op("pe", lambda e: e.matmul(PS(5)[:, 0:NE], lhsT=C["triu_b"], rhs=mask_b, start=True, stop=True),
             reads=["mask_b", "cbf"], writes=[("ps", 5)])
        S.op("pe", lambda e: e.matmul(PS(5)[:, 64:64 + NE], lhsT=C["ones_b"], rhs=mask_b, start=True, stop=True),
             reads=["mask_b", "cbf"], writes=[("ps", 5)])
        S.op("dve", lambda e: e.tensor_tensor(out=posc, in0=PS(5)[:, 0:NE], in1=C["cnt"], op=ALU.add),
             reads=[("ps", 5), "cnt"], writes=["posc"])
        S.op("dve", lambda e: e.tensor_tensor(out=C["cnt"], in0=PS(5)[:, 64:64 + NE], in1=C["cnt"], op=ALU.add),
             reads=[("ps", 5), "posc"], writes=["cnt"])
        S.op("dve", lambda e: e.tensor_scalar(out=big, in0=posc, scalar1=float(CAP) - 0.5, scalar2=BIG, op0=ALU.is_gt, op1=ALU.mult),
             reads=["posc"], writes=["big"])
        S.op("dve", lambda e: e.tensor_tensor(out=posc, in0=posc, in1=C["ecap"], op=ALU.add), reads=["posc", "c128"], writes=["posc"])
        S.op("dve", lambda e: e.tensor_tensor(out=posc, in0=posc, in1=big, op=ALU.add), reads=["posc", "big"], writes=["posc"])
        if sample is not None:
            S.op("dve", lambda e: e.tensor_scalar(out=posc, in0=posc, scalar1=C["rowbig"], scalar2=None, op0=ALU.add),
                 reads=["posc", "c128"], writes=["posc"])
        for k in range(4):
            S.op("dve", lambda e, k=k: e.scalar_tensor_tensor(
                out=scr, in0=lg, scalar=m8[:, k:k + 1], in1=posc, op0=ALU.is_equal, op1=ALU.mult, accum_out=dst_f[:, k:k + 1]),
                reads=["lg", "m8", "posc"], writes=["scr", ("dstf", k)])
            S.op("dve", lambda e, k=k, Gt=Gt, ti=ti: e.scalar_tensor_tensor(
                out=scr, in0=lg, scalar=m8[:, k:k + 1], in1=Gt, op0=ALU.is_equal, op1=ALU.mult,
                accum_out=C["g4_all"][:, ti, k:k + 1]),
                reads=["lg", "m8", "G"], writes=["scr", "g4"])
        S.op("dve", lambda e, ti=ti: e.tensor_copy(out=C["dsc_all"][:, ti, :], in_=dst_f[:, 0:4]),
             reads=[("dstf", k) for k in range(4)], writes=["dsc"])
        S.op("dve", lambda e: e.tensor_scalar(out=dst_f[:, 4:8], in0=dst_f[:, 0:4], scalar1=float(XGROWS), scalar2=None, op0=ALU.min),
             reads=[("dstf", k) for k in range(4)], writes=["dstf2"])
        S.op("dve", lambda e, ti=ti: e.tensor_copy(out=C["dga_all"][:, ti, :], in_=dst_f[:, 4:8]),
             reads=["dstf2"], writes=["dga"])
        for k in range(4):
            S.dma("pool", lambda e, k=k, ti=ti: e.indirect_dma_start(
                out=XG.ap(), out_offset=bass.IndirectOffsetOnAxis(ap=C["dsc_all"][:, ti, k:k + 1], axis=0),
                in_=x1bf, in_offset=None, bounds_check=Lazy(lambda: T["_regs"]["sc"]), oob_is_err=False),
                reads=["x1bf", "dsc", "XG"], writes=[("XGs", ti, k)])


def moe_phase(nc, S, A, PS, PSB, T, ident_b):
    XG, YS = T["XG"], T["YS"]
    w_up_d, w_dn_d = T["w_up_d"], T["w_dn_d"]
    wu = [A.bf(8, 2 * D), A.bf(8, 2 * D)]
    wd = [A.bf(8, D), A.bf(8, D)]
    xg = A.bf(CAP // 128, D)
    xgT = A.bf(8, CAP)
    aT = A.bf(8, CAP)
    gt = [A.f32(512), A.f32(512)]
    sg = [A.f32(512), A.f32(512)]
    t2 = [A.f32(512), A.f32(512)]
    yst = [A.f32(D), A.f32(D)]
    bupT = A.f32(NE * 16)
    bst = A.f32(128)
    for a in range(4):
        S.dma("sp", lambda e, a=a: e.dma_start(out=bst, in_=T["b_up_d"].ap()[a * 128:(a + 1) * 128, :]), writes=["bst"])
        S.op("pe", lambda e, a=a: e.transpose(PS(0)[:, a * 128:(a + 1) * 128], bst, T["_ident_f"]),
             reads=["bst"], writes=[("ps", 0)])
        S.op("dve", lambda e, a=a: e.tensor_copy(out=bupT[:, a * 128:(a + 1) * 128], in_=PS(0)[:, a * 128:(a + 1) * 128]),
             reads=[("ps", 0)], writes=["bupT"])
    pieces = [(0, 512), (512, CAP - 512)]

    def load_w(ex):
        b = ex % 2
        for kc2 in range(4):
            S.dma("pool", lambda e, ex=ex, b=b, kc2=kc2: e.dma_start(
                out=wu[b][:, 2 * kc2:2 * kc2 + 2, :],
                in_=w_up_d.ap()[ex, kc2 * 256:(kc2 + 1) * 256, :].rearrange("(k p) n -> p k n", p=128)),
                writes=[("wu", b, kc2)])
        for kc2 in range(2):
            S.dma("pool", lambda e, ex=ex, b=b, kc2=kc2: e.dma_start(
                out=wd[b][:, 4 * kc2:4 * kc2 + 4, :],
                in_=w_dn_d.ap()[ex, kc2 * 512:(kc2 + 1) * 512, :].rearrange("(k p) n -> p k n", p=128)),
                writes=[("wd", b, kc2)])

    load_w(0)
    for ex in range(NE):
        b = ex % 2
        if ex + 1 < NE:
            load_w(ex + 1)
        S.dma("pool", lambda e, ex=ex: e.dma_start(
            out=xg, in_=XG.ap()[ex * CAP:(ex + 1) * CAP, :].rearrange("(a p) d -> p a d", p=128)),
            reads=["XGall"], writes=["xg"])
        for a in range(CAP // 128):
            pb = 6 + (a % 2)
            for kc in range(8):
                S.op("pe", lambda e, a=a, kc=kc, pb=pb: e.transpose(PSB(pb)[:, kc * 128:(kc + 1) * 128],
                                                                    xg[:, a, kc * 128:(kc + 1) * 128], ident_b),
                     reads=["xg"], writes=[("ps", pb)], signal=(kc == 7))
            S.op("act", lambda e, a=a, pb=pb: e.activation(
                out=xgT[:, :, a * 128:(a + 1) * 128], in_=PSB(pb).rearrange("p (a b) -> p a b", a=8), func=AF.Copy),
                reads=[("ps", pb)], writes=[("xgT", a)])
        xgT_keys = [("xgT", a) for a in range(CAP // 128)]
        it = 0
        for (s0, sn) in pieces:
            for j in range(8):
                pg = (it % 2) * 2
                pl = pg + 1
                for (pb, fo) in ((pg, j), (pl, 8 + j)):
                    for kc in range(8):
                        S.op("pe", lambda e, pb=pb, fo=fo, kc=kc, s0=s0, sn=sn, b=b: e.matmul(
                            PS(pb)[:, 0:sn], lhsT=wu[b][:, kc, fo * 128:(fo + 1) * 128], rhs=xgT[:, kc, s0:s0 + sn],
                            start=(kc == 0), stop=(kc == 7)),
                            reads=[("wu", b, kc // 2)] + xgT_keys, writes=[("ps", pb)], signal=(kc == 7))
                bi = it % 2
                bg = bupT[:, ex * 16 + j:ex * 16 + j + 1]
                bl = bupT[:, ex * 16 + 8 + j:ex * 16 + 8 + j + 1]
                g_, s_, t_ = gt[bi][:, 0:sn], sg[bi][:, 0:sn], t2[bi][:, 0:sn]
                S.op("dve", lambda e, pg=pg, g_=g_, bg=bg, sn=sn: e.tensor_scalar(
                    out=g_, in0=PS(pg)[:, 0:sn], scalar1=bg, scalar2=7.0, op0=ALU.add, op1=ALU.min),
                    reads=[("ps", pg), "bupT"], writes=[("g", bi)])
                S.op("act", lambda e, g_=g_, s_=s_: e.activation(out=s_, in_=g_, func=AF.Sigmoid, scale=1.702),
                     reads=[("g", bi)], writes=[("sg", bi)])
                S.op("dve", lambda e, pl=pl, t_=t_, bl=bl, sn=sn: e.tensor_scalar(
                    out=t_, in0=PS(pl)[:, 0:sn], scalar1=bl, scalar2=7.0, op0=ALU.add, op1=ALU.min),
                    reads=[("ps", pl), "bupT"], writes=[("t2", bi)])
                S.op("pool", lambda e, t_=t_: e.tensor_scalar(
                    out=t_, in0=t_, scalar1=-7.0, scalar2=1.0, op0=ALU.max, op1=ALU.add),
                    reads=[("t2", bi)], writes=[("t2", bi)])
                S.op("pool", lambda e, g_=g_, s_=s_: e.tensor_tensor(out=g_, in0=g_, in1=s_, op=ALU.mult),
                     reads=[("g", bi), ("sg", bi)], writes=[("g", bi)])
                S.op("dve", lambda e, g_=g_, t_=t_, j=j, s0=s0, sn=sn: e.tensor_tensor(
                    out=aT[:, j, s0:s0 + sn], in0=g_, in1=t_, op=ALU.mult),
                    reads=[("g", bi), ("t2", bi)], writes=[("aT", j, s0)])
                it += 1
        aT_keys = [("aT", j, s0) for j in range(8) for (s0, sn) in pieces]
        for a in range(CAP // 128):
            yt = yst[a % 2]
            for half in range(2):
                pb = 4 + half
                for fc in range(8):
                    S.op("pe", lambda e, a=a, half=half, fc=fc, pb=pb, b=b: e.matmul(
                        PS(pb), lhsT=aT[:, fc, a * 128:(a + 1) * 128], rhs=wd[b][:, fc, half * 512:(half + 1) * 512],
                        start=(fc == 0), stop=(fc == 7)),
                        reads=aT_keys + [("wd", b, fc // 4)], writes=[("ps", pb)], signal=(fc == 7))
                S.op("act" if half == 0 else "dve",
                     (lambda e, yt=yt, pb=pb, half=half: e.activation(out=yt[:, half * 512:(half + 1) * 512], in_=PS(pb), func=AF.Identity))
                     if half == 0 else
                     (lambda e, yt=yt, pb=pb, half=half: e.tensor_copy(out=yt[:, half * 512:(half + 1) * 512], in_=PS(pb))),
                     reads=[("ps", pb)], writes=[("yst", a % 2, half)])
            S.dma("pool", lambda e, ex=ex, a=a, yt=yt: e.dma_start(
                out=YS.ap()[ex * CAP + a * 128:ex * CAP + (a + 1) * 128, :], in_=yt),
                reads=[("yst", a % 2, 0), ("yst", a % 2, 1)], writes=[("YS", ex, a)])


def combine_phase(nc, S, A, PS, T, C):
    bc_row = C["bc_row"]
    X1, YS = T["X1"], T["YS"]
    g_b = A.f32(D)
    b_b = A.f32(D)
    bdn = A.f32(D)
    yk = [A.f32(D) for _ in range(4)]
    x1 = A.f32(D)
    z = A.f32(D)
    junk = A.f32(D)
    out = A.f32(D)
    st = A.f32(8)
    GT = A.f32(128)
    S.dma("sp", lambda e: e.dma_start(out=g_b, in_=bc_row(T["ln2g_d"].ap(), D)), writes=["lnconst"])
    S.dma("sp", lambda e: e.dma_start(out=b_b, in_=bc_row(T["ln2b_d"].ap(), D)), writes=["lnconst"])
    S.dma("sp", lambda e: e.dma_start(out=bdn[0:NE, :], in_=T["b_dn_d"].ap()), writes=["bdn"])
    for ti in range(NT):
        S.dma("pool", lambda e, ti=ti: e.dma_start(out=x1, in_=X1.ap()[ti * 128:(ti + 1) * 128, :]), writes=["x1"])
        for k in range(4):
            S.dma("pool", lambda e, k=k, ti=ti: e.indirect_dma_start(
                out=yk[k], out_offset=None, in_=YS.ap(),
                in_offset=bass.IndirectOffsetOnAxis(ap=C["dga_all"][:, ti, k:k + 1], axis=0),
                bounds_check=Lazy(lambda: T["_regs"]["ga"]), oob_is_err=False), writes=[("yk", k)])
        S.op("pe", lambda e, ti=ti: e.transpose(PS(2)[0:NE, 0:128], C["G_all"][:, ti, :], C["ident_f"]),
             writes=[("ps", 2)])
        S.op("act", lambda e: e.activation(out=GT[0:NE, :], in_=PS(2)[0:NE, 0:128], func=AF.Identity),
             reads=[("ps", 2)], writes=["GT"])
        for half in range(2):
            S.op("pe", lambda e, half=half: e.matmul(PS(half), lhsT=GT[0:NE, :], rhs=bdn[0:NE, half * 512:(half + 1) * 512],
                                                     start=True, stop=True), reads=["GT", "bdn"], writes=[("ps", half)])
        S.op("pool", lambda e: e.tensor_scalar(out=z, in0=x1, scalar1=ALPHA, scalar2=None, op0=ALU.mult),
             reads=["x1"], writes=["z"])
        for k in range(4):
            S.op("dve", lambda e, k=k, ti=ti: e.scalar_tensor_tensor(
                out=z, in0=yk[k], scalar=C["g4_all"][:, ti, k:k + 1], in1=z, op0=ALU.mult, op1=ALU.add),
                reads=[("yk", k), "z"], writes=["z"])
        for half in range(2):
            S.op("dve", lambda e, half=half: e.tensor_tensor(out=z[:, half * 512:(half + 1) * 512],
                                                              in0=z[:, half * 512:(half + 1) * 512], in1=PS(half), op=ALU.add),
                 reads=["z", ("ps", half)], writes=["z"])
        layer_norm_tile(S, dict(junk=junk, st=st), z, g_b, b_b, out, "z", "out")
        if ti < NT - 1:
            S.dma("pool", lambda e, ti=ti: e.dma_start(out=T["yp_d"].ap()[ti * 128:(ti + 1) * 128, :], in_=out),
                  reads=["out"], writes=[("yp", ti)])
        else:
            S.dma("sp", lambda e: e.dma_start(out=T["ys_d"].ap(), in_=out[0:NSMP, :]), reads=["out"], writes=["ys"])


def sample_phase(nc, S, A, PS, T, C):
    bc_row = C["bc_row"]
    cache_d, ss_d = T["cache_d"], T["ss_d"]
    FD = T["FD"]
    cat_s = A.f32(8, 64)
    xs_t = A.f32(D)
    T["_cat_s"] = cat_s
    T["_xs_t"] = xs_t
    T["_ident_f"] = C["ident_f"]
    m0 = A.off
    w_in = A.bf(8, QKV)
    xsb = A.bf(D)
    xsT = A.bf(8, 128)
    binb = A.f32(QKV)
    qkv = A.f32(QKV)
    qrep = A.f32(1024)
    ck = A.f32(16, 512)
    ebs = A.f32(4, 16)
    prod = A.f32(16, 64)
    sc = A.f32(4, 16)
    part = A.f32(4, 65)
    tot = A.f32(4, 65)
    totd = A.f32(4, 65)
    snew = A.f32(16)
    enew = A.f32(16)
    f0 = A.f32(16)
    pr16 = A.f32(64)
    S.op("pool", lambda e: e.memset(xs_t, 0.0), writes=["xs_t"])
    S.op("pool", lambda e: e.memset(cat_s, 0.0), writes=["cat_s"])
    S.dma("sp", lambda e: e.dma_start(out=xs_t[0:NSMP, :], in_=T["xs_d"].ap()), writes=["xs_t"])
    for kc2 in range(4):
        S.dma("pool", lambda e, kc2=kc2: e.dma_start(
            out=w_in[:, 2 * kc2:2 * kc2 + 2, :],
            in_=T["w_in_d"].ap()[kc2 * 256:(kc2 + 1) * 256, :].rearrange("(k p) n -> p k n", p=128)), writes=["w_in"])
    S.dma("sp", lambda e: e.dma_start(out=binb, in_=bc_row(T["b_in_d"].ap(), QKV)), writes=["binb"])
    S.dma("sp", lambda e: e.dma_start(out=f0, in_=bass.AP(FD, 383 + 128, [[0, 128], [FW, 16]])), reads=["FD"], writes=["f0"])
    S.op("act", lambda e: e.activation(out=xsb, in_=xs_t, func=AF.Copy), reads=["xs_t"], writes=["xsb"])
    for kc in range(8):
        S.op("pe", lambda e, kc=kc: e.transpose(C["PSB"](6)[:, kc * 128:(kc + 1) * 128], xsb[:, kc * 128:(kc + 1) * 128], C["ident_b"]),
             reads=["xsb", "cbf"], writes=[("ps", 6)], signal=(kc == 7))
    S.op("dve", lambda e: e.tensor_copy(out=xsT, in_=C["PSB"](6).rearrange("p (a b) -> p a b", a=8)),
         reads=[("ps", 6)], writes=["xsT"])
    c0 = 0
    ci = 0
    while c0 < QKV:
        n = min(512, QKV - c0)
        pb = ci % 2
        for kc in range(8):
            S.op("pe", lambda e, kc=kc, pb=pb, c0=c0, n=n: e.matmul(
                PS(pb)[:, 0:n], lhsT=xsT[:, kc, :], rhs=w_in[:, kc, c0:c0 + n], start=(kc == 0), stop=(kc == 7)),
                reads=["xsT", "w_in"], writes=[("ps", pb)], signal=(kc == 7))
        S.op("dve", lambda e, pb=pb, c0=c0, n=n: e.tensor_tensor(out=qkv[:, c0:c0 + n], in0=PS(pb)[:, 0:n],
                                                                  in1=binb[:, c0:c0 + n], op=ALU.add),
             reads=[("ps", pb), "binb"], writes=["qkv"])
        c0 += n
        ci += 1
    S.dma("sp", lambda e: e.dma_start(out=ss_d[0].ap()[:, 127, :], in_=qkv[0:NSMP, 256:512]), reads=["qkv"], writes=["ssn0"])
    for g in range(3):
        L = GROUPS[g + 1][2]
        S.dma("sp", lambda e, g=g, L=L: e.dma_start(out=ss_d[g + 1].ap()[:, L - 1, 0:256],
                                                    in_=qkv[0:NSMP, 1280 + g * 256:1280 + (g + 1) * 256]),
              reads=["qkv"], writes=[("ssn", g, 0)])
        S.dma("sp", lambda e, g=g, L=L: e.dma_start(out=ss_d[g + 1].ap()[:, L - 1, 256:512],
                                                    in_=qkv[0:NSMP, 2048 + g * 256:2048 + (g + 1) * 256]),
              reads=["qkv"], writes=[("ssn", g, 1)])
    for (dst0, src0, n) in ((0, 0, 256), (256, 512, 512), (768, 1024, 256)):
        S.op("pe", lambda e, src0=src0, n=n: e.matmul(PS(2)[:, 0:n], lhsT=C["crep"][0:16, :], rhs=qkv[0:16, src0:src0 + n],
                                                      start=True, stop=True), reads=["qkv", "crep"], writes=[("ps", 2)])
        S.op("act", lambda e, dst0=dst0, n=n: e.activation(out=qrep[:, dst0:dst0 + n], in_=PS(2)[:, 0:n], func=AF.Identity, scale=0.125),
             reads=[("ps", 2)], writes=["qrep"])
    for gi, (d, maxd, L) in enumerate(GROUPS):
        H = 2 if gi == 0 else 4
        rowsz = 2 * H * 64
        ckv = ck[:, :, 0:rowsz]
        src = bass.AP(cache_d[gi], 0, [[16 * d * rowsz, 128], [d * rowsz, 16], [1, rowsz]])
        S.dma("sp", lambda e, ckv=ckv, src=src: e.dma_start(out=ckv, in_=src), writes=["ck"])
        for hq in range(4):
            c = (hq if gi == 0 else 4 + (gi - 1) * 4 + hq)
            S.dma("sp", lambda e, hq=hq, c=c: e.dma_start(
                out=ebs[:, hq, :], in_=bass.AP(T["FDS"], c * 2048, [[16, 128], [1, 16]])), reads=["FD"], writes=["ebs"])
        ck5 = ckv.rearrange("p k (a h c) -> p k a h c", a=2, h=H)
        for hq in range(4):
            kvh = hq // 2 if gi == 0 else hq
            qc = (hq * 64) if gi == 0 else (256 + ((gi - 1) * 4 + hq) * 64)
            kcol = (256 + kvh * 64) if gi == 0 else (1280 + ((gi - 1) * 4 + hq) * 64)
            vcol = (384 + kvh * 64) if gi == 0 else (2048 + ((gi - 1) * 4 + hq) * 64)
            c = (hq if gi == 0 else 4 + (gi - 1) * 4 + hq)
            S.op("dve", lambda e, kvh=kvh, qc=qc: e.tensor_tensor(
                out=prod, in0=ck5[:, :, 0, kvh, :], in1=qrep[:, qc:qc + 64].unsqueeze(1).broadcast_to([128, 16, 64]), op=ALU.mult),
                reads=["ck", "qrep"], writes=["prod"])
            S.op("dve", lambda e, hq=hq: e.reduce_sum(out=sc[:, hq, :], in_=prod, axis=AX.X), reads=["prod"], writes=["sc"])
            S.op("act", lambda e, hq=hq: e.activation(out=sc[:, hq, :], in_=sc[:, hq, :], func=AF.Exp), reads=["sc"], writes=["sc"])
            S.op("dve", lambda e, hq=hq: e.tensor_tensor(out=sc[:, hq, :], in0=sc[:, hq, :], in1=ebs[:, hq, :], op=ALU.mult),
                 reads=["sc", "ebs"], writes=["sc"])
            S.op("dve", lambda e, hq=hq: e.reduce_sum(out=part[:, hq, 64:65], in_=sc[:, hq, :], axis=AX.X), reads=["sc"], writes=["part"])
            S.op("dve", lambda e, kvh=kvh, hq=hq: e.tensor_tensor(
                out=prod.rearrange("p k c -> p c k"), in0=ck5[:, :, 1, kvh, :].rearrange("p k c -> p c k"),
                in1=sc[:, hq, :].unsqueeze(1).broadcast_to([128, 64, 16]), op=ALU.mult),
                reads=["ck", "sc"], writes=["prod"])
            S.op("dve", lambda e, hq=hq: e.reduce_sum(out=part[:, hq, 0:64], in_=prod.rearrange("p k c -> p c k"), axis=AX.X),
                 reads=["prod"], writes=["part"])
            S.op("dve", lambda e, qc=qc, kcol=kcol, hq=hq: e.tensor_tensor(
                out=pr16[0:16, :], in0=qkv[0:16, (qc if gi == 0 else 512 + qc - 256):(qc if gi == 0 else 512 + qc - 256) + 64],
                in1=qkv[0:16, kcol:kcol + 64], op=ALU.mult), reads=["qkv"], writes=["pr16"])
            S.op("dve", lambda e, hq=hq: e.reduce_sum(out=snew[0:16, hq:hq + 1], in_=pr16[0:16, :], axis=AX.X), reads=["pr16"], writes=["snew"])
            S.op("act", lambda e, hq=hq: e.activation(out=enew[0:16, hq:hq + 1], in_=snew[0:16, hq:hq + 1], func=AF.Exp, scale=0.125),
                 reads=["snew"], writes=["enew"])
            S.op("dve", lambda e, hq=hq, c=c: e.tensor_tensor(out=enew[0:16, hq:hq + 1], in0=enew[0:16, hq:hq + 1],
                                                               in1=f0[0:16, c:c + 1], op=ALU.mult), reads=["enew", "f0"], writes=["enew"])
        S.op("pe", lambda e: e.matmul(PS(3)[0:16, 0:260], lhsT=C["repT"], rhs=part.rearrange("p a b -> p (a b)"),
                                      start=True, stop=True), reads=["part", "c128"], writes=[("ps", 3)])
        S.op("dve", lambda e: e.tensor_copy(out=tot[0:16].rearrange("p a b -> p (a b)"), in_=PS(3)[0:16, 0:260]),
             reads=[("ps", 3)], writes=["tot"])
        for hq in range(4):
            kvh = hq // 2 if gi == 0 else hq
            vcol = (384 + kvh * 64) if gi == 0 else (2048 + ((gi - 1) * 4 + hq) * 64)
            S.op("dve", lambda e, hq=hq, vcol=vcol: e.scalar_tensor_tensor(
                out=tot[0:16, hq, 0:64], in0=qkv[0:16, vcol:vcol + 64], scalar=enew[0:16, hq:hq + 1], in1=tot[0:16, hq, 0:64],
                op0=ALU.mult, op1=ALU.add), reads=["qkv", "enew", "tot"], writes=["tot"])
            S.op("dve", lambda e, hq=hq: e.tensor_tensor(out=tot[0:16, hq, 64:65], in0=tot[0:16, hq, 64:65],
                                                          in1=enew[0:16, hq:hq + 1], op=ALU.add), reads=["enew", "tot"], writes=["tot"])
            if gi == 0:
                S.op("dve", lambda e, hq=hq: e.tensor_tensor(out=tot[0:16, hq, 64:65], in0=tot[0:16, hq, 64:65],
                                                              in1=C["expsink"][0:16, hq:hq + 1], op=ALU.add),
                     reads=["tot", "expsink"], writes=["tot"])
        if gi == 0:
            fin, base = tot, 0
        elif gi == 1:
            S.op("dve", lambda e: e.tensor_copy(out=totd[0:16], in_=tot[0:16]), reads=["tot"], writes=["totd"])
            fin = None
        else:
            S.op("dve", lambda e: e.tensor_tensor(out=totd[0:16], in0=totd[0:16], in1=tot[0:16], op=ALU.add),
                 reads=["tot", "totd"], writes=["totd"])
            fin, base = (totd, 4) if gi == 3 else (None, 0)
        if fin is not None:
            key = "tot" if gi == 0 else "totd"
            for hq in range(4):
                S.op("dve", lambda e, fin=fin, hq=hq: e.reciprocal(out=fin[0:16, hq, 64:65], in_=fin[0:16, hq, 64:65]),
                     reads=[key], writes=[key])
                S.op("dve", lambda e, fin=fin, hq=hq, base=base: e.tensor_scalar(
                    out=cat_s[0:16, base + hq, :], in0=fin[0:16, hq, 0:64], scalar1=fin[0:16, hq, 64:65], scalar2=None, op0=ALU.mult),
                    reads=[key], writes=["cat_s"])
    S.barrier()
    A.off = m0


_PROG = None


def kernel(x_prompt, x_sample, cache_swa_kv, cache_dil1_kv, cache_dil2_kv, cache_dil3_kv,
           rel_bias_table, w_in, b_in, attn_sinks, w_o, b_o, ln1_g, ln1_b,
           w_router, b_router, w_up, b_up, w_down, b_down, ln2_g, ln2_b):
    global _PROG
    f = lambda a: np.ascontiguousarray(np.asarray(a, dtype=np.float32))
    xp = f(x_prompt)
    B, SEQ, _ = xp.shape
    c128, coh, cval, crep = host_consts()
    shared = dict(
        table=f(rel_bias_table), w_in=f(w_in)[0], b_in=f(b_in)[0], sinks=f(attn_sinks)[0], w_o=f(w_o)[0],
        b_o=f(b_o)[0], ln1_g=f(ln1_g)[0], ln1_b=f(ln1_b)[0], ln2_g=f(ln2_g)[0], ln2_b=f(ln2_b)[0],
        w_r=f(w_router)[0], b_r=f(b_router)[0], w_up=f(w_up)[0][:NE_DECL], b_up=f(b_up)[0].reshape(NE * 16, 128),
        w_dn=f(w_down)[0][:NE_DECL], b_dn=f(b_down)[0], c128=c128, coh=coh, cval=cval, crep=crep)
    caches = [f(cache_swa_kv)[0], f(cache_dil1_kv)[0], f(cache_dil2_kv)[0], f(cache_dil3_kv)[0]]
    xs = f(x_sample)[:, 0, :]
    in_maps = []
    for c in range(NCORES):
        n, h = c // 2, c % 2
        xwin = np.zeros((HALO + SOWN, D), np.float32)
        xwin[HALO:] = xp[n, h * SOWN:(h + 1) * SOWN]
        fl = np.zeros((128, NUNIT), np.float32)
        fl[:, 1:] = 1.0
        if h == 1:
            xwin[:HALO] = xp[n, SOWN - HALO:SOWN]
            fl[:, 0] = 1.0
        m = dict(shared)
        m["xw"] = xwin
        m["flag"] = fl
        m["xs"] = np.ascontiguousarray(xs[c * NSMP:(c + 1) * NSMP])
        for nm, ca in zip(("c_swa", "c_d1", "c_d2", "c_d3"), caches):
            sl = ca[c * NSMP:(c + 1) * NSMP]
            m[nm] = np.ascontiguousarray(sl.reshape(NSMP, sl.shape[1], -1))
        in_maps.append(m)
    if _PROG is None:
        _PROG = build_program()
    res = run_bass_kernel_spmd(_PROG, in_maps, core_ids=list(range(NCORES)))
    R = res.results
    y_prompt = np.stack([np.concatenate([R[2 * n]["yp"], R[2 * n + 1]["yp"]], axis=0) for n in range(B)]).astype(np.float32)
    y_sample = np.concatenate([R[c]["ys"] for c in range(NCORES)], axis=0)[:, None, :].astype(np.float32)
    pst = []
    for nm, H in (("ps_swa", 2), ("ps_d1", 4), ("ps_d2", 4), ("ps_d3", 4)):
        pst.append(np.stack([R[2 * n + 1][nm] for n in range(B)])[None].astype(np.float32))
    sst = []
    for nm, H in (("ss_swa", 2), ("ss_d1", 4), ("ss_d2", 4), ("ss_d3", 4)):
        a = np.concatenate([R[c][nm] for c in range(NCORES)], axis=0)
        sst.append(a.reshape(1, a.shape[0], a.shape[1], 2, H, 64).astype(np.float32))
    return (y_prompt, y_sample, pst[0], pst[1], pst[2], pst[3], sst[0], sst[1], sst[2], sst[3])
```

```python
import os
import numpy as np
import concourse.bass as bass
import concourse.mybir as mybir
from concourse.bass_utils import run_bass_kernel_spmd

F32 = mybir.dt.float32
BF16 = mybir.dt.bfloat16
I32 = mybir.dt.int32
U8 = mybir.dt.uint8
ALU = mybir.AluOpType
AF = mybir.ActivationFunctionType
AX = mybir.AxisListType

NCORES = 8
D = 1024
QKV = 2816
NE = 32
CAP = 640
UNIT = 2048
HALO = 2048
WIN = HALO + UNIT
NUNIT = 2
SOWN = UNIT * NUNIT
NSMP = 16
NT = SOWN // 128 + 1
ALPHA = float(2.0 ** 0.25)
EPS = 1e-5
BIG = 1.0e6
XGROWS = NE * CAP
FW = 512
ARENA = 186 * 1024
STAGE = int(os.environ.get("K_STAGE", "99"))
NE_DECL = NE if STAGE >= 5 else 1

GROUPS = [
    (1, 127, 128), (1, 128, 128), (4, 128, 512), (16, 128, 2048)]


def t5_bucket_np(n):
    n = np.maximum(np.asarray(n, np.int64), 0)
    ratio = np.log(np.maximum(n, 16).astype(np.float32) / np.float32(16)) / np.float32(np.log(2048 / 16))
    large = 16 + (ratio.astype(np.float32) * np.float32(16)).astype(np.int32)
    return np.where(n < 16, n, np.minimum(large, 31)).astype(np.int64)


def host_consts():
    c128 = np.zeros((128, 3 * 128 + 32 + 2 + 16), np.float32)
    c128[:, 0:128] = np.eye(128, dtype=np.float32)
    c128[:, 128:256] = np.triu(np.ones((128, 128), np.float32), 1)
    c128[:, 256:384] = 1.0
    c128[:, 384:416] = (np.arange(NE, dtype=np.float32) * CAP)[None, :]
    c128[:NSMP, 416] = 1.0
    c128[NSMP:, 417] = BIG
    for p in range(128):
        c128[p, 418 + p // 8] = 1.0
    oh = np.zeros((32, 4, FW), np.float32)
    valid = np.zeros((4, 4, FW), np.float32)
    for gi, (d, maxd, L) in enumerate(GROUPS):
        for u in range(383):
            dist = u - 127
            if 0 <= dist <= maxd:
                oh[t5_bucket_np(dist * d), gi, u] = 1.0
                valid[:, gi, u] = 1.0
        for j in range(129):
            o = 128 - j
            if o <= maxd:
                oh[t5_bucket_np(o * d), gi, 383 + j] = 1.0
                valid[:, gi, 383 + j] = 1.0
    rep = np.zeros((16, 128), np.float32)
    for p in range(128):
        rep[p // 8, p] = 1.0
    return c128, oh.reshape(32, 4 * FW), valid.reshape(4, 4 * FW), rep


class Lazy:
    def __init__(self, f):
        self.f = f


class _Rec:
    def __init__(self):
        self.call = None

    def __getattr__(self, name):
        def m(*args, **kwargs):
            assert self.call is None
            self.call = (name, args, kwargs)
            return self
        return m


def _bind(fn):
    r = _Rec()
    fn(r)
    name, args, kwargs = r.call

    def run(eng):
        kw = {k: (v.f() if isinstance(v, Lazy) else v) for k, v in kwargs.items()}
        return getattr(eng, name)(*args, **kw)
    return run


class Sched:
    ENG = ("pe", "act", "dve", "pool", "sp")

    def __init__(self, nc, esems, dsems, bgsems=()):
        self.nc = nc
        self.bgsem = list(bgsems)
        self.bgcnt = [0] * len(self.bgsem)
        self.bgnext = 0
        self.bgnext_pool = 0
        self.q = {e: [] for e in self.ENG}
        self.cnt = {e: 0 for e in self.ENG}
        self.esem = esems
        self.dsem = dsems
        self.dcnt = [0] * len(dsems)
        self.dnext = 0
        self.dnext_pool = 0
        self.seen = {e: {} for e in self.ENG}
        self.lastw = {}
        self.readers = {}
        self.pending_pe = False

    def _need(self, e, tok):
        k, v = tok
        if k == e and e == "pe":
            return
        if self.seen[e].get(k, 0) >= v:
            return
        self.seen[e][k] = v
        if isinstance(k, str):
            sem = self.esem[k]
        elif k[0] == "bg":
            sem = self.bgsem[k[1]]
        else:
            sem = self.dsem[k[1]]
        self.q[e].append(lambda eng, s=sem, vv=v: eng.wait_ge(s, vv))

    def _deps(self, e, reads, writes):
        toks = []
        for k in reads:
            if k in self.lastw:
                toks.append(self.lastw[k])
        for k in writes:
            if k in self.lastw:
                toks.append(self.lastw[k])
            toks.extend(self.readers.get(k, {}).values())
        for t in toks:
            self._need(e, t)

    def _record(self, tok, reads, writes):
        for k in reads:
            self.readers.setdefault(k, {})[tok[0]] = tok
        for k in writes:
            self.lastw[k] = tok
            self.readers[k] = {}

    def op(self, e, fn, reads=(), writes=(), signal=True):
        fn = _bind(fn)
        self._deps(e, reads, writes)
        tok = (e, self.cnt[e] + 1)
        if signal:
            self.cnt[e] += 1
            sem = self.esem[e]
            self.q[e].append(lambda eng, f=fn, s=sem: f(eng).then_inc(s, 1))
        else:
            assert e == "pe"
            self.q[e].append(lambda eng, f=fn: f(eng))
        self._record(tok, reads, writes)

    def dma(self, e, fn, reads=(), writes=()):
        fn = _bind(fn)
        self._deps(e, reads, writes)
        npool = 16
        if e == "pool":
            idx = self.dnext_pool
            self.dnext_pool = (self.dnext_pool + 1) % npool
        else:
            idx = npool + self.dnext
            self.dnext = (self.dnext + 1) % (len(self.dsem) - npool)
        if self.dcnt[idx] > 0:
            self._need(e, (("dma", idx), self.dcnt[idx] * 16))
        self.dcnt[idx] += 1
        tok = (("dma", idx), self.dcnt[idx] * 16)
        sem = self.dsem[idx]
        self.q[e].append(lambda eng, f=fn, s=sem: f(eng).then_inc(s, 16))
        self._record(tok, reads, writes)

    def dma_bg(self, e, fn, reads=()):
        fn = _bind(fn)
        self._deps(e, reads, ())
        half = len(self.bgsem) // 2
        if e == "pool":
            idx = half + self.bgnext_pool
            self.bgnext_pool = (self.bgnext_pool + 1) % half
        else:
            idx = self.bgnext
            self.bgnext = (self.bgnext + 1) % half
        self.bgcnt[idx] += 1
        sem = self.bgsem[idx]
        self.q[e].append(lambda eng, f=fn, s=sem: f(eng).then_inc(s, 16))

    def wait_bg(self, engines=None):
        for e in (engines or self.ENG):
            for i in range(len(self.bgsem)):
                if self.bgcnt[i] > 0:
                    self._need(e, (("bg", i), self.bgcnt[i] * 16))

    def barrier(self):
        for e in self.ENG:
            for o in ("pe", "act", "dve", "pool"):
                if o != e and self.cnt[o] > 0:
                    self._need(e, (o, self.cnt[o]))
            for i in range(len(self.dsem)):
                if self.dcnt[i] > 0:
                    self._need(e, (("dma", i), self.dcnt[i] * 16))
        self.lastw = {}
        self.readers = {}

    def replay(self, block):
        q = self.q

        @block.tensor
        def _(eng):
            for t in q["pe"]:
                t(eng)

        @block.scalar
        def _(eng):
            for t in q["act"]:
                t(eng)

        @block.vector
        def _(eng):
            for t in q["dve"]:
                t(eng)

        @block.gpsimd
        def _(eng):
            for t in q["pool"]:
                t(eng)

        @block.sync
        def _(eng):
            for t in q["sp"]:
                t(eng)


class Arena:
    def __init__(self, t, nbytes):
        self.t = t
        self.n = nbytes
        self.off = 0

    def alloc(self, shape, dt, nbytes_el):
        free = int(np.prod(shape[1:]))
        nb = free * nbytes_el
        nb = (nb + 63) // 64 * 64
        assert self.off + nb <= self.n, ("arena overflow", self.off, nb, self.n)
        v = self.t[:, self.off:self.off + free * nbytes_el].bitcast(dt)
        self.off += nb
        if len(shape) == 3:
            v = v.rearrange("p (a b) -> p a b", a=shape[1])
        elif len(shape) == 4:
            v = v.rearrange("p (a b c) -> p a b c", a=shape[1], b=shape[2])
        return v

    def f32(self, *shape):
        return self.alloc((128,) + shape, F32, 4)

    def bf(self, *shape):
        return self.alloc((128,) + shape, BF16, 2)

    def i32(self, *shape):
        return self.alloc((128,) + shape, I32, 4)


def build_program():
    nc = bass.Bass("TRN2", target_bir_lowering=False)

    def din(name, shape, dt=F32):
        return nc.dram_tensor(name, list(shape), dt, kind="ExternalInput")

    def dout(name, shape, dt=F32):
        return nc.dram_tensor(name, list(shape), dt, kind="ExternalOutput")

    def dint(name, shape, dt=F32):
        return nc.dram_tensor(name, list(shape), dt, kind="Internal")

    xw = din("xw", [HALO + SOWN, D])
    flag_d = din("flag", [128, NUNIT])
    xs_d = din("xs", [NSMP, D])
    cache_d = [din("c_swa", [NSMP, 128, 256]), din("c_d1", [NSMP, 128, 512]),
               din("c_d2", [NSMP, 512, 512]), din("c_d3", [NSMP, 2048, 512])]
    table_d = din("table", [32, 16])
    w_in_d = din("w_in", [D, QKV])
    b_in_d = din("b_in", [QKV])
    sinks_d = din("sinks", [4])
    w_o_d = din("w_o", [512, D])
    b_o_d = din("b_o", [D])
    ln1g_d = din("ln1_g", [D]); ln1b_d = din("ln1_b", [D])
    ln2g_d = din("ln2_g", [D]); ln2b_d = din("ln2_b", [D])
    w_r_d = din("w_r", [D, NE]); b_r_d = din("b_r", [NE])
    w_up_d = din("w_up", [NE_DECL, D, 2 * D]); b_up_d = din("b_up", [NE * 16, 128])
    w_dn_d = din("w_dn", [NE_DECL, D, D]); b_dn_d = din("b_dn", [NE, D])
    c128_d = din("c128", [128, 434]); coh_d = din("coh", [32, 4 * FW])
    cval_d = din("cval", [4, 4 * FW]); crep_d = din("crep", [16, 128])

    yp_d = dout("yp", [SOWN, D]); ys_d = dout("ys", [NSMP, D])
    ps_d = [dout("ps_swa", [128, 2, 2, 64]), dout("ps_d1", [128, 2, 4, 64]),
            dout("ps_d2", [512, 2, 4, 64]), dout("ps_d3", [2048, 2, 4, 64])]
    ss_d = [dout("ss_swa", [NSMP, 128, 256]), dout("ss_d1", [NSMP, 128, 512]),
            dout("ss_d2", [NSMP, 512, 512]), dout("ss_d3", [NSMP, 2048, 512])]

    X1 = dint("X1", [NT * 128, D])
    XG = dint("XG", [XGROWS, D], BF16)
    YS = dint("YS", [XGROWS + 1, D])
    FD = dint("FD", [16, FW])
    FDR = dint("FDR", [16, 128, FW])
    FDS = dint("FDS", [16, 16, 128])

    import contextlib
    with contextlib.ExitStack() as es:
        arena_t = es.enter_context(nc.sbuf_tensor("arena", [128, ARENA], U8))
        psb = [es.enter_context(nc.psum_tensor("psb%d" % i, [128, 512], F32)) for i in range(8)]
        esems = {e: es.enter_context(nc.semaphore("s_" + e)) for e in ("pe", "act", "dve", "pool")}
        dsems = [es.enter_context(nc.semaphore("d%d" % i)) for i in range(56)]
        bgsems = [es.enter_context(nc.semaphore("g%d" % i)) for i in range(8)]
        es.enter_context(nc.allow_non_contiguous_dma(reason="small strided constant loads"))
        S = Sched(nc, esems, dsems, bgsems)
        A = Arena(arena_t, ARENA)
        emit(nc, S, A, psb, locals())
        block = es.enter_context(nc.Block())
        S.replay(block)
    return nc


def emit(nc, S, A, psb, T):
    xw, flag_d, xs_d, cache_d, table_d = T["xw"], T["flag_d"], T["xs_d"], T["cache_d"], T["table_d"]
    w_in_d, b_in_d, sinks_d, w_o_d, b_o_d = T["w_in_d"], T["b_in_d"], T["sinks_d"], T["w_o_d"], T["b_o_d"]
    X1, XG, YS, FD = T["X1"], T["XG"], T["YS"], T["FD"]
    FDR, FDS = T["FDR"], T["FDS"]
    ps_d, ss_d = T["ps_d"], T["ss_d"]

    def PS(i):
        return psb[i][:, :]

    def PSB(i):
        return psb[i][:, :].bitcast(BF16)

    def bc_row(dram_ap_1d, n):
        return dram_ap_1d.unsqueeze(0).broadcast_to([128, n])

    c128 = A.f32(434)
    ident_f = c128[:, 0:128]
    ones_f = c128[:, 256:384]
    ecap = c128[:, 384:416]
    rowvalid = c128[:, 416:417]
    rowbig = c128[:, 417:418]
    repT = c128[:, 418:434]
    cbf = A.bf(384)
    ident_b = cbf[:, 0:128]
    triu_b = cbf[:, 128:256]
    ones_b = cbf[:, 256:384]
    crep = A.f32(128)
    flag = A.f32(NUNIT)
    bqk = A.f32(22)
    bkdup = A.f32(2)
    expsink = A.f32(4)
    G_all = A.f32(NT, NE)
    g4_all = A.f32(NT, 4)
    dsc_all = A.i32(NT, 4)
    dga_all = A.i32(NT, 4)
    cnt = A.f32(NE)
    wr = A.f32(8, NE)
    brb = A.f32(NE)
    zero_bf = A.bf(2048)
    persist_mark = A.off

    REGS = T["_regs"] = {}

    def _mkregs(eng):
        REGS["sc"] = eng.alloc_register("bc_sc")
        eng.reg_mov(REGS["sc"], XGROWS - 1)
        REGS["ga"] = eng.alloc_register("bc_ga")
        eng.reg_mov(REGS["ga"], XGROWS)
    S.q["pool"].append(_mkregs)
    S.dma("sp", lambda e: e.dma_start(out=c128, in_=T["c128_d"].ap()), writes=["c128"])
    S.dma("sp", lambda e: e.dma_start(out=crep[0:16, :], in_=T["crep_d"].ap()), writes=["crep"])
    S.dma("sp", lambda e: e.dma_start(out=flag, in_=flag_d.ap()), writes=["flag"])
    S.dma("sp", lambda e: e.dma_start(out=bqk, in_=b_in_d.ap().rearrange("(j p) -> p j", p=128)), writes=["bqk"])
    for p in range(2):
        for hh in range(2):
            S.dma("sp", lambda e, p=p, hh=hh: e.dma_start(
                out=bkdup[hh * 64:(hh + 1) * 64, p:p + 1],
                in_=b_in_d.ap()[256 + p * 64:256 + (p + 1) * 64].unsqueeze(1)), writes=["bkdup"])
    S.dma("sp", lambda e: e.dma_start(out=expsink, in_=bc_row(sinks_d.ap(), 4)), writes=["expsink"])
    S.dma("sp", lambda e: e.dma_start(out=wr, in_=T["w_r_d"].ap().rearrange("(k p) n -> p k n", p=128)), writes=["wr"])
    S.dma("sp", lambda e: e.dma_start(out=brb, in_=bc_row(T["b_r_d"].ap(), NE)), writes=["brb"])
    S.op("act", lambda e: e.activation(out=expsink, in_=expsink, func=AF.Exp), reads=["expsink"], writes=["expsink"])
    S.op("dve", lambda e: e.tensor_copy(out=cbf, in_=c128[:, 0:384]), reads=["c128"], writes=["cbf"])
    S.op("pool", lambda e: e.memset(cnt, 0.0), writes=["cnt"])
    S.op("pool", lambda e: e.memset(zero_bf, 0.0), writes=["zero_bf"])

    xg_v = XG.ap().rearrange("(a p r) d -> a p (r d)", p=128, r=2)
    for a in range(XGROWS // 256):
        S.dma_bg("pool", lambda e, a=a: e.dma_start(out=xg_v[a], in_=zero_bf), reads=["zero_bf"])
    S.dma_bg("pool", lambda e: e.dma_start(out=YS.ap()[XGROWS:XGROWS + 1, :].bitcast(BF16),
                                         in_=zero_bf[0:1, :]), reads=["zero_bf"])

    for gi, (d, maxd, L) in enumerate(GROUPS):
        nch = max(1, L // 256)
        rows = (L - 1)
        per = (rows + nch - 1) // nch
        for ch in range(nch):
            r0, r1 = ch * per, min(rows, (ch + 1) * per)
            if r0 >= r1:
                continue
            S.dma_bg("sp", lambda e, gi=gi, r0=r0, r1=r1: e.dma_start(
                out=ss_d[gi].ap()[:, r0:r1, :].rearrange("b r c -> b (r c)"),
                in_=cache_d[gi].ap()[:, r0 + 1:r1 + 1, :].rearrange("b r c -> b (r c)")))

    m0 = A.off
    tab = A.f32(16)
    coh = A.f32(4 * FW)
    cval = A.f32(4 * FW)
    ft = A.f32(4, FW)
    S.dma("sp", lambda e: e.dma_start(out=tab[0:32, :], in_=table_d.ap()), writes=["tab"])
    S.dma("sp", lambda e: e.dma_start(out=coh[0:32, :], in_=T["coh_d"].ap()), writes=["coh"])
    S.dma("sp", lambda e: e.dma_start(out=cval[0:4, :], in_=T["cval_d"].ap()), writes=["cval"])
    for gi in range(4):
        S.op("pe", lambda e, gi=gi: e.matmul(PS(gi)[0:4, :], lhsT=tab[0:32, gi * 4:gi * 4 + 4],
                                             rhs=coh[0:32, gi * FW:(gi + 1) * FW], start=True, stop=True),
             reads=["tab", "coh"], writes=[("ps", gi)])
        S.op("act", lambda e, gi=gi: e.activation(out=ft[0:4, gi, :], in_=PS(gi)[0:4, :], func=AF.Exp),
             reads=[("ps", gi)], writes=[("ft", gi)])
        S.op("dve", lambda e, gi=gi: e.tensor_tensor(out=ft[0:4, gi, :], in0=ft[0:4, gi, :],
                                                      in1=cval[0:4, gi * FW:(gi + 1) * FW], op=ALU.mult),
             reads=[("ft", gi), "cval"], writes=[("ft", gi)])
        S.dma("sp", lambda e, gi=gi: e.dma_start(out=FD.ap()[gi * 4:gi * 4 + 4, :], in_=ft[0:4, gi, :]),
              reads=[("ft", gi)], writes=[("FDw", gi)])
        S.dma("sp", lambda e, gi=gi: e.dma_start(out=FDR.ap()[gi * 4:gi * 4 + 4, :, :],
                                                 in_=ft[0:4, gi, :].unsqueeze(1).broadcast_to([4, 128, FW])),
              reads=[("ft", gi)], writes=[("FDRw", gi)])
        S.dma("sp", lambda e, gi=gi: e.dma_start(out=FDS.ap()[gi * 4:gi * 4 + 4, :, :],
                                                 in_=ft[0:4, gi, 383:383 + 128].unsqueeze(1).broadcast_to([4, 16, 128])),
              reads=[("ft", gi)], writes=[("FDSw", gi)])
    S.barrier()
    A.off = m0
    if STAGE == 1:
        S.barrier()
        S.wait_bg()
        return

    sample_phase(nc, S, A, PS, T, dict(c128=c128, ident_f=ident_f, ident_b=ident_b, repT=repT, crep=crep,
                                        expsink=expsink, bc_row=bc_row, PSB=PSB))
    cat_s = T["_cat_s"]
    xs_t = T["_xs_t"]
    S.barrier()
    if STAGE == 2:
        S.barrier()
        S.wait_bg()
        return

    jobs = []
    for p in range(2):
        jobs.append(dict(gi=0, d=1, maxd=127, qcol=p * 128, kcol=256 + p * 64, vcol=384 + p * 64, dup=True,
                         tcol=[2 * p, 2 * p + 1], slot=[2 * p, 2 * p + 1], first=True, last=True,
                         sink=[2 * p, 2 * p + 1], bq=p, bk=None, st_h=p, pair=p))
    for pr in range(2):
        for g in range(3):
            base = (g * 4 + 2 * pr) * 64
            jobs.append(dict(gi=g + 1, d=GROUPS[g + 1][0], maxd=128, qcol=512 + base, kcol=1280 + base,
                             vcol=2048 + base, dup=False, tcol=[4 + g * 4 + 2 * pr, 4 + g * 4 + 2 * pr + 1],
                             slot=[4 + 2 * pr, 4 + 2 * pr + 1], first=(g == 0), last=(g == 2), sink=None,
                             bq=(512 + base) // 128, bk=(1280 + base) // 128, st_h=2 * pr, pair=pr))

    pm = A.off
    xT = A.bf(8, WIN)
    catT = A.bf(8, UNIT)
    xst = A.f32(D)
    xbf = A.bf(D)
    sfull = [A.f32(256), A.f32(256)]
    wq = A.bf(8, 128)
    wkv = A.bf(8, 256)
    jm = A.off

    tile_ctx = dict(cnt=cnt, G_all=G_all, g4_all=g4_all, dsc_all=dsc_all, dga_all=dga_all, wr=wr, brb=brb,
                    ecap=ecap, rowvalid=rowvalid, rowbig=rowbig, ident_f=ident_f, ident_b=ident_b,
                    triu_b=triu_b, ones_b=ones_b, bc_row=bc_row, PSB=PSB)

    for u in range(NUNIT):
        for t in range(WIN // 128):
            S.dma("act", lambda e, t=t, u=u: e.dma_start(out=xst, in_=xw.ap()[u * UNIT + t * 128:u * UNIT + (t + 1) * 128, :]),
                  writes=["xst"])
            S.op("act", lambda e: e.activation(out=xbf, in_=xst, func=AF.Copy), reads=["xst"], writes=["xbf"])
            pb = 6 + (t % 2)
            for kc in range(8):
                S.op("pe", lambda e, kc=kc, pb=pb: e.transpose(PSB(pb)[:, kc * 128:(kc + 1) * 128],
                                                              xbf[:, kc * 128:(kc + 1) * 128], ident_b),
                     reads=["xbf", "cbf"], writes=[("ps", pb)], signal=(kc == 7))
            S.op("dve", lambda e, t=t, pb=pb: e.tensor_copy(
                out=xT[:, :, t * 128:(t + 1) * 128], in_=PSB(pb).rearrange("p (a b) -> p a b", a=8)),
                reads=[("ps", pb)], writes=[("xT", t)])
        xT_keys = [("xT", t) for t in range(WIN // 128)]

        for ji, jb in enumerate(jobs):
            A.off = jm
            d, gi = jb["d"], jb["gi"]
            halo_len = 128 * d
            T0 = HALO - halo_len
            nbo = UNIT // (128 * d)
            nb = nbo + 1
            QT = A.bf(UNIT)
            KT = A.bf(WIN)
            Vaug = A.bf(32, 2, 65)
            acc = A.f32(2, UNIT)
            EB = A.f32(2, 256)
            EBf = A.f32(2, 256)
            Et = [A.bf(512), A.bf(512)]
            Pt = [A.bf(512), A.bf(512)]
            kvb = A.f32(256)
            K = lambda name: (name, 0)
            S.dma("pool", lambda e, jb=jb: e.dma_start(
                out=wq, in_=w_in_d.ap()[:, jb["qcol"]:jb["qcol"] + 128].rearrange("(k p) n -> p k n", p=128)),
                writes=["wq"])
            if jb["dup"]:
                for hh in range(2):
                    S.dma("pool", lambda e, jb=jb, hh=hh: e.dma_start(
                        out=wkv[:, :, hh * 64:(hh + 1) * 64],
                        in_=w_in_d.ap()[:, jb["kcol"]:jb["kcol"] + 64].rearrange("(k p) n -> p k n", p=128)),
                        writes=["wkv"])
                    S.dma("pool", lambda e, jb=jb, hh=hh: e.dma_start(
                        out=wkv[:, :, 128 + hh * 64:128 + (hh + 1) * 64],
                        in_=w_in_d.ap()[:, jb["vcol"]:jb["vcol"] + 64].rearrange("(k p) n -> p k n", p=128)),
                        writes=["wkv"])
                    S.dma("sp", lambda e, jb=jb, hh=hh: e.dma_start(
                        out=kvb[:, hh * 64:(hh + 1) * 64], in_=bc_row(b_in_d.ap()[jb["kcol"]:jb["kcol"] + 64], 64)),
                        writes=["kvb"])
                    S.dma("sp", lambda e, jb=jb, hh=hh: e.dma_start(
                        out=kvb[:, 128 + hh * 64:128 + (hh + 1) * 64],
                        in_=bc_row(b_in_d.ap()[jb["vcol"]:jb["vcol"] + 64], 64)), writes=["kvb"])
            else:
                S.dma("pool", lambda e, jb=jb: e.dma_start(
                    out=wkv[:, :, 0:128],
                    in_=w_in_d.ap()[:, jb["kcol"]:jb["kcol"] + 128].rearrange("(k p) n -> p k n", p=128)),
                    writes=["wkv"])
                S.dma("pool", lambda e, jb=jb: e.dma_start(
                    out=wkv[:, :, 128:256],
                    in_=w_in_d.ap()[:, jb["vcol"]:jb["vcol"] + 128].rearrange("(k p) n -> p k n", p=128)),
                    writes=["wkv"])
                S.dma("sp", lambda e, jb=jb: e.dma_start(
                    out=kvb[:, 0:128], in_=bc_row(b_in_d.ap()[jb["kcol"]:jb["kcol"] + 128], 128)), writes=["kvb"])
                S.dma("sp", lambda e, jb=jb: e.dma_start(
                    out=kvb[:, 128:256], in_=bc_row(b_in_d.ap()[jb["vcol"]:jb["vcol"] + 128], 128)), writes=["kvb"])
            for hh in range(2):
                c = jb["tcol"][hh]
                S.dma("act", lambda e, c=c, hh=hh: e.dma_start(
                    out=EB[:, hh, 0:128], in_=bass.AP(FDR, c * 128 * FW + 255, [[FW - 1, 128], [1, 128]])),
                    reads=["FD"], writes=["EB"])
                S.dma("act", lambda e, c=c, hh=hh: e.dma_start(
                    out=EB[:, hh, 128:256], in_=bass.AP(FDR, c * 128 * FW + 127, [[FW - 1, 128], [1, 128]])),
                    reads=["FD"], writes=["EB"])
            S.op("pool", lambda e, u=u: e.tensor_scalar(out=EBf[:, :, 0:128], in0=EB[:, :, 0:128],
                                                         scalar1=flag[:, u:u + 1], scalar2=None, op0=ALU.mult),
                 reads=["EB", "flag"], writes=["EBf"])
            S.op("pool", lambda e: e.tensor_copy(out=EBf[:, :, 128:256], in_=EB[:, :, 128:256]),
                 reads=["EB"], writes=["EBf"])
            S.op("pool", lambda e: e.memset(Vaug[:, :, :, 64:65], 1.0), writes=["Vones"])
            bqc = bqk[:, jb["bq"]:jb["bq"] + 1]
            for c4 in range(UNIT // 512):
                pb = c4 % 2
                for kc in range(8):
                    S.op("pe", lambda e, kc=kc, pb=pb, c4=c4: e.matmul(
                        PS(pb), lhsT=wq[:, kc, :], rhs=xT[:, kc, HALO + c4 * 512:HALO + (c4 + 1) * 512],
                        start=(kc == 0), stop=(kc == 7)),
                        reads=["wq"] + xT_keys[16 + c4 * 4:16 + c4 * 4 + 4], writes=[("ps", pb)], signal=(kc == 7))
                S.op("dve", lambda e, pb=pb, c4=c4, bqc=bqc: e.tensor_scalar(
                    out=QT[:, c4 * 512:(c4 + 1) * 512], in0=PS(pb), scalar1=bqc, scalar2=0.125,
                    op0=ALU.add, op1=ALU.mult), reads=[("ps", pb), "bqk"], writes=["QT"])
            bkc = bkdup[:, jb["pair"]:jb["pair"] + 1] if jb["dup"] else bqk[:, jb["bk"]:jb["bk"] + 1]
            pos = T0
            ci = 0
            while pos < WIN:
                n = min(512, WIN - pos)
                pb = ci % 2
                for kc in range(8):
                    S.op("pe", lambda e, kc=kc, pb=pb, pos=pos, n=n: e.matmul(
                        PS(pb)[:, 0:n], lhsT=wkv[:, kc, 0:128], rhs=xT[:, kc, pos:pos + n],
                        start=(kc == 0), stop=(kc == 7)),
                        reads=["wkv"] + xT_keys[pos // 128:(pos + n + 127) // 128], writes=[("ps", pb)],
                        signal=(kc == 7))
                S.op("act", lambda e, pb=pb, pos=pos, n=n, bkc=bkc: e.activation(
                    out=KT[:, pos:pos + n], in_=PS(pb)[:, 0:n], func=AF.Identity, bias=bkc, scale=1.0),
                    reads=[("ps", pb), "bqk", "bkdup"], writes=["KT"])
                pos += n
                ci += 1
            for r in range(d):
                for ib in range(nb):
                    blk = r * nb + ib
                    pb = 2 + (blk % 2)
                    start = T0 + r + d * 128 * ib
                    for kc in range(8):
                        S.op("pe", lambda e, kc=kc, pb=pb, start=start, d=d: e.matmul(
                            PS(pb)[:, 0:256], lhsT=xT[:, kc, start:start + 127 * d + 1:d], rhs=wkv[:, kc, :],
                            start=(kc == 0), stop=(kc == 7)),
                            reads=["wkv"] + xT_keys[start // 128:(start + 128 * d + 127) // 128],
                            writes=[("ps", pb)], signal=(kc == 7))
                    st_ib = nbo
                    is_state = (u == NUNIT - 1 and ib == st_ib and not os.environ.get("K_NOSTATE"))
                    if not is_state:
                        S.op("dve", lambda e, pb=pb, blk=blk: e.tensor_tensor(
                            out=Vaug[:, blk, :, 0:64], in0=PS(pb)[:, 128:256].rearrange("p (a b) -> p a b", a=2),
                            in1=kvb[:, 128:256].rearrange("p (a b) -> p a b", a=2), op=ALU.add),
                            reads=[("ps", pb), "kvb"], writes=[("V", blk)])
                    else:
                        sf = sfull[blk % 2]
                        S.op("dve", lambda e, pb=pb, sf=sf: e.tensor_tensor(out=sf, in0=PS(pb)[:, 0:256], in1=kvb, op=ALU.add),
                             reads=[("ps", pb), "kvb"], writes=[("sf", blk % 2)])
                        S.op("pool", lambda e, sf=sf, blk=blk: e.tensor_copy(
                            out=Vaug[:, blk, :, 0:64], in_=sf[:, 128:256].rearrange("p (a b) -> p a b", a=2)),
                            reads=[("sf", blk % 2)], writes=[("V", blk)])
                        for kv in range(2):
                            if jb["dup"]:
                                dst = ps_d[0].ap()[:, kv, jb["st_h"], :]
                                src = sf[:, kv * 128:kv * 128 + 64]
                            else:
                                dst = ps_d[gi].ap()[r::d, kv, jb["st_h"]:jb["st_h"] + 2, :].rearrange("p b c -> p (b c)")
                                src = sf[:, kv * 128:(kv + 1) * 128]
                            S.dma("pool", lambda e, dst=dst, src=src: e.dma_start(out=dst, in_=src),
                                  reads=[("sf", blk % 2)], writes=[("psd", gi, jb["pair"], r, kv)])
            qbs = [(r, ib) for r in range(d) for ib in range(1, nb)]
            for hh in range(2):
                hp = slice(hh * 64, (hh + 1) * 64)
                for bi in range(0, len(qbs), 2):
                    bsel = (bi // 2) % 2
                    stb = 4 + bsel
                    ob = 6 + bsel
                    ST = PS(stb).rearrange("p (a b) -> p a b", a=2)
                    for qi in range(2):
                        r, ib = qbs[bi + qi]
                        qs = r + d * 128 * (ib - 1)
                        qap = QT[hp, qs:qs + 127 * d + 1:d]
                        for side in range(2):
                            ks = T0 + r + d * 128 * (ib - 1 + side)
                            S.op("pe", lambda e, ST=ST, qi=qi, side=side, ks=ks, qap=qap, hp=hp, d=d: e.matmul(
                                ST[:, qi, side * 128:(side + 1) * 128], lhsT=KT[hp, ks:ks + 127 * d + 1:d], rhs=qap,
                                start=True, stop=True),
                                reads=["QT", "KT"], writes=[("ps", stb)], signal=(qi == 1 and side == 1))
                    E = Et[bsel]
                    S.op("act", lambda e, E=E, stb=stb: e.activation(out=E, in_=PS(stb), func=AF.Exp),
                         reads=[("ps", stb)], writes=[("E", bsel)])
                    Pq = Pt[bsel].rearrange("p (a b) -> p a b", a=2)
                    Ev = E.rearrange("p (a b) -> p a b", a=2)
                    for qi in range(2):
                        r, ib = qbs[bi + qi]
                        ebt = EBf if ib == 1 else EB
                        S.op("pool" if qi == 0 else "dve", lambda e, Pq=Pq, Ev=Ev, qi=qi, ebt=ebt, hh=hh: e.tensor_tensor(
                            out=Pq[:, qi, :], in0=Ev[:, qi, :], in1=ebt[:, hh, :], op=ALU.mult),
                            reads=[("E", bsel), "EB", "EBf"], writes=[("P", bsel, qi)])
                    OT = PS(ob).rearrange("p (a b) -> p a b", a=4)
                    for qi in range(2):
                        r, ib = qbs[bi + qi]
                        for side in range(2):
                            blk = r * nb + ib - 1 + side
                            S.op("pe", lambda e, OT=OT, qi=qi, side=side, blk=blk, Pq=Pq, hh=hh: e.matmul(
                                OT[0:65, qi, :], lhsT=Vaug[:, blk, hh, :], rhs=Pq[:, qi, side * 128:(side + 1) * 128],
                                start=(side == 0), stop=(side == 1)),
                                reads=[("V", blk), "Vones", ("P", bsel, qi)], writes=[("ps", ob)],
                                signal=(qi == 1 and side == 1))
                    for qi in range(2):
                        r, ib = qbs[bi + qi]
                        qs = r + d * 128 * (ib - 1)
                        dst = acc[0:65, hh, qs:qs + 127 * d + 1:d]
                        if jb["first"]:
                            S.op("act", lambda e, dst=dst, OT=OT, qi=qi: e.activation(out=dst, in_=OT[0:65, qi, :], func=AF.Identity),
                                 reads=[("ps", ob)], writes=[("acc", hh)])
                        else:
                            S.op("dve", lambda e, dst=dst, OT=OT, qi=qi: e.tensor_tensor(
                                out=dst, in0=OT[0:65, qi, :], in1=dst, op=ALU.add),
                                reads=[("ps", ob)], writes=[("acc", hh)])
            if jb["last"]:
                for hh in range(2):
                    slot = jb["slot"][hh]
                    den = acc[64:65, hh, :]
                    if jb["sink"] is not None:
                        si = jb["sink"][hh]
                        S.op("dve", lambda e, den=den, si=si: e.tensor_scalar(
                            out=den, in0=den, scalar1=expsink[64:65, si:si + 1], scalar2=None, op0=ALU.add),
                            reads=[("acc", hh), "expsink"], writes=[("acc", hh)])
                    S.op("dve", lambda e, den=den: e.reciprocal(out=den, in_=den), reads=[("acc", hh)], writes=[("acc", hh)])
                    for c4 in range(UNIT // 512):
                        pb = c4 % 2
                        S.op("pe", lambda e, pb=pb, c4=c4, hh=hh: e.matmul(
                            PS(pb)[0:64, :], lhsT=ones_f[64:65, 0:64], rhs=acc[64:65, hh, c4 * 512:(c4 + 1) * 512],
                            start=True, stop=True), reads=[("acc", hh), "c128"], writes=[("ps", pb)])
                        S.op("dve", lambda e, pb=pb, c4=c4, hh=hh, slot=slot: e.tensor_tensor(
                            out=catT[0:64, slot, c4 * 512:(c4 + 1) * 512], in0=acc[0:64, hh, c4 * 512:(c4 + 1) * 512],
                            in1=PS(pb)[0:64, :], op=ALU.mult), reads=[("ps", pb), ("acc", hh)], writes=[("catT", slot)])
        if STAGE == 3:
            S.barrier()
            S.wait_bg()
            return
        S.barrier()
        S.wait_bg()
        A.off = jm
        if STAGE == 32 and u == 1:
            return
        post_tiles(nc, S, A, PS, T, tile_ctx, catT, u, list(range(UNIT // 128)), xst)
        S.barrier()
        if STAGE == 31 or (STAGE == 33 and u == 1):
            S.wait_bg()
            return
    A.off = jm
    post_tiles(nc, S, A, PS, T, tile_ctx, None, None, [None], xst, sample=(cat_s, xs_t, catT))
    S.barrier()
    A.off = persist_mark
    if STAGE == 4:
        S.barrier()
        S.wait_bg()
        return

    moe_phase(nc, S, A, PS, PSB, T, ident_b)
    S.barrier()
    A.off = persist_mark
    if STAGE == 5:
        S.barrier()
        S.wait_bg()
        return
    combine_phase(nc, S, A, PS, T, tile_ctx)
    S.barrier()
    S.wait_bg()


def layer_norm_tile(S, A_tiles, z, g_b, b_b, out, key_in, key_out):
    junk, st = A_tiles["junk"], A_tiles["st"]
    S.op("act", lambda e: e.activation(out=junk, in_=z, func=AF.Identity, accum_out=st[:, 0:1]),
         reads=[key_in], writes=["ln_junk", "ln_st0"])
    S.op("act", lambda e: e.activation(out=junk, in_=z, func=AF.Square, accum_out=st[:, 1:2]),
         reads=[key_in], writes=["ln_junk", "ln_st1"])
    S.op("dve", lambda e: e.tensor_scalar(out=st[:, 2:3], in0=st[:, 0:1], scalar1=1.0 / D, scalar2=None, op0=ALU.mult),
         reads=["ln_st0"], writes=["ln_st2"])
    S.op("dve", lambda e: e.tensor_tensor(out=st[:, 3:4], in0=st[:, 2:3], in1=st[:, 2:3], op=ALU.mult),
         reads=["ln_st2"], writes=["ln_st3"])
    S.op("dve", lambda e: e.scalar_tensor_tensor(out=st[:, 4:5], in0=st[:, 1:2], scalar=1.0 / D, in1=st[:, 3:4],
                                                  op0=ALU.mult, op1=ALU.subtract),
         reads=["ln_st1", "ln_st3"], writes=["ln_st4"])
    S.op("dve", lambda e: e.tensor_scalar(out=st[:, 4:5], in0=st[:, 4:5], scalar1=EPS, scalar2=None, op0=ALU.add),
         reads=["ln_st4"], writes=["ln_st4"])
    S.op("act", lambda e: e.activation(out=st[:, 5:6], in_=st[:, 4:5], func=AF.Ln), reads=["ln_st4"], writes=["ln_st5"])
    S.op("act", lambda e: e.activation(out=st[:, 5:6], in_=st[:, 5:6], func=AF.Exp, scale=-0.5), reads=["ln_st5"], writes=["ln_st5"])
    S.op("dve", lambda e: e.scalar_tensor_tensor(out=st[:, 6:7], in0=st[:, 2:3], scalar=-1.0, in1=st[:, 5:6],
                                                  op0=ALU.mult, op1=ALU.mult),
         reads=["ln_st2", "ln_st5"], writes=["ln_st6"])
    S.op("act", lambda e: e.activation(out=junk, in_=z, func=AF.Identity, bias=st[:, 6:7], scale=st[:, 5:6]),
         reads=[key_in, "ln_st5", "ln_st6"], writes=["ln_junk"])
    S.op("pool", lambda e: e.tensor_tensor(out=junk, in0=junk, in1=g_b, op=ALU.mult),
         reads=["ln_junk", "lnconst"], writes=["ln_junk"])
    S.op("pool", lambda e: e.tensor_tensor(out=out, in0=junk, in1=b_b, op=ALU.add),
         reads=["ln_junk", "lnconst"], writes=[key_out])


def post_tiles(nc, S, A, PS, T, C, catT, u, tiles, xst, sample=None):
    bc_row = C["bc_row"]
    X1, XG = T["X1"], T["XG"]
    w_o = A.bf(8, D)
    bo_b = A.f32(D)
    g_b = A.f32(D)
    b_b = A.f32(D)
    z = A.f32(D)
    junk = A.f32(D)
    x1 = A.f32(D)
    x1bf = A.bf(D)
    x1T = A.f32(8, 128)
    st = A.f32(8)
    lg = A.f32(NE)
    m8 = A.f32(8)
    mask = A.f32(NE)
    mask_b = A.bf(NE)
    ex = A.f32(NE)
    ssum = A.f32(2)
    posc = A.f32(NE)
    big = A.f32(NE)
    dst_f = A.f32(8)
    scr = A.f32(NE)
    S.dma("pool", lambda e: e.dma_start(out=w_o[0:64, :, :], in_=T["w_o_d"].ap().rearrange("(s p) n -> p s n", p=64)),
          writes=["w_o"])
    S.dma("sp", lambda e: e.dma_start(out=bo_b, in_=bc_row(T["b_o_d"].ap(), D)), writes=["lnconst"])
    S.dma("sp", lambda e: e.dma_start(out=g_b, in_=bc_row(T["ln1g_d"].ap(), D)), writes=["lnconst"])
    S.dma("sp", lambda e: e.dma_start(out=b_b, in_=bc_row(T["ln1b_d"].ap(), D)), writes=["lnconst"])
    if sample is not None:
        cat_s, xs_t, catT_buf = sample
        catT = catT_buf
        for slot in range(8):
            S.op("pe", lambda e, slot=slot: e.transpose(PS(4 + slot // 4)[0:64, (slot % 4) * 128:(slot % 4) * 128 + 128],
                                                        cat_s[:, slot, :], C["ident_f"]),
                 reads=["cat_s", "c128"], writes=[("ps", 4 + slot // 4)], signal=(slot % 4 == 3))
        for half in range(2):
            S.op("dve", lambda e, half=half: e.tensor_copy(
                out=catT[0:64, half * 4:half * 4 + 4, 0:128], in_=PS(4 + half)[0:64, :].rearrange("p (a b) -> p a b", a=4)),
                reads=[("ps", 4 + half)], writes=[("catT", "s")])
    for tl in tiles:
        if sample is None:
            ti = u * (UNIT // 128) + tl
            tok0 = tl * 128
            S.dma("sp", lambda e, ti=ti: e.dma_start(out=xst, in_=T["xw"].ap()[HALO + ti * 128:HALO + (ti + 1) * 128, :]),
                  writes=["xst"])
            xin = xst
        else:
            ti = NT - 1
            tok0 = 0
            xin = sample[1]
        for half in range(2):
            for slot in range(8):
                S.op("pe", lambda e, half=half, slot=slot, tok0=tok0: e.matmul(
                    PS(half), lhsT=catT[0:64, slot, tok0:tok0 + 128], rhs=w_o[0:64, slot, half * 512:(half + 1) * 512],
                    start=(slot == 0), stop=(slot == 7)),
                    reads=["w_o"] + [("catT", s) for s in list(range(8)) + ["s"]], writes=[("ps", half)], signal=(slot == 7))
        S.op("pool", lambda e, xin=xin: e.tensor_scalar(out=z, in0=xin, scalar1=ALPHA, scalar2=None, op0=ALU.mult),
             reads=["xst", "xs_t"], writes=["z"])
        S.op("pool", lambda e: e.tensor_tensor(out=z, in0=z, in1=bo_b, op=ALU.add), reads=["z", "lnconst"], writes=["z"])
        for half in range(2):
            S.op("dve", lambda e, half=half: e.tensor_tensor(out=z[:, half * 512:(half + 1) * 512],
                                                              in0=z[:, half * 512:(half + 1) * 512], in1=PS(half), op=ALU.add),
                 reads=["z", ("ps", half)], writes=["z"])
        layer_norm_tile(S, dict(junk=junk, st=st), z, g_b, b_b, x1, "z", "x1")
        S.dma("pool", lambda e, ti=ti: e.dma_start(out=X1.ap()[ti * 128:(ti + 1) * 128, :], in_=x1), reads=["x1"], writes=[("X1", ti)])
        S.op("act", lambda e: e.activation(out=x1bf, in_=x1, func=AF.Copy), reads=["x1"], writes=["x1bf"])
        for kc in range(8):
            S.op("pe", lambda e, kc=kc: e.transpose(PS(2 + kc // 4)[:, (kc % 4) * 128:(kc % 4) * 128 + 128],
                                                    x1[:, kc * 128:(kc + 1) * 128], C["ident_f"]),
                 reads=["x1", "c128"], writes=[("ps", 2 + kc // 4)], signal=(kc % 4 == 3))
        for half in range(2):
            S.op("act", lambda e, half=half: e.activation(
                out=x1T[:, half * 4:half * 4 + 4, :], in_=PS(2 + half).rearrange("p (a b) -> p a b", a=4), func=AF.Identity),
                reads=[("ps", 2 + half)], writes=[("x1T", half)])
        for kc in range(8):
            S.op("pe", lambda e, kc=kc: e.matmul(PS(4)[:, 0:NE], lhsT=x1T[:, kc, :], rhs=C["wr"][:, kc, :],
                                                 start=(kc == 0), stop=(kc == 7)),
                 reads=[("x1T", kc // 4), "wr"], writes=[("ps", 4)], signal=(kc == 7))
        S.op("dve", lambda e: e.tensor_tensor(out=lg, in0=PS(4)[:, 0:NE], in1=C["brb"], op=ALU.add),
             reads=[("ps", 4), "brb"], writes=["lg"])
        S.op("dve", lambda e: e.max(out=m8, in_=lg), reads=["lg"], writes=["m8"])
        S.op("dve", lambda e: e.tensor_scalar(out=mask, in0=lg, scalar1=m8[:, 3:4], scalar2=None, op0=ALU.is_ge),
             reads=["lg", "m8"], writes=["mask"])
        if sample is not None:
            S.op("dve", lambda e: e.tensor_scalar(out=mask, in0=mask, scalar1=C["rowvalid"], scalar2=None, op0=ALU.mult),
                 reads=["mask", "c128"], writes=["mask"])
        S.op("dve", lambda e: e.tensor_scalar(out=ssum[:, 0:1], in0=m8[:, 0:1], scalar1=-1.0, scalar2=None, op0=ALU.mult),
             reads=["m8"], writes=["nmax"])
        S.op("act", lambda e: e.activation(out=ex, in_=lg, func=AF.Exp, bias=ssum[:, 0:1], scale=1.0),
             reads=["lg", "nmax"], writes=["ex"])
        S.op("dve", lambda e: e.tensor_tensor(out=ex, in0=ex, in1=mask, op=ALU.mult), reads=["ex", "mask"], writes=["ex"])
        S.op("dve", lambda e: e.reduce_sum(out=ssum[:, 1:2], in_=ex, axis=AX.X), reads=["ex"], writes=["esum"])
        if sample is not None:
            S.op("dve", lambda e: e.tensor_tensor(out=ssum[:, 1:2], in0=ssum[:, 1:2], in1=C["rowbig"], op=ALU.add),
                 reads=["esum", "c128"], writes=["esum"])
        S.op("dve", lambda e: e.reciprocal(out=ssum[:, 1:2], in_=ssum[:, 1:2]), reads=["esum"], writes=["esum"])
        Gt = C["G_all"][:, ti, :]
        S.op("dve", lambda e, Gt=Gt: e.tensor_scalar(out=Gt, in0=ex, scalar1=ssum[:, 1:2], scalar2=None, op0=ALU.mult),
             reads=["ex", "esum"], writes=["G"])
        S.op("act", lambda e: e.activation(out=mask_b, in_=mask, func=AF.Copy), reads=["mask"], writes=["mask_b"])
        S.op("pe", lambda e: e.matmul(PS(5)[:, 0:NE], lhsT=C["triu_b"], rhs=mask_b, start=True, stop=True),
             reads=["mask_b", "cbf"], writes=[("ps", 5)])
        S.op("pe", lambda e: e.matmul(PS(5)[:, 64:64 + NE], lhsT=C["ones_b"], rhs=mask_b, start=True, stop=True),
             reads=["mask_b", "cbf"], writes=[("ps", 5)])
        S.op("dve", lambda e: e.tensor_tensor(out=posc, in0=PS(5)[:, 0:NE], in1=C["cnt"], op=ALU.add),
             reads=[("ps", 5), "cnt"], writes=["posc"])
        S.op("dve", lambda e: e.tensor_tensor(out=C["cnt"], in0=PS(5)[:, 64:64 + NE], in1=C["cnt"], op=ALU.add),
             reads=[("ps", 5), "posc"], writes=["cnt"])
        S.op("dve", lambda e: e.tensor_scalar(out=big, in0=posc, scalar1=float(CAP) - 0.5, scalar2=BIG, op0=ALU.is_gt, op1=ALU.mult),
             reads=["posc"], writes=["big"])
        S.op("dve", lambda e: e.tensor_tensor(out=posc, in0=posc, in1=C["ecap"], op=ALU.add), reads=["posc", "c128"], writes=["posc"])
        S.op("dve", lambda e: e.tensor_tensor(out=posc, in0=posc, in1=big, op=ALU.add), reads=["posc", "big"], writes=["posc"])
        if sample is not None:
            S.op("dve", lambda e: e.tensor_scalar(out=posc, in0=posc, scalar1=C["rowbig"], scalar2=None, op0=ALU.add),
                 reads=["posc", "c128"], writes=["posc"])
        for k in range(4):
            S.op("dve", lambda e, k=k: e.scalar_tensor_tensor(
                out=scr, in0=lg, scalar=m8[:, k:k + 1], in1=posc, op0=ALU.is_equal, op1=ALU.mult, accum_out=dst_f[:, k:k + 1]),
                reads=["lg", "m8", "posc"], writes=["scr", ("dstf", k)])
            S.op("dve", lambda e, k=k, Gt=Gt, ti=ti: e.scalar_tensor_tensor(
                out=scr, in0=lg, scalar=m8[:, k:k + 1], in1=Gt, op0=ALU.is_equal, op1=ALU.mult,
                accum_out=C["g4_all"][:, ti, k:k + 1]),
                reads=["lg", "m8", "G"], writes=["scr", "g4"])
        S.op("dve", lambda e, ti=ti: e.tensor_copy(out=C["dsc_all"][:, ti, :], in_=dst_f[:, 0:4]),
             reads=[("dstf", k) for k in range(4)], writes=["dsc"])
        S.op("dve", lambda e: e.tensor_scalar(out=dst_f[:, 4:8], in0=dst_f[:, 0:4], scalar1=float(XGROWS), scalar2=None, op0=ALU.min),
             reads=[("dstf", k) for k in range(4)], writes=["dstf2"])
        S.op("dve", lambda e, ti=ti: e.tensor_copy(out=C["dga_all"][:, ti, :], in_=dst_f[:, 4:8]),
             reads=["dstf2"], writes=["dga"])
        for k in range(4):
            S.dma("pool", lambda e, k=k, ti=ti: e.indirect_dma_start(
                out=XG.ap(), out_offset=bass.IndirectOffsetOnAxis(ap=C["dsc_all"][:, ti, k:k + 1], axis=0),
                in_=x1bf, in_offset=None, bounds_check=Lazy(lambda: T["_regs"]["sc"]), oob_is_err=False),
                reads=["x1bf", "dsc", "XG"], writes=[("XGs", ti, k)])


def moe_phase(nc, S, A, PS, PSB, T, ident_b):
    XG, YS = T["XG"], T["YS"]
    w_up_d, w_dn_d = T["w_up_d"], T["w_dn_d"]
    wu = [A.bf(8, 2 * D), A.bf(8, 2 * D)]
    wd = [A.bf(8, D), A.bf(8, D)]
    xg = [A.bf(CAP // 128, D), A.bf(CAP // 128, D)]
    xgT = [A.bf(8, CAP), A.bf(8, CAP)]
    aT = A.bf(8, CAP)
    gt = [A.f32(512), A.f32(512)]
    sg = [A.f32(512), A.f32(512)]
    t2 = [A.f32(512), A.f32(512)]
    yst = [A.f32(D), A.f32(D)]
    bupT = A.f32(NE * 16)
    bst = A.f32(128)
    for a in range(4):
        S.dma("sp", lambda e, a=a: e.dma_start(out=bst, in_=T["b_up_d"].ap()[a * 128:(a + 1) * 128, :]), writes=["bst"])
        S.op("pe", lambda e, a=a: e.transpose(PS(0)[:, a * 128:(a + 1) * 128], bst, T["_ident_f"]),
             reads=["bst"], writes=[("ps", 0)])
        S.op("dve", lambda e, a=a: e.tensor_copy(out=bupT[:, a * 128:(a + 1) * 128], in_=PS(0)[:, a * 128:(a + 1) * 128]),
             reads=[("ps", 0)], writes=["bupT"])
    pieces = [(0, 512), (512, CAP - 512)]

    def load_w(ex):
        b = ex % 2
        for kc2 in range(4):
            S.dma("pool", lambda e, ex=ex, b=b, kc2=kc2: e.dma_start(
                out=wu[b][:, 2 * kc2:2 * kc2 + 2, :],
                in_=w_up_d.ap()[ex, kc2 * 256:(kc2 + 1) * 256, :].rearrange("(k p) n -> p k n", p=128)),
                writes=[("wu", b, kc2)])
        for kc2 in range(2):
            S.dma("pool", lambda e, ex=ex, b=b, kc2=kc2: e.dma_start(
                out=wd[b][:, 4 * kc2:4 * kc2 + 4, :],
                in_=w_dn_d.ap()[ex, kc2 * 512:(kc2 + 1) * 512, :].rearrange("(k p) n -> p k n", p=128)),
                writes=[("wd", b, kc2)])

    def load_xg(ex):
        S.dma("pool", lambda e, ex=ex: e.dma_start(
            out=xg[ex % 2], in_=XG.ap()[ex * CAP:(ex + 1) * CAP, :].rearrange("(a p) d -> p a d", p=128)),
            reads=["XGall"], writes=[("xg", ex % 2)])

    def transposes(ex):
        xb = ex % 2
        for a in range(CAP // 128):
            pb = 6 + (a % 2)
            for kc in range(8):
                S.op("pe", lambda e, a=a, kc=kc, pb=pb, xb=xb: e.transpose(PSB(pb)[:, kc * 128:(kc + 1) * 128],
                                                                           xg[xb][:, a, kc * 128:(kc + 1) * 128], ident_b),
                     reads=[("xg", xb)], writes=[("ps", pb)], signal=(kc == 7))
            S.op("act", lambda e, a=a, pb=pb, xb=xb: e.activation(
                out=xgT[xb][:, :, a * 128:(a + 1) * 128], in_=PSB(pb).rearrange("p (a b) -> p a b", a=8), func=AF.Copy),
                reads=[("ps", pb)], writes=[("xgT", xb, a)])

    load_w(0)
    load_xg(0)
    transposes(0)
    load_xg(1)
    for ex in range(NE):
        b = ex % 2
        if ex + 1 < NE:
            load_w(ex + 1)
        xgT_keys = [("xgT", b, a) for a in range(CAP // 128)]
        it = 0
        for (s0, sn) in pieces:
            for j in range(8):
                pg = (it % 2) * 2
                pl = pg + 1
                for (pb, fo) in ((pg, j), (pl, 8 + j)):
                    for kc in range(8):
                        S.op("pe", lambda e, pb=pb, fo=fo, kc=kc, s0=s0, sn=sn, b=b: e.matmul(
                            PS(pb)[:, 0:sn], lhsT=wu[b][:, kc, fo * 128:(fo + 1) * 128], rhs=xgT[b][:, kc, s0:s0 + sn],
                            start=(kc == 0), stop=(kc == 7)),
                            reads=[("wu", b, kc // 2)] + xgT_keys, writes=[("ps", pb)], signal=(kc == 7))
                bi = it % 2
                bg = bupT[:, ex * 16 + j:ex * 16 + j + 1]
                bl = bupT[:, ex * 16 + 8 + j:ex * 16 + 8 + j + 1]
                g_, s_, t_ = gt[bi][:, 0:sn], sg[bi][:, 0:sn], t2[bi][:, 0:sn]
                S.op("dve", lambda e, pg=pg, g_=g_, bg=bg, sn=sn: e.tensor_scalar(
                    out=g_, in0=PS(pg)[:, 0:sn], scalar1=bg, scalar2=7.0, op0=ALU.add, op1=ALU.min),
                    reads=[("ps", pg), "bupT"], writes=[("g", bi)])
                S.op("act", lambda e, g_=g_, s_=s_: e.activation(out=s_, in_=g_, func=AF.Sigmoid, scale=1.702),
                     reads=[("g", bi)], writes=[("sg", bi)])
                S.op("dve", lambda e, pl=pl, t_=t_, bl=bl, sn=sn: e.tensor_scalar(
                    out=t_, in0=PS(pl)[:, 0:sn], scalar1=bl, scalar2=7.0, op0=ALU.add, op1=ALU.min),
                    reads=[("ps", pl), "bupT"], writes=[("t2", bi)])
                S.op("pool", lambda e, t_=t_: e.tensor_scalar(
                    out=t_, in0=t_, scalar1=-7.0, scalar2=1.0, op0=ALU.max, op1=ALU.add),
                    reads=[("t2", bi)], writes=[("t2", bi)])
                S.op("pool", lambda e, g_=g_, s_=s_: e.tensor_tensor(out=g_, in0=g_, in1=s_, op=ALU.mult),
                     reads=[("g", bi), ("sg", bi)], writes=[("g", bi)])
                S.op("dve", lambda e, g_=g_, t_=t_, j=j, s0=s0, sn=sn: e.tensor_tensor(
                    out=aT[:, j, s0:s0 + sn], in0=g_, in1=t_, op=ALU.mult),
                    reads=[("g", bi), ("t2", bi)], writes=[("aT", j, s0)])
                it += 1
        if ex + 1 < NE:
            transposes(ex + 1)
            if ex + 2 < NE:
                load_xg(ex + 2)
        aT_keys = [("aT", j, s0) for j in range(8) for (s0, sn) in pieces]
        for a in range(CAP // 128):
            yt = yst[a % 2]
            for half in range(2):
                pb = 4 + half
                for fc in range(8):
                    S.op("pe", lambda e, a=a, half=half, fc=fc, pb=pb, b=b: e.matmul(
                        PS(pb), lhsT=aT[:, fc, a * 128:(a + 1) * 128], rhs=wd[b][:, fc, half * 512:(half + 1) * 512],
                        start=(fc == 0), stop=(fc == 7)),
                        reads=aT_keys + [("wd", b, fc // 4)], writes=[("ps", pb)], signal=(fc == 7))
                S.op("act" if half == 0 else "dve",
                     (lambda e, yt=yt, pb=pb, half=half: e.activation(out=yt[:, half * 512:(half + 1) * 512], in_=PS(pb), func=AF.Identity))
                     if half == 0 else
                     (lambda e, yt=yt, pb=pb, half=half: e.tensor_copy(out=yt[:, half * 512:(half + 1) * 512], in_=PS(pb))),
                     reads=[("ps", pb)], writes=[("yst", a % 2, half)])
            S.dma("act", lambda e, ex=ex, a=a, yt=yt: e.dma_start(
                out=YS.ap()[ex * CAP + a * 128:ex * CAP + (a + 1) * 128, :], in_=yt),
                reads=[("yst", a % 2, 0), ("yst", a % 2, 1)], writes=[("YS", ex, a)])


def combine_phase(nc, S, A, PS, T, C):
    bc_row = C["bc_row"]
    X1, YS = T["X1"], T["YS"]
    g_b = A.f32(D)
    b_b = A.f32(D)
    bdn = A.f32(D)
    yk = [A.f32(D) for _ in range(4)]
    x1 = A.f32(D)
    z = A.f32(D)
    junk = A.f32(D)
    out = A.f32(D)
    st = A.f32(8)
    GT = A.f32(128)
    S.dma("sp", lambda e: e.dma_start(out=g_b, in_=bc_row(T["ln2g_d"].ap(), D)), writes=["lnconst"])
    S.dma("sp", lambda e: e.dma_start(out=b_b, in_=bc_row(T["ln2b_d"].ap(), D)), writes=["lnconst"])
    S.dma("sp", lambda e: e.dma_start(out=bdn[0:NE, :], in_=T["b_dn_d"].ap()), writes=["bdn"])
    for ti in range(NT):
        S.dma("pool", lambda e, ti=ti: e.dma_start(out=x1, in_=X1.ap()[ti * 128:(ti + 1) * 128, :]), writes=["x1"])
        for k in range(4):
            S.dma("pool", lambda e, k=k, ti=ti: e.indirect_dma_start(
                out=yk[k], out_offset=None, in_=YS.ap(),
                in_offset=bass.IndirectOffsetOnAxis(ap=C["dga_all"][:, ti, k:k + 1], axis=0),
                bounds_check=Lazy(lambda: T["_regs"]["ga"]), oob_is_err=False), writes=[("yk", k)])
        S.op("pe", lambda e, ti=ti: e.transpose(PS(2)[0:NE, 0:128], C["G_all"][:, ti, :], C["ident_f"]),
             writes=[("ps", 2)])
        S.op("act", lambda e: e.activation(out=GT[0:NE, :], in_=PS(2)[0:NE, 0:128], func=AF.Identity),
             reads=[("ps", 2)], writes=["GT"])
        for half in range(2):
            S.op("pe", lambda e, half=half: e.matmul(PS(half), lhsT=GT[0:NE, :], rhs=bdn[0:NE, half * 512:(half + 1) * 512],
                                                     start=True, stop=True), reads=["GT", "bdn"], writes=[("ps", half)])
        S.op("pool", lambda e: e.tensor_scalar(out=z, in0=x1, scalar1=ALPHA, scalar2=None, op0=ALU.mult),
             reads=["x1"], writes=["z"])
        for k in range(4):
            S.op("dve", lambda e, k=k, ti=ti: e.scalar_tensor_tensor(
                out=z, in0=yk[k], scalar=C["g4_all"][:, ti, k:k + 1], in1=z, op0=ALU.mult, op1=ALU.add),
                reads=[("yk", k), "z"], writes=["z"])
        for half in range(2):
            S.op("dve", lambda e, half=half: e.tensor_tensor(out=z[:, half * 512:(half + 1) * 512],
                                                              in0=z[:, half * 512:(half + 1) * 512], in1=PS(half), op=ALU.add),
                 reads=["z", ("ps", half)], writes=["z"])
        layer_norm_tile(S, dict(junk=junk, st=st), z, g_b, b_b, out, "z", "out")
        if ti < NT - 1:
            S.dma("pool", lambda e, ti=ti: e.dma_start(out=T["yp_d"].ap()[ti * 128:(ti + 1) * 128, :], in_=out),
                  reads=["out"], writes=[("yp", ti)])
        else:
            S.dma("sp", lambda e: e.dma_start(out=T["ys_d"].ap(), in_=out[0:NSMP, :]), reads=["out"], writes=["ys"])


def sample_phase(nc, S, A, PS, T, C):
    bc_row = C["bc_row"]
    cache_d, ss_d = T["cache_d"], T["ss_d"]
    FD = T["FD"]
    cat_s = A.f32(8, 64)
    xs_t = A.f32(D)
    T["_cat_s"] = cat_s
    T["_xs_t"] = xs_t
    T["_ident_f"] = C["ident_f"]
    m0 = A.off
    w_in = A.bf(8, QKV)
    xsb = A.bf(D)
    xsT = A.bf(8, 128)
    binb = A.f32(QKV)
    qkv = A.f32(QKV)
    qrep = A.f32(1024)
    ck = A.f32(16, 512)
    ebs = A.f32(4, 16)
    prod = A.f32(16, 64)
    sc = A.f32(4, 16)
    part = A.f32(4, 65)
    tot = A.f32(4, 65)
    totd = A.f32(4, 65)
    snew = A.f32(16)
    enew = A.f32(16)
    f0 = A.f32(16)
    pr16 = A.f32(64)
    S.op("pool", lambda e: e.memset(xs_t, 0.0), writes=["xs_t"])
    S.op("pool", lambda e: e.memset(cat_s, 0.0), writes=["cat_s"])
    S.dma("sp", lambda e: e.dma_start(out=xs_t[0:NSMP, :], in_=T["xs_d"].ap()), writes=["xs_t"])
    for kc2 in range(4):
        S.dma("pool", lambda e, kc2=kc2: e.dma_start(
            out=w_in[:, 2 * kc2:2 * kc2 + 2, :],
            in_=T["w_in_d"].ap()[kc2 * 256:(kc2 + 1) * 256, :].rearrange("(k p) n -> p k n", p=128)), writes=["w_in"])
    S.dma("sp", lambda e: e.dma_start(out=binb, in_=bc_row(T["b_in_d"].ap(), QKV)), writes=["binb"])
    S.dma("sp", lambda e: e.dma_start(out=f0, in_=bass.AP(FD, 383 + 128, [[0, 128], [FW, 16]])), reads=["FD"], writes=["f0"])
    S.op("act", lambda e: e.activation(out=xsb, in_=xs_t, func=AF.Copy), reads=["xs_t"], writes=["xsb"])
    for kc in range(8):
        S.op("pe", lambda e, kc=kc: e.transpose(C["PSB"](6)[:, kc * 128:(kc + 1) * 128], xsb[:, kc * 128:(kc + 1) * 128], C["ident_b"]),
             reads=["xsb", "cbf"], writes=[("ps", 6)], signal=(kc == 7))
    S.op("dve", lambda e: e.tensor_copy(out=xsT, in_=C["PSB"](6).rearrange("p (a b) -> p a b", a=8)),
         reads=[("ps", 6)], writes=["xsT"])
    c0 = 0
    ci = 0
    while c0 < QKV:
        n = min(512, QKV - c0)
        pb = ci % 2
        for kc in range(8):
            S.op("pe", lambda e, kc=kc, pb=pb, c0=c0, n=n: e.matmul(
                PS(pb)[:, 0:n], lhsT=xsT[:, kc, :], rhs=w_in[:, kc, c0:c0 + n], start=(kc == 0), stop=(kc == 7)),
                reads=["xsT", "w_in"], writes=[("ps", pb)], signal=(kc == 7))
        S.op("dve", lambda e, pb=pb, c0=c0, n=n: e.tensor_tensor(out=qkv[:, c0:c0 + n], in0=PS(pb)[:, 0:n],
                                                                  in1=binb[:, c0:c0 + n], op=ALU.add),
             reads=[("ps", pb), "binb"], writes=["qkv"])
        c0 += n
        ci += 1
    S.dma("sp", lambda e: e.dma_start(out=ss_d[0].ap()[:, 127, :], in_=qkv[0:NSMP, 256:512]), reads=["qkv"], writes=["ssn0"])
    for g in range(3):
        L = GROUPS[g + 1][2]
        S.dma("sp", lambda e, g=g, L=L: e.dma_start(out=ss_d[g + 1].ap()[:, L - 1, 0:256],
                                                    in_=qkv[0:NSMP, 1280 + g * 256:1280 + (g + 1) * 256]),
              reads=["qkv"], writes=[("ssn", g, 0)])
        S.dma("sp", lambda e, g=g, L=L: e.dma_start(out=ss_d[g + 1].ap()[:, L - 1, 256:512],
                                                    in_=qkv[0:NSMP, 2048 + g * 256:2048 + (g + 1) * 256]),
              reads=["qkv"], writes=[("ssn", g, 1)])
    for (dst0, src0, n) in ((0, 0, 256), (256, 512, 512), (768, 1024, 256)):
        S.op("pe", lambda e, src0=src0, n=n: e.matmul(PS(2)[:, 0:n], lhsT=C["crep"][0:16, :], rhs=qkv[0:16, src0:src0 + n],
                                                      start=True, stop=True), reads=["qkv", "crep"], writes=[("ps", 2)])
        S.op("act", lambda e, dst0=dst0, n=n: e.activation(out=qrep[:, dst0:dst0 + n], in_=PS(2)[:, 0:n], func=AF.Identity, scale=0.125),
             reads=[("ps", 2)], writes=["qrep"])
    for gi, (d, maxd, L) in enumerate(GROUPS):
        H = 2 if gi == 0 else 4
        rowsz = 2 * H * 64
        ckv = ck[:, :, 0:rowsz]
        src = bass.AP(cache_d[gi], 0, [[16 * d * rowsz, 128], [d * rowsz, 16], [1, rowsz]])
        S.dma("sp", lambda e, ckv=ckv, src=src: e.dma_start(out=ckv, in_=src), writes=["ck"])
        for hq in range(4):
            c = (hq if gi == 0 else 4 + (gi - 1) * 4 + hq)
            S.dma("sp", lambda e, hq=hq, c=c: e.dma_start(
                out=ebs[:, hq, :], in_=bass.AP(T["FDS"], c * 2048, [[16, 128], [1, 16]])), reads=["FD"], writes=["ebs"])
        ck5 = ckv.rearrange("p k (a h c) -> p k a h c", a=2, h=H)
        for hq in range(4):
            kvh = hq // 2 if gi == 0 else hq
            qc = (hq * 64) if gi == 0 else (256 + ((gi - 1) * 4 + hq) * 64)
            kcol = (256 + kvh * 64) if gi == 0 else (1280 + ((gi - 1) * 4 + hq) * 64)
            vcol = (384 + kvh * 64) if gi == 0 else (2048 + ((gi - 1) * 4 + hq) * 64)
            c = (hq if gi == 0 else 4 + (gi - 1) * 4 + hq)
            S.op("dve", lambda e, kvh=kvh, qc=qc: e.tensor_tensor(
                out=prod, in0=ck5[:, :, 0, kvh, :], in1=qrep[:, qc:qc + 64].unsqueeze(1).broadcast_to([128, 16, 64]), op=ALU.mult),
                reads=["ck", "qrep"], writes=["prod"])
            S.op("dve", lambda e, hq=hq: e.reduce_sum(out=sc[:, hq, :], in_=prod, axis=AX.X), reads=["prod"], writes=["sc"])
            S.op("act", lambda e, hq=hq: e.activation(out=sc[:, hq, :], in_=sc[:, hq, :], func=AF.Exp), reads=["sc"], writes=["sc"])
            S.op("dve", lambda e, hq=hq: e.tensor_tensor(out=sc[:, hq, :], in0=sc[:, hq, :], in1=ebs[:, hq, :], op=ALU.mult),
                 reads=["sc", "ebs"], writes=["sc"])
            S.op("dve", lambda e, hq=hq: e.reduce_sum(out=part[:, hq, 64:65], in_=sc[:, hq, :], axis=AX.X), reads=["sc"], writes=["part"])
            S.op("dve", lambda e, kvh=kvh, hq=hq: e.tensor_tensor(
                out=prod.rearrange("p k c -> p c k"), in0=ck5[:, :, 1, kvh, :].rearrange("p k c -> p c k"),
                in1=sc[:, hq, :].unsqueeze(1).broadcast_to([128, 64, 16]), op=ALU.mult),
                reads=["ck", "sc"], writes=["prod"])
            S.op("dve", lambda e, hq=hq: e.reduce_sum(out=part[:, hq, 0:64], in_=prod.rearrange("p k c -> p c k"), axis=AX.X),
                 reads=["prod"], writes=["part"])
            S.op("dve", lambda e, qc=qc, kcol=kcol, hq=hq: e.tensor_tensor(
                out=pr16[0:16, :], in0=qkv[0:16, (qc if gi == 0 else 512 + qc - 256):(qc if gi == 0 else 512 + qc - 256) + 64],
                in1=qkv[0:16, kcol:kcol + 64], op=ALU.mult), reads=["qkv"], writes=["pr16"])
            S.op("dve", lambda e, hq=hq: e.reduce_sum(out=snew[0:16, hq:hq + 1], in_=pr16[0:16, :], axis=AX.X), reads=["pr16"], writes=["snew"])
            S.op("act", lambda e, hq=hq: e.activation(out=enew[0:16, hq:hq + 1], in_=snew[0:16, hq:hq + 1], func=AF.Exp, scale=0.125),
                 reads=["snew"], writes=["enew"])
            S.op("dve", lambda e, hq=hq, c=c: e.tensor_tensor(out=enew[0:16, hq:hq + 1], in0=enew[0:16, hq:hq + 1],
                                                               in1=f0[0:16, c:c + 1], op=ALU.mult), reads=["enew", "f0"], writes=["enew"])
        S.op("pe", lambda e: e.matmul(PS(3)[0:16, 0:260], lhsT=C["repT"], rhs=part.rearrange("p a b -> p (a b)"),
                                      start=True, stop=True), reads=["part", "c128"], writes=[("ps", 3)])
        S.op("dve", lambda e: e.tensor_copy(out=tot[0:16].rearrange("p a b -> p (a b)"), in_=PS(3)[0:16, 0:260]),
             reads=[("ps", 3)], writes=["tot"])
        for hq in range(4):
            kvh = hq // 2 if gi == 0 else hq
            vcol = (384 + kvh * 64) if gi == 0 else (2048 + ((gi - 1) * 4 + hq) * 64)
            S.op("dve", lambda e, hq=hq, vcol=vcol: e.scalar_tensor_tensor(
                out=tot[0:16, hq, 0:64], in0=qkv[0:16, vcol:vcol + 64], scalar=enew[0:16, hq:hq + 1], in1=tot[0:16, hq, 0:64],
                op0=ALU.mult, op1=ALU.add), reads=["qkv", "enew", "tot"], writes=["tot"])
            S.op("dve", lambda e, hq=hq: e.tensor_tensor(out=tot[0:16, hq, 64:65], in0=tot[0:16, hq, 64:65],
                                                          in1=enew[0:16, hq:hq + 1], op=ALU.add), reads=["enew", "tot"], writes=["tot"])
            if gi == 0:
                S.op("dve", lambda e, hq=hq: e.tensor_tensor(out=tot[0:16, hq, 64:65], in0=tot[0:16, hq, 64:65],
                                                              in1=C["expsink"][0:16, hq:hq + 1], op=ALU.add),
                     reads=["tot", "expsink"], writes=["tot"])
        if gi == 0:
            fin, base = tot, 0
        elif gi == 1:
            S.op("dve", lambda e: e.tensor_copy(out=totd[0:16], in_=tot[0:16]), reads=["tot"], writes=["totd"])
            fin = None
        else:
            S.op("dve", lambda e: e.tensor_tensor(out=totd[0:16], in0=totd[0:16], in1=tot[0:16], op=ALU.add),
                 reads=["tot", "totd"], writes=["totd"])
            fin, base = (totd, 4) if gi == 3 else (None, 0)
        if fin is not None:
            key = "tot" if gi == 0 else "totd"
            for hq in range(4):
                S.op("dve", lambda e, fin=fin, hq=hq: e.reciprocal(out=fin[0:16, hq, 64:65], in_=fin[0:16, hq, 64:65]),
                     reads=[key], writes=[key])
                S.op("dve", lambda e, fin=fin, hq=hq, base=base: e.tensor_scalar(
                    out=cat_s[0:16, base + hq, :], in0=fin[0:16, hq, 0:64], scalar1=fin[0:16, hq, 64:65], scalar2=None, op0=ALU.mult),
                    reads=[key], writes=["cat_s"])
    S.barrier()
    A.off = m0


_PROG = None


def kernel(x_prompt, x_sample, cache_swa_kv, cache_dil1_kv, cache_dil2_kv, cache_dil3_kv,
           rel_bias_table, w_in, b_in, attn_sinks, w_o, b_o, ln1_g, ln1_b,
           w_router, b_router, w_up, b_up, w_down, b_down, ln2_g, ln2_b):
    global _PROG
    f = lambda a: np.ascontiguousarray(np.asarray(a, dtype=np.float32))
    xp = f(x_prompt)
    B, SEQ, _ = xp.shape
    c128, coh, cval, crep = host_consts()
    shared = dict(
        table=f(rel_bias_table), w_in=f(w_in)[0], b_in=f(b_in)[0], sinks=f(attn_sinks)[0], w_o=f(w_o)[0],
        b_o=f(b_o)[0], ln1_g=f(ln1_g)[0], ln1_b=f(ln1_b)[0], ln2_g=f(ln2_g)[0], ln2_b=f(ln2_b)[0],
        w_r=f(w_router)[0], b_r=f(b_router)[0], w_up=f(w_up)[0][:NE_DECL], b_up=f(b_up)[0].reshape(NE * 16, 128),
        w_dn=f(w_down)[0][:NE_DECL], b_dn=f(b_down)[0], c128=c128, coh=coh, cval=cval, crep=crep)
    caches = [f(cache_swa_kv)[0], f(cache_dil1_kv)[0], f(cache_dil2_kv)[0], f(cache_dil3_kv)[0]]
    xs = f(x_sample)[:, 0, :]
    in_maps = []
    for c in range(NCORES):
        n, h = c // 2, c % 2
        xwin = np.zeros((HALO + SOWN, D), np.float32)
        xwin[HALO:] = xp[n, h * SOWN:(h + 1) * SOWN]
        fl = np.zeros((128, NUNIT), np.float32)
        fl[:, 1:] = 1.0
        if h == 1:
            xwin[:HALO] = xp[n, SOWN - HALO:SOWN]
            fl[:, 0] = 1.0
        m = dict(shared)
        m["xw"] = xwin
        m["flag"] = fl
        m["xs"] = np.ascontiguousarray(xs[c * NSMP:(c + 1) * NSMP])
        for nm, ca in zip(("c_swa", "c_d1", "c_d2", "c_d3"), caches):
            sl = ca[c * NSMP:(c + 1) * NSMP]
            m[nm] = np.ascontiguousarray(sl.reshape(NSMP, sl.shape[1], -1))
        in_maps.append(m)
    if _PROG is None:
        _PROG = build_program()
    res = run_bass_kernel_spmd(_PROG, in_maps, core_ids=list(range(NCORES)))
    R = res.results
    y_prompt = np.stack([np.concatenate([R[2 * n]["yp"], R[2 * n + 1]["yp"]], axis=0) for n in range(B)]).astype(np.float32)
    y_sample = np.concatenate([R[c]["ys"] for c in range(NCORES)], axis=0)[:, None, :].astype(np.float32)
    pst = []
    for nm, H in (("ps_swa", 2), ("ps_d1", 4), ("ps_d2", 4), ("ps_d3", 4)):
        pst.append(np.stack([R[2 * n + 1][nm] for n in range(B)])[None].astype(np.float32))
    sst = []
    for nm, H in (("ss_swa", 2), ("ss_d1", 4), ("ss_d2", 4), ("ss_d3", 4)):
        a = np.concatenate([R[c][nm] for c in range(NCORES)], axis=0)
        sst.append(a.reshape(1, a.shape[0], a.shape[1], 2, H, 64).astype(np.float32))
    return (y_prompt, y_sample, pst[0], pst[1], pst[2], pst[3], sst[0], sst[1], sst[2], sst[3])
```

```python
import os
import numpy as np
import concourse.bass as bass
import concourse.mybir as mybir
from concourse.bass_utils import run_bass_kernel_spmd

F32 = mybir.dt.float32
BF16 = mybir.dt.bfloat16
I32 = mybir.dt.int32
U8 = mybir.dt.uint8
ALU = mybir.AluOpType
AF = mybir.ActivationFunctionType
AX = mybir.AxisListType

NCORES = 8
D = 1024
QKV = 2816
NE = 32
CAP = 640
UNIT = 2048
HALO = 2048
WIN = HALO + UNIT
NUNIT = 2
SOWN = UNIT * NUNIT
NSMP = 16
NT = SOWN // 128 + 1
ALPHA = float(2.0 ** 0.25)
EPS = 1e-5
BIG = 1.0e6
XGROWS = NE * CAP
FW = 512
ARENA = 186 * 1024
STAGE = int(os.environ.get("K_STAGE", "99"))
NE_DECL = NE if STAGE >= 5 else 1

GROUPS = [
    (1, 127, 128), (1, 128, 128), (4, 128, 512), (16, 128, 2048)]


def t5_bucket_np(n):
    n = np.maximum(np.asarray(n, np.int64), 0)
    ratio = np.log(np.maximum(n, 16).astype(np.float32) / np.float32(16)) / np.float32(np.log(2048 / 16))
    large = 16 + (ratio.astype(np.float32) * np.float32(16)).astype(np.int32)
    return np.where(n < 16, n, np.minimum(large, 31)).astype(np.int64)


def host_consts():
    c128 = np.zeros((128, 3 * 128 + 32 + 2 + 16), np.float32)
    c128[:, 0:128] = np.eye(128, dtype=np.float32)
    c128[:, 128:256] = np.triu(np.ones((128, 128), np.float32), 1)
    c128[:, 256:384] = 1.0
    c128[:, 384:416] = (np.arange(NE, dtype=np.float32) * CAP)[None, :]
    c128[:NSMP, 416] = 1.0
    c128[NSMP:, 417] = BIG
    for p in range(128):
        c128[p, 418 + p // 8] = 1.0
    oh = np.zeros((32, 4, FW), np.float32)
    valid = np.zeros((4, 4, FW), np.float32)
    for gi, (d, maxd, L) in enumerate(GROUPS):
        for u in range(383):
            dist = u - 127
            if 0 <= dist <= maxd:
                oh[t5_bucket_np(dist * d), gi, u] = 1.0
                valid[:, gi, u] = 1.0
        for j in range(129):
            o = 128 - j
            if o <= maxd:
                oh[t5_bucket_np(o * d), gi, 383 + j] = 1.0
                valid[:, gi, 383 + j] = 1.0
    rep = np.zeros((16, 128), np.float32)
    for p in range(128):
        rep[p // 8, p] = 1.0
    return c128, oh.reshape(32, 4 * FW), valid.reshape(4, 4 * FW), rep


class Lazy:
    def __init__(self, f):
        self.f = f


class _Rec:
    def __init__(self):
        self.call = None

    def __getattr__(self, name):
        def m(*args, **kwargs):
            assert self.call is None
            self.call = (name, args, kwargs)
            return self
        return m


def _bind(fn):
    r = _Rec()
    fn(r)
    name, args, kwargs = r.call

    def run(eng):
        kw = {k: (v.f() if isinstance(v, Lazy) else v) for k, v in kwargs.items()}
        return getattr(eng, name)(*args, **kw)
    return run


class Sched:
    ENG = ("pe", "act", "dve", "pool", "sp")

    def __init__(self, nc, esems, dsems, bgsems=()):
        self.nc = nc
        self.bgsem = list(bgsems)
        self.bgcnt = [0] * len(self.bgsem)
        self.bgnext = 0
        self.bgnext_pool = 0
        self.q = {e: [] for e in self.ENG}
        self.cnt = {e: 0 for e in self.ENG}
        self.esem = esems
        self.dsem = dsems
        self.dcnt = [0] * len(dsems)
        self.dnext = 0
        self.dnext_pool = 0
        self.seen = {e: {} for e in self.ENG}
        self.lastw = {}
        self.readers = {}
        self.pending_pe = False

    def _need(self, e, tok):
        k, v = tok
        if k == e and e == "pe":
            return
        if self.seen[e].get(k, 0) >= v:
            return
        self.seen[e][k] = v
        if isinstance(k, str):
            sem = self.esem[k]
        elif k[0] == "bg":
            sem = self.bgsem[k[1]]
        else:
            sem = self.dsem[k[1]]
        self.q[e].append(lambda eng, s=sem, vv=v: eng.wait_ge(s, vv))

    def _deps(self, e, reads, writes):
        toks = []
        for k in reads:
            if k in self.lastw:
                toks.append(self.lastw[k])
        for k in writes:
            if k in self.lastw:
                toks.append(self.lastw[k])
            toks.extend(self.readers.get(k, {}).values())
        for t in toks:
            self._need(e, t)

    def _record(self, tok, reads, writes):
        for k in reads:
            self.readers.setdefault(k, {})[tok[0]] = tok
        for k in writes:
            self.lastw[k] = tok
            self.readers[k] = {}

    def op(self, e, fn, reads=(), writes=(), signal=True):
        fn = _bind(fn)
        self._deps(e, reads, writes)
        tok = (e, self.cnt[e] + 1)
        if signal:
            self.cnt[e] += 1
            sem = self.esem[e]
            self.q[e].append(lambda eng, f=fn, s=sem: f(eng).then_inc(s, 1))
        else:
            assert e == "pe"
            self.q[e].append(lambda eng, f=fn: f(eng))
        self._record(tok, reads, writes)

    def dma(self, e, fn, reads=(), writes=()):
        fn = _bind(fn)
        self._deps(e, reads, writes)
        npool = 16
        if e == "pool":
            idx = self.dnext_pool
            self.dnext_pool = (self.dnext_pool + 1) % npool
        else:
            idx = npool + self.dnext
            self.dnext = (self.dnext + 1) % (len(self.dsem) - npool)
        if self.dcnt[idx] > 0:
            self._need(e, (("dma", idx), self.dcnt[idx] * 16))
        self.dcnt[idx] += 1
        tok = (("dma", idx), self.dcnt[idx] * 16)
        sem = self.dsem[idx]
        self.q[e].append(lambda eng, f=fn, s=sem: f(eng).then_inc(s, 16))
        self._record(tok, reads, writes)

    def dma_bg(self, e, fn, reads=()):
        fn = _bind(fn)
        self._deps(e, reads, ())
        half = len(self.bgsem) // 2
        if e == "pool":
            idx = half + self.bgnext_pool
            self.bgnext_pool = (self.bgnext_pool + 1) % half
        else:
            idx = self.bgnext
            self.bgnext = (self.bgnext + 1) % half
        self.bgcnt[idx] += 1
        sem = self.bgsem[idx]
        self.q[e].append(lambda eng, f=fn, s=sem: f(eng).then_inc(s, 16))

    def wait_bg(self, engines=None):
        for e in (engines or self.ENG):
            for i in range(len(self.bgsem)):
                if self.bgcnt[i] > 0:
                    self._need(e, (("bg", i), self.bgcnt[i] * 16))

    def barrier(self):
        for e in self.ENG:
            for o in ("pe", "act", "dve", "pool"):
                if o != e and self.cnt[o] > 0:
                    self._need(e, (o, self.cnt[o]))
            for i in range(len(self.dsem)):
                if self.dcnt[i] > 0:
                    self._need(e, (("dma", i), self.dcnt[i] * 16))
        self.lastw = {}
        self.readers = {}

    def replay(self, block):
        q = self.q

        @block.tensor
        def _(eng):
            for t in q["pe"]:
                t(eng)

        @block.scalar
        def _(eng):
            for t in q["act"]:
                t(eng)

        @block.vector
        def _(eng):
            for t in q["dve"]:
                t(eng)

        @block.gpsimd
        def _(eng):
            for t in q["pool"]:
                t(eng)

        @block.sync
        def _(eng):
            for t in q["sp"]:
                t(eng)


class Arena:
    def __init__(self, t, nbytes):
        self.t = t
        self.n = nbytes
        self.off = 0

    def alloc(self, shape, dt, nbytes_el):
        free = int(np.prod(shape[1:]))
        nb = free * nbytes_el
        nb = (nb + 63) // 64 * 64
        assert self.off + nb <= self.n, ("arena overflow", self.off, nb, self.n)
        v = self.t[:, self.off:self.off + free * nbytes_el].bitcast(dt)
        self.off += nb
        if len(shape) == 3:
            v = v.rearrange("p (a b) -> p a b", a=shape[1])
        elif len(shape) == 4:
            v = v.rearrange("p (a b c) -> p a b c", a=shape[1], b=shape[2])
        return v

    def f32(self, *shape):
        return self.alloc((128,) + shape, F32, 4)

    def bf(self, *shape):
        return self.alloc((128,) + shape, BF16, 2)

    def i32(self, *shape):
        return self.alloc((128,) + shape, I32, 4)


def build_program():
    nc = bass.Bass("TRN2", target_bir_lowering=False)

    def din(name, shape, dt=F32):
        return nc.dram_tensor(name, list(shape), dt, kind="ExternalInput")

    def dout(name, shape, dt=F32):
        return nc.dram_tensor(name, list(shape), dt, kind="ExternalOutput")

    def dint(name, shape, dt=F32):
        return nc.dram_tensor(name, list(shape), dt, kind="Internal")

    xw = din("xw", [HALO + SOWN, D])
    flag_d = din("flag", [128, NUNIT])
    xs_d = din("xs", [NSMP, D])
    cache_d = [din("c_swa", [NSMP, 128, 256]), din("c_d1", [NSMP, 128, 512]),
               din("c_d2", [NSMP, 512, 512]), din("c_d3", [NSMP, 2048, 512])]
    table_d = din("table", [32, 16])
    w_in_d = din("w_in", [D, QKV])
    b_in_d = din("b_in", [QKV])
    sinks_d = din("sinks", [4])
    w_o_d = din("w_o", [512, D])
    b_o_d = din("b_o", [D])
    ln1g_d = din("ln1_g", [D]); ln1b_d = din("ln1_b", [D])
    ln2g_d = din("ln2_g", [D]); ln2b_d = din("ln2_b", [D])
    w_r_d = din("w_r", [D, NE]); b_r_d = din("b_r", [NE])
    w_up_d = din("w_up", [NE_DECL, D, 2 * D]); b_up_d = din("b_up", [NE * 16, 128])
    w_dn_d = din("w_dn", [NE_DECL, D, D]); b_dn_d = din("b_dn", [NE, D])
    c128_d = din("c128", [128, 434]); coh_d = din("coh", [32, 4 * FW])
    cval_d = din("cval", [4, 4 * FW]); crep_d = din("crep", [16, 128])

    yp_d = dout("yp", [SOWN, D]); ys_d = dout("ys", [NSMP, D])
    ps_d = [dout("ps_swa", [128, 2, 2, 64]), dout("ps_d1", [128, 2, 4, 64]),
            dout("ps_d2", [512, 2, 4, 64]), dout("ps_d3", [2048, 2, 4, 64])]
    ss_d = [dout("ss_swa", [NSMP, 128, 256]), dout("ss_d1", [NSMP, 128, 512]),
            dout("ss_d2", [NSMP, 512, 512]), dout("ss_d3", [NSMP, 2048, 512])]

    X1 = dint("X1", [NT * 128, D])
    XG = dint("XG", [XGROWS, D], BF16)
    YS = dint("YS", [XGROWS + 1, D])
    FD = dint("FD", [16, FW])
    FDR = dint("FDR", [16, 128, FW])
    FDS = dint("FDS", [16, 16, 128])

    import contextlib
    with contextlib.ExitStack() as es:
        arena_t = es.enter_context(nc.sbuf_tensor("arena", [128, ARENA], U8))
        psb = [es.enter_context(nc.psum_tensor("psb%d" % i, [128, 512], F32)) for i in range(8)]
        esems = {e: es.enter_context(nc.semaphore("s_" + e)) for e in ("pe", "act", "dve", "pool")}
        dsems = [es.enter_context(nc.semaphore("d%d" % i)) for i in range(56)]
        bgsems = [es.enter_context(nc.semaphore("g%d" % i)) for i in range(8)]
        es.enter_context(nc.allow_non_contiguous_dma(reason="small strided constant loads"))
        S = Sched(nc, esems, dsems, bgsems)
        A = Arena(arena_t, ARENA)
        emit(nc, S, A, psb, locals())
        block = es.enter_context(nc.Block())
        S.replay(block)
    return nc


def emit(nc, S, A, psb, T):
    xw, flag_d, xs_d, cache_d, table_d = T["xw"], T["flag_d"], T["xs_d"], T["cache_d"], T["table_d"]
    w_in_d, b_in_d, sinks_d, w_o_d, b_o_d = T["w_in_d"], T["b_in_d"], T["sinks_d"], T["w_o_d"], T["b_o_d"]
    X1, XG, YS, FD = T["X1"], T["XG"], T["YS"], T["FD"]
    FDR, FDS = T["FDR"], T["FDS"]
    ps_d, ss_d = T["ps_d"], T["ss_d"]

    def PS(i):
        return psb[i][:, :]

    def PSB(i):
        return psb[i][:, :].bitcast(BF16)

    def bc_row(dram_ap_1d, n):
        return dram_ap_1d.unsqueeze(0).broadcast_to([128, n])

    c128 = A.f32(434)
    ident_f = c128[:, 0:128]
    ones_f = c128[:, 256:384]
    ecap = c128[:, 384:416]
    rowvalid = c128[:, 416:417]
    rowbig = c128[:, 417:418]
    repT = c128[:, 418:434]
    cbf = A.bf(384)
    ident_b = cbf[:, 0:128]
    triu_b = cbf[:, 128:256]
    ones_b = cbf[:, 256:384]
    crep = A.f32(128)
    flag = A.f32(NUNIT)
    bqk = A.f32(22)
    bkdup = A.f32(2)
    expsink = A.f32(4)
    G_all = A.f32(NT, NE)
    g4_all = A.f32(NT, 4)
    dsc_all = A.i32(NT, 4)
    dga_all = A.i32(NT, 4)
    cnt = A.f32(NE)
    wr = A.f32(8, NE)
    brb = A.f32(NE)
    zero_bf = A.bf(2048)
    persist_mark = A.off

    REGS = T["_regs"] = {}

    def _mkregs(eng):
        REGS["sc"] = eng.alloc_register("bc_sc")
        eng.reg_mov(REGS["sc"], XGROWS - 1)
        REGS["ga"] = eng.alloc_register("bc_ga")
        eng.reg_mov(REGS["ga"], XGROWS)
    S.q["pool"].append(_mkregs)
    S.dma("sp", lambda e: e.dma_start(out=c128, in_=T["c128_d"].ap()), writes=["c128"])
    S.dma("sp", lambda e: e.dma_start(out=crep[0:16, :], in_=T["crep_d"].ap()), writes=["crep"])
    S.dma("sp", lambda e: e.dma_start(out=flag, in_=flag_d.ap()), writes=["flag"])
    S.dma("sp", lambda e: e.dma_start(out=bqk, in_=b_in_d.ap().rearrange("(j p) -> p j", p=128)), writes=["bqk"])
    for p in range(2):
        for hh in range(2):
            S.dma("sp", lambda e, p=p, hh=hh: e.dma_start(
                out=bkdup[hh * 64:(hh + 1) * 64, p:p + 1],
                in_=b_in_d.ap()[256 + p * 64:256 + (p + 1) * 64].unsqueeze(1)), writes=["bkdup"])
    S.dma("sp", lambda e: e.dma_start(out=expsink, in_=bc_row(sinks_d.ap(), 4)), writes=["expsink"])
    S.dma("sp", lambda e: e.dma_start(out=wr, in_=T["w_r_d"].ap().rearrange("(k p) n -> p k n", p=128)), writes=["wr"])
    S.dma("sp", lambda e: e.dma_start(out=brb, in_=bc_row(T["b_r_d"].ap(), NE)), writes=["brb"])
    S.op("act", lambda e: e.activation(out=expsink, in_=expsink, func=AF.Exp), reads=["expsink"], writes=["expsink"])
    S.op("dve", lambda e: e.tensor_copy(out=cbf, in_=c128[:, 0:384]), reads=["c128"], writes=["cbf"])
    S.op("pool", lambda e: e.memset(cnt, 0.0), writes=["cnt"])
    S.op("pool", lambda e: e.memset(zero_bf, 0.0), writes=["zero_bf"])

    xg_v = XG.ap().rearrange("(a p r) d -> a p (r d)", p=128, r=2)
    for a in range(XGROWS // 256):
        S.dma_bg("pool", lambda e, a=a: e.dma_start(out=xg_v[a], in_=zero_bf), reads=["zero_bf"])
    S.dma_bg("pool", lambda e: e.dma_start(out=YS.ap()[XGROWS:XGROWS + 1, :].bitcast(BF16),
                                         in_=zero_bf[0:1, :]), reads=["zero_bf"])

    for gi, (d, maxd, L) in enumerate(GROUPS):
        nch = max(1, L // 256)
        rows = (L - 1)
        per = (rows + nch - 1) // nch
        for ch in range(nch):
            r0, r1 = ch * per, min(rows, (ch + 1) * per)
            if r0 >= r1:
                continue
            S.dma_bg("sp", lambda e, gi=gi, r0=r0, r1=r1: e.dma_start(
                out=ss_d[gi].ap()[:, r0:r1, :].rearrange("b r c -> b (r c)"),
                in_=cache_d[gi].ap()[:, r0 + 1:r1 + 1, :].rearrange("b r c -> b (r c)")))

    m0 = A.off
    tab = A.f32(16)
    coh = A.f32(4 * FW)
    cval = A.f32(4 * FW)
    ft = A.f32(4, FW)
    S.dma("sp", lambda e: e.dma_start(out=tab[0:32, :], in_=table_d.ap()), writes=["tab"])
    S.dma("sp", lambda e: e.dma_start(out=coh[0:32, :], in_=T["coh_d"].ap()), writes=["coh"])
    S.dma("sp", lambda e: e.dma_start(out=cval[0:4, :], in_=T["cval_d"].ap()), writes=["cval"])
    for gi in range(4):
        S.op("pe", lambda e, gi=gi: e.matmul(PS(gi)[0:4, :], lhsT=tab[0:32, gi * 4:gi * 4 + 4],
                                             rhs=coh[0:32, gi * FW:(gi + 1) * FW], start=True, stop=True),
             reads=["tab", "coh"], writes=[("ps", gi)])
        S.op("act", lambda e, gi=gi: e.activation(out=ft[0:4, gi, :], in_=PS(gi)[0:4, :], func=AF.Exp),
             reads=[("ps", gi)], writes=[("ft", gi)])
        S.op("dve", lambda e, gi=gi: e.tensor_tensor(out=ft[0:4, gi, :], in0=ft[0:4, gi, :],
                                                      in1=cval[0:4, gi * FW:(gi + 1) * FW], op=ALU.mult),
             reads=[("ft", gi), "cval"], writes=[("ft", gi)])
        S.dma("sp", lambda e, gi=gi: e.dma_start(out=FD.ap()[gi * 4:gi * 4 + 4, :], in_=ft[0:4, gi, :]),
              reads=[("ft", gi)], writes=[("FDw", gi)])
        S.dma("sp", lambda e, gi=gi: e.dma_start(out=FDR.ap()[gi * 4:gi * 4 + 4, :, :],
                                                 in_=ft[0:4, gi, :].unsqueeze(1).broadcast_to([4, 128, FW])),
              reads=[("ft", gi)], writes=[("FDRw", gi)])
        S.dma("sp", lambda e, gi=gi: e.dma_start(out=FDS.ap()[gi * 4:gi * 4 + 4, :, :],
                                                 in_=ft[0:4, gi, 383:383 + 128].unsqueeze(1).broadcast_to([4, 16, 128])),
              reads=[("ft", gi)], writes=[("FDSw", gi)])
    S.barrier()
    A.off = m0
    if STAGE == 1:
        S.barrier()
        S.wait_bg()
        return

    sample_phase(nc, S, A, PS, T, dict(c128=c128, ident_f=ident_f, ident_b=ident_b, repT=repT, crep=crep,
                                        expsink=expsink, bc_row=bc_row, PSB=PSB))
    cat_s = T["_cat_s"]
    xs_t = T["_xs_t"]
    S.barrier()
    if STAGE == 2:
        S.barrier()
        S.wait_bg()
        return

    jobs = []
    for p in range(2):
        jobs.append(dict(gi=0, d=1, maxd=127, qcol=p * 128, kcol=256 + p * 64, vcol=384 + p * 64, dup=True,
                         tcol=[2 * p, 2 * p + 1], slot=[2 * p, 2 * p + 1], first=True, last=True,
                         sink=[2 * p, 2 * p + 1], bq=p, bk=None, st_h=p, pair=p))
    for pr in range(2):
        for g in range(3):
            base = (g * 4 + 2 * pr) * 64
            jobs.append(dict(gi=g + 1, d=GROUPS[g + 1][0], maxd=128, qcol=512 + base, kcol=1280 + base,
                             vcol=2048 + base, dup=False, tcol=[4 + g * 4 + 2 * pr, 4 + g * 4 + 2 * pr + 1],
                             slot=[4 + 2 * pr, 4 + 2 * pr + 1], first=(g == 0), last=(g == 2), sink=None,
                             bq=(512 + base) // 128, bk=(1280 + base) // 128, st_h=2 * pr, pair=pr))

    pm = A.off
    xT = A.bf(8, WIN)
    catT = A.bf(8, UNIT)
    xst = A.f32(D)
    xbf = A.bf(D)
    sfull = [A.f32(256), A.f32(256)]
    wq = A.bf(8, 128)
    wkv = A.bf(8, 256)
    jm = A.off

    tile_ctx = dict(cnt=cnt, G_all=G_all, g4_all=g4_all, dsc_all=dsc_all, dga_all=dga_all, wr=wr, brb=brb,
                    ecap=ecap, rowvalid=rowvalid, rowbig=rowbig, ident_f=ident_f, ident_b=ident_b,
                    triu_b=triu_b, ones_b=ones_b, bc_row=bc_row, PSB=PSB)

    for u in range(NUNIT):
        for t in range(WIN // 128):
            S.dma("act", lambda e, t=t, u=u: e.dma_start(out=xst, in_=xw.ap()[u * UNIT + t * 128:u * UNIT + (t + 1) * 128, :]),
                  writes=["xst"])
            S.op("act", lambda e: e.activation(out=xbf, in_=xst, func=AF.Copy), reads=["xst"], writes=["xbf"])
            pb = 6 + (t % 2)
            for kc in range(8):
                S.op("pe", lambda e, kc=kc, pb=pb: e.transpose(PSB(pb)[:, kc * 128:(kc + 1) * 128],
                                                              xbf[:, kc * 128:(kc + 1) * 128], ident_b),
                     reads=["xbf", "cbf"], writes=[("ps", pb)], signal=(kc == 7))
            S.op("dve", lambda e, t=t, pb=pb: e.tensor_copy(
                out=xT[:, :, t * 128:(t + 1) * 128], in_=PSB(pb).rearrange("p (a b) -> p a b", a=8)),
                reads=[("ps", pb)], writes=[("xT", t)])
        xT_keys = [("xT", t) for t in range(WIN // 128)]

        for ji, jb in enumerate(jobs):
            A.off = jm
            d, gi = jb["d"], jb["gi"]
            halo_len = 128 * d
            T0 = HALO - halo_len
            nbo = UNIT // (128 * d)
            nb = nbo + 1
            QT = A.bf(UNIT)
            KT = A.bf(WIN)
            Vaug = A.bf(32, 2, 65)
            acc = A.f32(2, UNIT)
            EB = A.f32(2, 256)
            EBf = A.f32(2, 256)
            Et = [A.bf(512), A.bf(512)]
            Pt = [A.bf(512), A.bf(512)]
            kvb = A.f32(256)
            K = lambda name: (name, 0)
            S.dma("pool", lambda e, jb=jb: e.dma_start(
                out=wq, in_=w_in_d.ap()[:, jb["qcol"]:jb["qcol"] + 128].rearrange("(k p) n -> p k n", p=128)),
                writes=["wq"])
            if jb["dup"]:
                for hh in range(2):
                    S.dma("pool", lambda e, jb=jb, hh=hh: e.dma_start(
                        out=wkv[:, :, hh * 64:(hh + 1) * 64],
                        in_=w_in_d.ap()[:, jb["kcol"]:jb["kcol"] + 64].rearrange("(k p) n -> p k n", p=128)),
                        writes=["wkv"])
                    S.dma("pool", lambda e, jb=jb, hh=hh: e.dma_start(
                        out=wkv[:, :, 128 + hh * 64:128 + (hh + 1) * 64],
                        in_=w_in_d.ap()[:, jb["vcol"]:jb["vcol"] + 64].rearrange("(k p) n -> p k n", p=128)),
                        writes=["wkv"])
                    S.dma("sp", lambda e, jb=jb, hh=hh: e.dma_start(
                        out=kvb[:, hh * 64:(hh + 1) * 64], in_=bc_row(b_in_d.ap()[jb["kcol"]:jb["kcol"] + 64], 64)),
                        writes=["kvb"])
                    S.dma("sp", lambda e, jb=jb, hh=hh: e.dma_start(
                        out=kvb[:, 128 + hh * 64:128 + (hh + 1) * 64],
                        in_=bc_row(b_in_d.ap()[jb["vcol"]:jb["vcol"] + 64], 64)), writes=["kvb"])
            else:
                S.dma("pool", lambda e, jb=jb: e.dma_start(
                    out=wkv[:, :, 0:128],
                    in_=w_in_d.ap()[:, jb["kcol"]:jb["kcol"] + 128].rearrange("(k p) n -> p k n", p=128)),
                    writes=["wkv"])
                S.dma("pool", lambda e, jb=jb: e.dma_start(
                    out=wkv[:, :, 128:256],
                    in_=w_in_d.ap()[:, jb["vcol"]:jb["vcol"] + 128].rearrange("(k p) n -> p k n", p=128)),
                    writes=["wkv"])
                S.dma("sp", lambda e, jb=jb: e.dma_start(
                    out=kvb[:, 0:128], in_=bc_row(b_in_d.ap()[jb["kcol"]:jb["kcol"] + 128], 128)), writes=["kvb"])
                S.dma("sp", lambda e, jb=jb: e.dma_start(
                    out=kvb[:, 128:256], in_=bc_row(b_in_d.ap()[jb["vcol"]:jb["vcol"] + 128], 128)), writes=["kvb"])
            for hh in range(2):
                c = jb["tcol"][hh]
                S.dma("act", lambda e, c=c, hh=hh: e.dma_start(
                    out=EB[:, hh, 0:128], in_=bass.AP(FDR, c * 128 * FW + 255, [[FW - 1, 128], [1, 128]])),
                    reads=["FD"], writes=["EB"])
                S.dma("act", lambda e, c=c, hh=hh: e.dma_start(
                    out=EB[:, hh, 128:256], in_=bass.AP(FDR, c * 128 * FW + 127, [[FW - 1, 128], [1, 128]])),
                    reads=["FD"], writes=["EB"])
            S.op("pool", lambda e, u=u: e.tensor_scalar(out=EBf[:, :, 0:128], in0=EB[:, :, 0:128],
                                                         scalar1=flag[:, u:u + 1], scalar2=None, op0=ALU.mult),
                 reads=["EB", "flag"], writes=["EBf"])
            S.op("pool", lambda e: e.tensor_copy(out=EBf[:, :, 128:256], in_=EB[:, :, 128:256]),
                 reads=["EB"], writes=["EBf"])
            S.op("pool", lambda e: e.memset(Vaug[:, :, :, 64:65], 1.0), writes=["Vones"])
            bqc = bqk[:, jb["bq"]:jb["bq"] + 1]
            for c4 in range(UNIT // 512):
                pb = c4 % 2
                for kc in range(8):
                    S.op("pe", lambda e, kc=kc, pb=pb, c4=c4: e.matmul(
                        PS(pb), lhsT=wq[:, kc, :], rhs=xT[:, kc, HALO + c4 * 512:HALO + (c4 + 1) * 512],
                        start=(kc == 0), stop=(kc == 7)),
                        reads=["wq"] + xT_keys[16 + c4 * 4:16 + c4 * 4 + 4], writes=[("ps", pb)], signal=(kc == 7))
                S.op("dve", lambda e, pb=pb, c4=c4, bqc=bqc: e.tensor_scalar(
                    out=QT[:, c4 * 512:(c4 + 1) * 512], in0=PS(pb), scalar1=bqc, scalar2=0.125,
                    op0=ALU.add, op1=ALU.mult), reads=[("ps", pb), "bqk"], writes=["QT"])
            bkc = bkdup[:, jb["pair"]:jb["pair"] + 1] if jb["dup"] else bqk[:, jb["bk"]:jb["bk"] + 1]
            pos = T0
            ci = 0
            while pos < WIN:
                n = min(512, WIN - pos)
                pb = ci % 2
                for kc in range(8):
                    S.op("pe", lambda e, kc=kc, pb=pb, pos=pos, n=n: e.matmul(
                        PS(pb)[:, 0:n], lhsT=wkv[:, kc, 0:128], rhs=xT[:, kc, pos:pos + n],
                        start=(kc == 0), stop=(kc == 7)),
                        reads=["wkv"] + xT_keys[pos // 128:(pos + n + 127) // 128], writes=[("ps", pb)],
                        signal=(kc == 7))
                S.op("act", lambda e, pb=pb, pos=pos, n=n, bkc=bkc: e.activation(
                    out=KT[:, pos:pos + n], in_=PS(pb)[:, 0:n], func=AF.Identity, bias=bkc, scale=1.0),
                    reads=[("ps", pb), "bqk", "bkdup"], writes=["KT"])
                pos += n
                ci += 1
            for r in range(d):
                for ib in range(nb):
                    blk = r * nb + ib
                    pb = 2 + (blk % 2)
                    start = T0 + r + d * 128 * ib
                    for kc in range(8):
                        S.op("pe", lambda e, kc=kc, pb=pb, start=start, d=d: e.matmul(
                            PS(pb)[:, 0:256], lhsT=xT[:, kc, start:start + 127 * d + 1:d], rhs=wkv[:, kc, :],
                            start=(kc == 0), stop=(kc == 7)),
                            reads=["wkv"] + xT_keys[start // 128:(start + 128 * d + 127) // 128],
                            writes=[("ps", pb)], signal=(kc == 7))
                    st_ib = nbo
                    is_state = (u == NUNIT - 1 and ib == st_ib and not os.environ.get("K_NOSTATE"))
                    if not is_state:
                        S.op("dve", lambda e, pb=pb, blk=blk: e.tensor_tensor(
                            out=Vaug[:, blk, :, 0:64], in0=PS(pb)[:, 128:256].rearrange("p (a b) -> p a b", a=2),
                            in1=kvb[:, 128:256].rearrange("p (a b) -> p a b", a=2), op=ALU.add),
                            reads=[("ps", pb), "kvb"], writes=[("V", blk)])
                    else:
                        sf = sfull[blk % 2]
                        S.op("dve", lambda e, pb=pb, sf=sf: e.tensor_tensor(out=sf, in0=PS(pb)[:, 0:256], in1=kvb, op=ALU.add),
                             reads=[("ps", pb), "kvb"], writes=[("sf", blk % 2)])
                        S.op("pool", lambda e, sf=sf, blk=blk: e.tensor_copy(
                            out=Vaug[:, blk, :, 0:64], in_=sf[:, 128:256].rearrange("p (a b) -> p a b", a=2)),
                            reads=[("sf", blk % 2)], writes=[("V", blk)])
                        for kv in range(2):
                            if jb["dup"]:
                                dst = ps_d[0].ap()[:, kv, jb["st_h"], :]
                                src = sf[:, kv * 128:kv * 128 + 64]
                            else:
                                dst = ps_d[gi].ap()[r::d, kv, jb["st_h"]:jb["st_h"] + 2, :].rearrange("p b c -> p (b c)")
                                src = sf[:, kv * 128:(kv + 1) * 128]
                            S.dma("pool", lambda e, dst=dst, src=src: e.dma_start(out=dst, in_=src),
                                  reads=[("sf", blk % 2)], writes=[("psd", gi, jb["pair"], r, kv)])
            qbs = [(r, ib) for r in range(d) for ib in range(1, nb)]
            for hh in range(2):
                hp = slice(hh * 64, (hh + 1) * 64)
                for bi in range(0, len(qbs), 2):
                    bsel = (bi // 2) % 2
                    stb = 4 + bsel
                    ob = 6 + bsel
                    ST = PS(stb).rearrange("p (a b) -> p a b", a=2)
                    for qi in range(2):
                        r, ib = qbs[bi + qi]
                        qs = r + d * 128 * (ib - 1)
                        qap = QT[hp, qs:qs + 127 * d + 1:d]
                        for side in range(2):
                            ks = T0 + r + d * 128 * (ib - 1 + side)
                            S.op("pe", lambda e, ST=ST, qi=qi, side=side, ks=ks, qap=qap, hp=hp, d=d: e.matmul(
                                ST[:, qi, side * 128:(side + 1) * 128], lhsT=KT[hp, ks:ks + 127 * d + 1:d], rhs=qap,
                                start=True, stop=True),
                                reads=["QT", "KT"], writes=[("ps", stb)], signal=(qi == 1 and side == 1))
                    E = Et[bsel]
                    S.op("act", lambda e, E=E, stb=stb: e.activation(out=E, in_=PS(stb), func=AF.Exp),
                         reads=[("ps", stb)], writes=[("E", bsel)])
                    Pq = Pt[bsel].rearrange("p (a b) -> p a b", a=2)
                    Ev = E.rearrange("p (a b) -> p a b", a=2)
                    for qi in range(2):
                        r, ib = qbs[bi + qi]
                        ebt = EBf if ib == 1 else EB
                        S.op("pool" if qi == 0 else "dve", lambda e, Pq=Pq, Ev=Ev, qi=qi, ebt=ebt, hh=hh: e.tensor_tensor(
                            out=Pq[:, qi, :], in0=Ev[:, qi, :], in1=ebt[:, hh, :], op=ALU.mult),
                            reads=[("E", bsel), "EB", "EBf"], writes=[("P", bsel, qi)])
                    OT = PS(ob).rearrange("p (a b) -> p a b", a=4)
                    for qi in range(2):
                        r, ib = qbs[bi + qi]
                        for side in range(2):
                            blk = r * nb + ib - 1 + side
                            S.op("pe", lambda e, OT=OT, qi=qi, side=side, blk=blk, Pq=Pq, hh=hh: e.matmul(
                                OT[0:65, qi, :], lhsT=Vaug[:, blk, hh, :], rhs=Pq[:, qi, side * 128:(side + 1) * 128],
                                start=(side == 0), stop=(side == 1)),
                                reads=[("V", blk), "Vones", ("P", bsel, qi)], writes=[("ps", ob)],
                                signal=(qi == 1 and side == 1))
                    for qi in range(2):
                        r, ib = qbs[bi + qi]
                        qs = r + d * 128 * (ib - 1)
                        dst = acc[0:65, hh, qs:qs + 127 * d + 1:d]
                        if jb["first"]:
                            S.op("act", lambda e, dst=dst, OT=OT, qi=qi: e.activation(out=dst, in_=OT[0:65, qi, :], func=AF.Identity),
                                 reads=[("ps", ob)], writes=[("acc", hh)])
                        else:
                            S.op("dve", lambda e, dst=dst, OT=OT, qi=qi: e.tensor_tensor(
                                out=dst, in0=OT[0:65, qi, :], in1=dst, op=ALU.add),
                                reads=[("ps", ob)], writes=[("acc", hh)])
            if jb["last"]:
                for hh in range(2):
                    slot = jb["slot"][hh]
                    den = acc[64:65, hh, :]
                    if jb["sink"] is not None:
                        si = jb["sink"][hh]
                        S.op("dve", lambda e, den=den, si=si: e.tensor_scalar(
                            out=den, in0=den, scalar1=expsink[64:65, si:si + 1], scalar2=None, op0=ALU.add),
                            reads=[("acc", hh), "expsink"], writes=[("acc", hh)])
                    S.op("dve", lambda e, den=den: e.reciprocal(out=den, in_=den), reads=[("acc", hh)], writes=[("acc", hh)])
                    for c4 in range(UNIT // 512):
                        pb = c4 % 2
                        S.op("pe", lambda e, pb=pb, c4=c4, hh=hh: e.matmul(
                            PS(pb)[0:64, :], lhsT=ones_f[64:65, 0:64], rhs=acc[64:65, hh, c4 * 512:(c4 + 1) * 512],
                            start=True, stop=True), reads=[("acc", hh), "c128"], writes=[("ps", pb)])
                        S.op("dve", lambda e, pb=pb, c4=c4, hh=hh, slot=slot: e.tensor_tensor(
                            out=catT[0:64, slot, c4 * 512:(c4 + 1) * 512], in0=acc[0:64, hh, c4 * 512:(c4 + 1) * 512],
                            in1=PS(pb)[0:64, :], op=ALU.mult), reads=[("ps", pb), ("acc", hh)], writes=[("catT", slot)])
        if STAGE == 3:
            S.barrier()
            S.wait_bg()
            return
        S.barrier()
        S.wait_bg()
        A.off = jm
        if STAGE == 32 and u == 1:
            return
        post_tiles(nc, S, A, PS, T, tile_ctx, catT, u, list(range(UNIT // 128)), xst)
        S.barrier()
        if STAGE == 31 or (STAGE == 33 and u == 1):
            S.wait_bg()
            return
    A.off = jm
    post_tiles(nc, S, A, PS, T, tile_ctx, None, None, [None], xst, sample=(cat_s, xs_t, catT))
    S.barrier()
    A.off = persist_mark
    if STAGE == 4:
        S.barrier()
        S.wait_bg()
        return

    moe_phase(nc, S, A, PS, PSB, T, ident_b)
    S.barrier()
    A.off = persist_mark
    if STAGE == 5:
        S.barrier()
        S.wait_bg()
        return
    combine_phase(nc, S, A, PS, T, tile_ctx)
    S.barrier()
    S.wait_bg()


def layer_norm_tile(S, A_tiles, z, g_b, b_b, out, key_in, key_out):
    junk, st = A_tiles["junk"], A_tiles["st"]
    S.op("act", lambda e: e.activation(out=junk, in_=z, func=AF.Identity, accum_out=st[:, 0:1]),
         reads=[key_in], writes=["ln_junk", "ln_st0"])
    S.op("act", lambda e: e.activation(out=junk, in_=z, func=AF.Square, accum_out=st[:, 1:2]),
         reads=[key_in], writes=["ln_junk", "ln_st1"])
    S.op("dve", lambda e: e.tensor_scalar(out=st[:, 2:3], in0=st[:, 0:1], scalar1=1.0 / D, scalar2=None, op0=ALU.mult),
         reads=["ln_st0"], writes=["ln_st2"])
    S.op("dve", lambda e: e.tensor_tensor(out=st[:, 3:4], in0=st[:, 2:3], in1=st[:, 2:3], op=ALU.mult),
         reads=["ln_st2"], writes=["ln_st3"])
    S.op("dve", lambda e: e.scalar_tensor_tensor(out=st[:, 4:5], in0=st[:, 1:2], scalar=1.0 / D, in1=st[:, 3:4],
                                                  op0=ALU.mult, op1=ALU.subtract),
         reads=["ln_st1", "ln_st3"], writes=["ln_st4"])
    S.op("dve", lambda e: e.tensor_scalar(out=st[:, 4:5], in0=st[:, 4:5], scalar1=EPS, scalar2=None, op0=ALU.add),
         reads=["ln_st4"], writes=["ln_st4"])
    S.op("act", lambda e: e.activation(out=st[:, 5:6], in_=st[:, 4:5], func=AF.Ln), reads=["ln_st4"], writes=["ln_st5"])
    S.op("act", lambda e: e.activation(out=st[:, 5:6], in_=st[:, 5:6], func=AF.Exp, scale=-0.5), reads=["ln_st5"], writes=["ln_st5"])
    S.op("dve", lambda e: e.scalar_tensor_tensor(out=st[:, 6:7], in0=st[:, 2:3], scalar=-1.0, in1=st[:, 5:6],
                                                  op0=ALU.mult, op1=ALU.mult),
         reads=["ln_st2", "ln_st5"], writes=["ln_st6"])
    S.op("act", lambda e: e.activation(out=junk, in_=z, func=AF.Identity, bias=st[:, 6:7], scale=st[:, 5:6]),
         reads=[key_in, "ln_st5", "ln_st6"], writes=["ln_junk"])
    S.op("pool", lambda e: e.tensor_tensor(out=junk, in0=junk, in1=g_b, op=ALU.mult),
         reads=["ln_junk", "lnconst"], writes=["ln_junk"])
    S.op("pool", lambda e: e.tensor_tensor(out=out, in0=junk, in1=b_b, op=ALU.add),
         reads=["ln_junk", "lnconst"], writes=[key_out])


def post_tiles(nc, S, A, PS, T, C, catT, u, tiles, xst, sample=None):
    bc_row = C["bc_row"]
    X1, XG = T["X1"], T["XG"]
    w_o = A.bf(8, D)
    bo_b = A.f32(D)
    g_b = A.f32(D)
    b_b = A.f32(D)
    z = A.f32(D)
    junk = A.f32(D)
    x1 = A.f32(D)
    x1bf = A.bf(D)
    x1T = A.f32(8, 128)
    st = A.f32(8)
    lg = A.f32(NE)
    m8 = A.f32(8)
    mask = A.f32(NE)
    mask_b = A.bf(NE)
    ex = A.f32(NE)
    ssum = A.f32(2)
    posc = A.f32(NE)
    big = A.f32(NE)
    dst_f = A.f32(8)
    scr = A.f32(NE)
    S.dma("pool", lambda e: e.dma_start(out=w_o[0:64, :, :], in_=T["w_o_d"].ap().rearrange("(s p) n -> p s n", p=64)),
          writes=["w_o"])
    S.dma("sp", lambda e: e.dma_start(out=bo_b, in_=bc_row(T["b_o_d"].ap(), D)), writes=["lnconst"])
    S.dma("sp", lambda e: e.dma_start(out=g_b, in_=bc_row(T["ln1g_d"].ap(), D)), writes=["lnconst"])
    S.dma("sp", lambda e: e.dma_start(out=b_b, in_=bc_row(T["ln1b_d"].ap(), D)), writes=["lnconst"])
    if sample is not None:
        cat_s, xs_t, catT_buf = sample
        catT = catT_buf
        for slot in range(8):
            S.op("pe", lambda e, slot=slot: e.transpose(PS(4 + slot // 4)[0:64, (slot % 4) * 128:(slot % 4) * 128 + 128],
                                                        cat_s[:, slot, :], C["ident_f"]),
                 reads=["cat_s", "c128"], writes=[("ps", 4 + slot // 4)], signal=(slot % 4 == 3))
        for half in range(2):
            S.op("dve", lambda e, half=half: e.tensor_copy(
                out=catT[0:64, half * 4:half * 4 + 4, 0:128], in_=PS(4 + half)[0:64, :].rearrange("p (a b) -> p a b", a=4)),
                reads=[("ps", 4 + half)], writes=[("catT", "s")])
    for tl in tiles:
        if sample is None:
            ti = u * (UNIT // 128) + tl
            tok0 = tl * 128
            S.dma("sp", lambda e, ti=ti: e.dma_start(out=xst, in_=T["xw"].ap()[HALO + ti * 128:HALO + (ti + 1) * 128, :]),
                  writes=["xst"])
            xin = xst
        else:
            ti = NT - 1
            tok0 = 0
            xin = sample[1]
        for half in range(2):
            for slot in range(8):
                S.op("pe", lambda e, half=half, slot=slot, tok0=tok0: e.matmul(
                    PS(half), lhsT=catT[0:64, slot, tok0:tok0 + 128], rhs=w_o[0:64, slot, half * 512:(half + 1) * 512],
                    start=(slot == 0), stop=(slot == 7)),
                    reads=["w_o"] + [("catT", s) for s in list(range(8)) + ["s"]], writes=[("ps", half)], signal=(slot == 7))
        S.op("pool", lambda e, xin=xin: e.tensor_scalar(out=z, in0=xin, scalar1=ALPHA, scalar2=None, op0=ALU.mult),
             reads=["xst", "xs_t"], writes=["z"])
        S.op("pool", lambda e: e.tensor_tensor(out=z, in0=z, in1=bo_b, op=ALU.add), reads=["z", "lnconst"], writes=["z"])
        for half in range(2):
            S.op("dve", lambda e, half=half: e.tensor_tensor(out=z[:, half * 512:(half + 1) * 512],
                                                              in0=z[:, half * 512:(half + 1) * 512], in1=PS(half), op=ALU.add),
                 reads=["z", ("ps", half)], writes=["z"])
        layer_norm_tile(S, dict(junk=junk, st=st), z, g_b, b_b, x1, "z", "x1")
        S.dma("pool", lambda e, ti=ti: e.dma_start(out=X1.ap()[ti * 128:(ti + 1) * 128, :], in_=x1), reads=["x1"], writes=[("X1", ti)])
        S.op("act", lambda e: e.activation(out=x1bf, in_=x1, func=AF.Copy), reads=["x1"], writes=["x1bf"])
        for kc in range(8):
            S.op("pe", lambda e, kc=kc: e.transpose(PS(2 + kc // 4)[:, (kc % 4) * 128:(kc % 4) * 128 + 128],
                                                    x1[:, kc * 128:(kc + 1) * 128], C["ident_f"]),
                 reads=["x1", "c128"], writes=[("ps", 2 + kc // 4)], signal=(kc % 4 == 3))
        for half in range(2):
            S.op("act", lambda e, half=half: e.activation(
                out=x1T[:, half * 4:half * 4 + 4, :], in_=PS(2 + half).rearrange("p (a b) -> p a b", a=4), func=AF.Identity),
                reads=[("ps", 2 + half)], writes=[("x1T", half)])
        for kc in range(8):
            S.op("pe", lambda e, kc=kc: e.matmul(PS(4)[:, 0:NE], lhsT=x1T[:, kc, :], rhs=C["wr"][:, kc, :],
                                                 start=(kc == 0), stop=(kc == 7)),
                 reads=[("x1T", kc // 4), "wr"], writes=[("ps", 4)], signal=(kc == 7))
        S.op("dve", lambda e: e.tensor_tensor(out=lg, in0=PS(4)[:, 0:NE], in1=C["brb"], op=ALU.add),
             reads=[("ps", 4), "brb"], writes=["lg"])
        S.op("dve", lambda e: e.max(out=m8, in_=lg), reads=["lg"], writes=["m8"])
        S.op("dve", lambda e: e.tensor_scalar(out=mask, in0=lg, scalar1=m8[:, 3:4], scalar2=None, op0=ALU.is_ge),
             reads=["lg", "m8"], writes=["mask"])
        if sample is not None:
            S.op("dve", lambda e: e.tensor_scalar(out=mask, in0=mask, scalar1=C["rowvalid"], scalar2=None, op0=ALU.mult),
                 reads=["mask", "c128"], writes=["mask"])
        S.op("dve", lambda e: e.tensor_scalar(out=ssum[:, 0:1], in0=m8[:, 0:1], scalar1=-1.0, scalar2=None, op0=ALU.mult),
             reads=["m8"], writes=["nmax"])
        S.op("act", lambda e: e.activation(out=ex, in_=lg, func=AF.Exp, bias=ssum[:, 0:1], scale=1.0),
             reads=["lg", "nmax"], writes=["ex"])
        S.op("dve", lambda e: e.tensor_tensor(out=ex, in0=ex, in1=mask, op=ALU.mult), reads=["ex", "mask"], writes=["ex"])
        S.op("dve", lambda e: e.reduce_sum(out=ssum[:, 1:2], in_=ex, axis=AX.X), reads=["ex"], writes=["esum"])
        if sample is not None:
            S.op("dve", lambda e: e.tensor_tensor(out=ssum[:, 1:2], in0=ssum[:, 1:2], in1=C["rowbig"], op=ALU.add),
                 reads=["esum", "c128"], writes=["esum"])
        S.op("dve", lambda e: e.reciprocal(out=ssum[:, 1:2], in_=ssum[:, 1:2]), reads=["esum"], writes=["esum"])
        Gt = C["G_all"][:, ti, :]
        S.op("dve", lambda e, Gt=Gt: e.tensor_scalar(out=Gt, in0=ex, scalar1=ssum[:, 1:2], scalar2=None, op0=ALU.mult),
             reads=["ex", "esum"], writes=["G"])
        S.op("act", lambda e: e.activation(out=mask_b, in_=mask, func=AF.Copy), reads=["mask"], writes=["mask_b"])
        S.op("pe", lambda e: e.matmul(PS(5)[:, 0:NE], lhsT=C["triu_b"], rhs=mask_b, start=True, stop=True),
             reads=["mask_b", "cbf"], writes=[("ps", 5)])
        S.op("pe", lambda e: e.matmul(PS(5)[:, 64:64 + NE], lhsT=C["ones_b"], rhs=mask_b, start=True, stop=True),
             reads=["mask_b", "cbf"], writes=[("ps", 5)])
        S.op("dve", lambda e: e.tensor_tensor(out=posc, in0=PS(5)[:, 0:NE], in1=C["cnt"], op=ALU.add),
             reads=[("ps", 5), "cnt"], writes=["posc"])
        S.op("dve", lambda e: e.tensor_tensor(out=C["cnt"], in0=PS(5)[:, 64:64 + NE], in1=C["cnt"], op=ALU.add),
             reads=[("ps", 5), "posc"], writes=["cnt"])
        S.op("dve", lambda e: e.tensor_scalar(out=big, in0=posc, scalar1=float(CAP) - 0.5, scalar2=BIG, op0=ALU.is_gt, op1=ALU.mult),
             reads=["posc"], writes=["big"])
        S.op("dve", lambda e: e.tensor_tensor(out=posc, in0=posc, in1=C["ecap"], op=ALU.add), reads=["posc", "c128"], writes=["posc"])
        S.op("dve", lambda e: e.tensor_tensor(out=posc, in0=posc, in1=big, op=ALU.add), reads=["posc", "big"], writes=["posc"])
        if sample is not None:
            S.op("dve", lambda e: e.tensor_scalar(out=posc, in0=posc, scalar1=C["rowbig"], scalar2=None, op0=ALU.add),
                 reads=["posc", "c128"], writes=["posc"])
        for k in range(4):
            S.op("dve", lambda e, k=k: e.scalar_tensor_tensor(
                out=scr, in0=lg, scalar=m8[:, k:k + 1], in1=posc, op0=ALU.is_equal, op1=ALU.mult, accum_out=dst_f[:, k:k + 1]),
                reads=["lg", "m8", "posc"], writes=["scr", ("dstf", k)])
            S.op("dve", lambda e, k=k, Gt=Gt, ti=ti: e.scalar_tensor_tensor(
                out=scr, in0=lg, scalar=m8[:, k:k + 1], in1=Gt, op0=ALU.is_equal, op1=ALU.mult,
                accum_out=C["g4_all"][:, ti, k:k + 1]),
                reads=["lg", "m8", "G"], writes=["scr", "g4"])
        S.op("dve", lambda e, ti=ti: e.tensor_copy(out=C["dsc_all"][:, ti, :], in_=dst_f[:, 0:4]),
             reads=[("dstf", k) for k in range(4)], writes=["dsc"])
        S.op("dve", lambda e: e.tensor_scalar(out=dst_f[:, 4:8], in0=dst_f[:, 0:4], scalar1=float(XGROWS), scalar2=None, op0=ALU.min),
             reads=[("dstf", k) for k in range(4)], writes=["dstf2"])
        S.op("dve", lambda e, ti=ti: e.tensor_copy(out=C["dga_all"][:, ti, :], in_=dst_f[:, 4:8]),
             reads=["dstf2"], writes=["dga"])
        for k in range(4):
            S.dma("pool", lambda e, k=k, ti=ti: e.indirect_dma_start(
                out=XG.ap(), out_offset=bass.IndirectOffsetOnAxis(ap=C["dsc_all"][:, ti, k:k + 1], axis=0),
                in_=x1bf, in_offset=None, bounds_check=Lazy(lambda: T["_regs"]["sc"]), oob_is_err=False),
                reads=["x1bf", "dsc", "XG"], writes=[("XGs", ti, k)])


def moe_phase(nc, S, A, PS, PSB, T, ident_b):
    XG, YS = T["XG"], T["YS"]
    w_up_d, w_dn_d = T["w_up_d"], T["w_dn_d"]
    wu = [A.bf(8, 2 * D), A.bf(8, 2 * D)]
    wd = [A.bf(8, D), A.bf(8, D)]
    xg = [A.bf(CAP // 128, D), A.bf(CAP // 128, D)]
    xgT = [A.bf(8, CAP), A.bf(8, CAP)]
    aT = A.bf(8, CAP)
    gt = [A.f32(512), A.f32(512)]
    sg = [A.f32(512), A.f32(512)]
    t2 = [A.f32(512), A.f32(512)]
    yst = [A.f32(D), A.f32(D)]
    bupT = A.f32(NE * 16)
    bst = A.f32(128)
    for a in range(4):
        S.dma("sp", lambda e, a=a: e.dma_start(out=bst, in_=T["b_up_d"].ap()[a * 128:(a + 1) * 128, :]), writes=["bst"])
        S.op("pe", lambda e, a=a: e.transpose(PS(0)[:, a * 128:(a + 1) * 128], bst, T["_ident_f"]),
             reads=["bst"], writes=[("ps", 0)])
        S.op("dve", lambda e, a=a: e.tensor_copy(out=bupT[:, a * 128:(a + 1) * 128], in_=PS(0)[:, a * 128:(a + 1) * 128]),
             reads=[("ps", 0)], writes=["bupT"])
    bup1 = A.f32(NE * 16)
    S.op("dve", lambda e: e.tensor_scalar(out=bup1, in0=bupT, scalar1=1.0, scalar2=None, op0=ALU.add),
         reads=["bupT"], writes=["bup1"])
    pieces = [(0, 512), (512, CAP - 512)]

    def load_w(ex):
        b = ex % 2
        for kc2 in range(4):
            S.dma("pool", lambda e, ex=ex, b=b, kc2=kc2: e.dma_start(
                out=wu[b][:, 2 * kc2:2 * kc2 + 2, :],
                in_=w_up_d.ap()[ex, kc2 * 256:(kc2 + 1) * 256, :].rearrange("(k p) n -> p k n", p=128)),
                writes=[("wu", b, kc2)])
        for kc2 in range(2):
            S.dma("pool", lambda e, ex=ex, b=b, kc2=kc2: e.dma_start(
                out=wd[b][:, 4 * kc2:4 * kc2 + 4, :],
                in_=w_dn_d.ap()[ex, kc2 * 512:(kc2 + 1) * 512, :].rearrange("(k p) n -> p k n", p=128)),
                writes=[("wd", b, kc2)])

    def load_xg(ex):
        S.dma("pool", lambda e, ex=ex: e.dma_start(
            out=xg[ex % 2], in_=XG.ap()[ex * CAP:(ex + 1) * CAP, :].rearrange("(a p) d -> p a d", p=128)),
            reads=["XGall"], writes=[("xg", ex % 2)])

    def transposes(ex):
        xb = ex % 2
        for a in range(CAP // 128):
            pb = 6 + (a % 2)
            for kc in range(8):
                S.op("pe", lambda e, a=a, kc=kc, pb=pb, xb=xb: e.transpose(PSB(pb)[:, kc * 128:(kc + 1) * 128],
                                                                           xg[xb][:, a, kc * 128:(kc + 1) * 128], ident_b),
                     reads=[("xg", xb)], writes=[("ps", pb)], signal=(kc == 7))
            S.op("act", lambda e, a=a, pb=pb, xb=xb: e.activation(
                out=xgT[xb][:, :, a * 128:(a + 1) * 128], in_=PSB(pb).rearrange("p (a b) -> p a b", a=8), func=AF.Copy),
                reads=[("ps", pb)], writes=[("xgT", xb, a)])

    load_w(0)
    load_xg(0)
    transposes(0)
    load_xg(1)
    for ex in range(NE):
        b = ex % 2
        if ex + 1 < NE:
            load_w(ex + 1)
        xgT_keys = [("xgT", b, a) for a in range(CAP // 128)]
        it = 0
        for (s0, sn) in pieces:
            for j in range(8):
                pg = (it % 2) * 2
                pl = pg + 1
                for (pb, fo) in ((pg, j), (pl, 8 + j)):
                    for kc in range(8):
                        S.op("pe", lambda e, pb=pb, fo=fo, kc=kc, s0=s0, sn=sn, b=b: e.matmul(
                            PS(pb)[:, 0:sn], lhsT=wu[b][:, kc, fo * 128:(fo + 1) * 128], rhs=xgT[b][:, kc, s0:s0 + sn],
                            start=(kc == 0), stop=(kc == 7)),
                            reads=[("wu", b, kc // 2)] + xgT_keys, writes=[("ps", pb)], signal=(kc == 7))
                bi = it % 2
                bg = bupT[:, ex * 16 + j:ex * 16 + j + 1]
                bl = bupT[:, ex * 16 + 8 + j:ex * 16 + 8 + j + 1]
                g_, s_, t_ = gt[bi][:, 0:sn], sg[bi][:, 0:sn], t2[bi][:, 0:sn]
                S.op("dve", lambda e, pg=pg, g_=g_, bg=bg, sn=sn: e.tensor_scalar(
                    out=g_, in0=PS(pg)[:, 0:sn], scalar1=bg, scalar2=7.0, op0=ALU.add, op1=ALU.min),
                    reads=[("ps", pg), "bupT"], writes=[("g", bi)])
                S.op("act", lambda e, g_=g_, s_=s_: e.activation(out=s_, in_=g_, func=AF.Sigmoid, scale=1.702),
                     reads=[("g", bi)], writes=[("sg", bi)])
                bl1 = bup1[:, ex * 16 + 8 + j:ex * 16 + 8 + j + 1]
                S.op("dve", lambda e, pl=pl, t_=t_, bl1=bl1, sn=sn: e.tensor_scalar(
                    out=t_, in0=PS(pl)[:, 0:sn], scalar1=bl1, scalar2=8.0, op0=ALU.add, op1=ALU.min),
                    reads=[("ps", pl), "bup1"], writes=[("t2", bi)])
                S.op("dve", lambda e, t_=t_, g_=g_: e.scalar_tensor_tensor(
                    out=t_, in0=t_, scalar=-6.0, in1=g_, op0=ALU.max, op1=ALU.mult),
                    reads=[("t2", bi), ("g", bi)], writes=[("t2", bi)])
                S.op("pool", lambda e, s_=s_, t_=t_, j=j, s0=s0, sn=sn: e.tensor_tensor(
                    out=aT[:, j, s0:s0 + sn], in0=t_, in1=s_, op=ALU.mult),
                    reads=[("sg", bi), ("t2", bi)], writes=[("aT", j, s0)])
                it += 1
        if ex + 1 < NE:
            transposes(ex + 1)
            if ex + 2 < NE:
                load_xg(ex + 2)
        aT_keys = [("aT", j, s0) for j in range(8) for (s0, sn) in pieces]
        for a in range(CAP // 128):
            yt = yst[a % 2]
            for half in range(2):
                pb = 4 + half
                for fc in range(8):
                    S.op("pe", lambda e, a=a, half=half, fc=fc, pb=pb, b=b: e.matmul(
                        PS(pb), lhsT=aT[:, fc, a * 128:(a + 1) * 128], rhs=wd[b][:, fc, half * 512:(half + 1) * 512],
                        start=(fc == 0), stop=(fc == 7)),
                        reads=aT_keys + [("wd", b, fc // 4)], writes=[("ps", pb)], signal=(fc == 7))
                S.op("act" if half == 0 else "dve",
                     (lambda e, yt=yt, pb=pb, half=half: e.activation(out=yt[:, half * 512:(half + 1) * 512], in_=PS(pb), func=AF.Identity))
                     if half == 0 else
                     (lambda e, yt=yt, pb=pb, half=half: e.tensor_copy(out=yt[:, half * 512:(half + 1) * 512], in_=PS(pb))),
                     reads=[("ps", pb)], writes=[("yst", a % 2, half)])
            S.dma("act", lambda e, ex=ex, a=a, yt=yt: e.dma_start(
                out=YS.ap()[ex * CAP + a * 128:ex * CAP + (a + 1) * 128, :], in_=yt),
                reads=[("yst", a % 2, 0), ("yst", a % 2, 1)], writes=[("YS", ex, a)])


def combine_phase(nc, S, A, PS, T, C):
    bc_row = C["bc_row"]
    X1, YS = T["X1"], T["YS"]
    g_b = A.f32(D)
    b_b = A.f32(D)
    bdn = A.f32(D)
    yk = [A.f32(D) for _ in range(4)]
    x1 = A.f32(D)
    z = A.f32(D)
    junk = A.f32(D)
    out = A.f32(D)
    st = A.f32(8)
    GT = A.f32(128)
    S.dma("sp", lambda e: e.dma_start(out=g_b, in_=bc_row(T["ln2g_d"].ap(), D)), writes=["lnconst"])
    S.dma("sp", lambda e: e.dma_start(out=b_b, in_=bc_row(T["ln2b_d"].ap(), D)), writes=["lnconst"])
    S.dma("sp", lambda e: e.dma_start(out=bdn[0:NE, :], in_=T["b_dn_d"].ap()), writes=["bdn"])
    for ti in range(NT):
        S.dma("pool", lambda e, ti=ti: e.dma_start(out=x1, in_=X1.ap()[ti * 128:(ti + 1) * 128, :]), writes=["x1"])
        for k in range(4):
            S.dma("pool", lambda e, k=k, ti=ti: e.indirect_dma_start(
                out=yk[k], out_offset=None, in_=YS.ap(),
                in_offset=bass.IndirectOffsetOnAxis(ap=C["dga_all"][:, ti, k:k + 1], axis=0),
                bounds_check=Lazy(lambda: T["_regs"]["ga"]), oob_is_err=False), writes=[("yk", k)])
        S.op("pe", lambda e, ti=ti: e.transpose(PS(2)[0:NE, 0:128], C["G_all"][:, ti, :], C["ident_f"]),
             writes=[("ps", 2)])
        S.op("act", lambda e: e.activation(out=GT[0:NE, :], in_=PS(2)[0:NE, 0:128], func=AF.Identity),
             reads=[("ps", 2)], writes=["GT"])
        for half in range(2):
            S.op("pe", lambda e, half=half: e.matmul(PS(half), lhsT=GT[0:NE, :], rhs=bdn[0:NE, half * 512:(half + 1) * 512],
                                                     start=True, stop=True), reads=["GT", "bdn"], writes=[("ps", half)])
        S.op("pool", lambda e: e.tensor_scalar(out=z, in0=x1, scalar1=ALPHA, scalar2=None, op0=ALU.mult),
             reads=["x1"], writes=["z"])
        for k in range(4):
            S.op("dve", lambda e, k=k, ti=ti: e.scalar_tensor_tensor(
                out=z, in0=yk[k], scalar=C["g4_all"][:, ti, k:k + 1], in1=z, op0=ALU.mult, op1=ALU.add),
                reads=[("yk", k), "z"], writes=["z"])
        for half in range(2):
            S.op("dve", lambda e, half=half: e.tensor_tensor(out=z[:, half * 512:(half + 1) * 512],
                                                              in0=z[:, half * 512:(half + 1) * 512], in1=PS(half), op=ALU.add),
                 reads=["z", ("ps", half)], writes=["z"])
        layer_norm_tile(S, dict(junk=junk, st=st), z, g_b, b_b, out, "z", "out")
        if ti < NT - 1:
            S.dma("pool", lambda e, ti=ti: e.dma_start(out=T["yp_d"].ap()[ti * 128:(ti + 1) * 128, :], in_=out),
                  reads=["out"], writes=[("yp", ti)])
        else:
            S.dma("sp", lambda e: e.dma_start(out=T["ys_d"].ap(), in_=out[0:NSMP, :]), reads=["out"], writes=["ys"])


def sample_phase(nc, S, A, PS, T, C):
    bc_row = C["bc_row"]
    cache_d, ss_d = T["cache_d"], T["ss_d"]
    FD = T["FD"]
    cat_s = A.f32(8, 64)
    xs_t = A.f32(D)
    T["_cat_s"] = cat_s
    T["_xs_t"] = xs_t
    T["_ident_f"] = C["ident_f"]
    m0 = A.off
    w_in = A.bf(8, QKV)
    xsb = A.bf(D)
    xsT = A.bf(8, 128)
    binb = A.f32(QKV)
    qkv = A.f32(QKV)
    qrep = A.f32(1024)
    ck = A.f32(16, 512)
    ebs = A.f32(4, 16)
    prod = A.f32(16, 64)
    sc = A.f32(4, 16)
    part = A.f32(4, 65)
    tot = A.f32(4, 65)
    totd = A.f32(4, 65)
    snew = A.f32(16)
    enew = A.f32(16)
    f0 = A.f32(16)
    pr16 = A.f32(64)
    S.op("pool", lambda e: e.memset(xs_t, 0.0), writes=["xs_t"])
    S.op("pool", lambda e: e.memset(cat_s, 0.0), writes=["cat_s"])
    S.dma("sp", lambda e: e.dma_start(out=xs_t[0:NSMP, :], in_=T["xs_d"].ap()), writes=["xs_t"])
    for kc2 in range(4):
        S.dma("pool", lambda e, kc2=kc2: e.dma_start(
            out=w_in[:, 2 * kc2:2 * kc2 + 2, :],
            in_=T["w_in_d"].ap()[kc2 * 256:(kc2 + 1) * 256, :].rearrange("(k p) n -> p k n", p=128)), writes=["w_in"])
    S.dma("sp", lambda e: e.dma_start(out=binb, in_=bc_row(T["b_in_d"].ap(), QKV)), writes=["binb"])
    S.dma("sp", lambda e: e.dma_start(out=f0, in_=bass.AP(FD, 383 + 128, [[0, 128], [FW, 16]])), reads=["FD"], writes=["f0"])
    S.op("act", lambda e: e.activation(out=xsb, in_=xs_t, func=AF.Copy), reads=["xs_t"], writes=["xsb"])
    for kc in range(8):
        S.op("pe", lambda e, kc=kc: e.transpose(C["PSB"](6)[:, kc * 128:(kc + 1) * 128], xsb[:, kc * 128:(kc + 1) * 128], C["ident_b"]),
             reads=["xsb", "cbf"], writes=[("ps", 6)], signal=(kc == 7))
    S.op("dve", lambda e: e.tensor_copy(out=xsT, in_=C["PSB"](6).rearrange("p (a b) -> p a b", a=8)),
         reads=[("ps", 6)], writes=["xsT"])
    c0 = 0
    ci = 0
    while c0 < QKV:
        n = min(512, QKV - c0)
        pb = ci % 2
        for kc in range(8):
            S.op("pe", lambda e, kc=kc, pb=pb, c0=c0, n=n: e.matmul(
                PS(pb)[:, 0:n], lhsT=xsT[:, kc, :], rhs=w_in[:, kc, c0:c0 + n], start=(kc == 0), stop=(kc == 7)),
                reads=["xsT", "w_in"], writes=[("ps", pb)], signal=(kc == 7))
        S.op("dve", lambda e, pb=pb, c0=c0, n=n: e.tensor_tensor(out=qkv[:, c0:c0 + n], in0=PS(pb)[:, 0:n],
                                                                  in1=binb[:, c0:c0 + n], op=ALU.add),
             reads=[("ps", pb), "binb"], writes=["qkv"])
        c0 += n
        ci += 1
    S.dma("sp", lambda e: e.dma_start(out=ss_d[0].ap()[:, 127, :], in_=qkv[0:NSMP, 256:512]), reads=["qkv"], writes=["ssn0"])
    for g in range(3):
        L = GROUPS[g + 1][2]
        S.dma("sp", lambda e, g=g, L=L: e.dma_start(out=ss_d[g + 1].ap()[:, L - 1, 0:256],
                                                    in_=qkv[0:NSMP, 1280 + g * 256:1280 + (g + 1) * 256]),
              reads=["qkv"], writes=[("ssn", g, 0)])
        S.dma("sp", lambda e, g=g, L=L: e.dma_start(out=ss_d[g + 1].ap()[:, L - 1, 256:512],
                                                    in_=qkv[0:NSMP, 2048 + g * 256:2048 + (g + 1) * 256]),
              reads=["qkv"], writes=[("ssn", g, 1)])
    for (dst0, src0, n) in ((0, 0, 256), (256, 512, 512), (768, 1024, 256)):
        S.op("pe", lambda e, src0=src0, n=n: e.matmul(PS(2)[:, 0:n], lhsT=C["crep"][0:16, :], rhs=qkv[0:16, src0:src0 + n],
                                                      start=True, stop=True), reads=["qkv", "crep"], writes=[("ps", 2)])
        S.op("act", lambda e, dst0=dst0, n=n: e.activation(out=qrep[:, dst0:dst0 + n], in_=PS(2)[:, 0:n], func=AF.Identity, scale=0.125),
             reads=[("ps", 2)], writes=["qrep"])
    for gi, (d, maxd, L) in enumerate(GROUPS):
        H = 2 if gi == 0 else 4
        rowsz = 2 * H * 64
        ckv = ck[:, :, 0:rowsz]
        src = bass.AP(cache_d[gi], 0, [[16 * d * rowsz, 128], [d * rowsz, 16], [1, rowsz]])
        S.dma("sp", lambda e, ckv=ckv, src=src: e.dma_start(out=ckv, in_=src), writes=["ck"])
        for hq in range(4):
            c = (hq if gi == 0 else 4 + (gi - 1) * 4 + hq)
            S.dma("sp", lambda e, hq=hq, c=c: e.dma_start(
                out=ebs[:, hq, :], in_=bass.AP(T["FDS"], c * 2048, [[16, 128], [1, 16]])), reads=["FD"], writes=["ebs"])
        ck5 = ckv.rearrange("p k (a h c) -> p k a h c", a=2, h=H)
        for hq in range(4):
            kvh = hq // 2 if gi == 0 else hq
            qc = (hq * 64) if gi == 0 else (256 + ((gi - 1) * 4 + hq) * 64)
            kcol = (256 + kvh * 64) if gi == 0 else (1280 + ((gi - 1) * 4 + hq) * 64)
            vcol = (384 + kvh * 64) if gi == 0 else (2048 + ((gi - 1) * 4 + hq) * 64)
            c = (hq if gi == 0 else 4 + (gi - 1) * 4 + hq)
            S.op("dve", lambda e, kvh=kvh, qc=qc: e.tensor_tensor(
                out=prod, in0=ck5[:, :, 0, kvh, :], in1=qrep[:, qc:qc + 64].unsqueeze(1).broadcast_to([128, 16, 64]), op=ALU.mult),
                reads=["ck", "qrep"], writes=["prod"])
            S.op("dve", lambda e, hq=hq: e.reduce_sum(out=sc[:, hq, :], in_=prod, axis=AX.X), reads=["prod"], writes=["sc"])
            S.op("act", lambda e, hq=hq: e.activation(out=sc[:, hq, :], in_=sc[:, hq, :], func=AF.Exp), reads=["sc"], writes=["sc"])
            S.op("dve", lambda e, hq=hq: e.tensor_tensor(out=sc[:, hq, :], in0=sc[:, hq, :], in1=ebs[:, hq, :], op=ALU.mult),
                 reads=["sc", "ebs"], writes=["sc"])
            S.op("dve", lambda e, hq=hq: e.reduce_sum(out=part[:, hq, 64:65], in_=sc[:, hq, :], axis=AX.X), reads=["sc"], writes=["part"])
            S.op("dve", lambda e, kvh=kvh, hq=hq: e.tensor_tensor(
                out=prod.rearrange("p k c -> p c k"), in0=ck5[:, :, 1, kvh, :].rearrange("p k c -> p c k"),
                in1=sc[:, hq, :].unsqueeze(1).broadcast_to([128, 64, 16]), op=ALU.mult),
                reads=["ck", "sc"], writes=["prod"])
            S.op("dve", lambda e, hq=hq: e.reduce_sum(out=part[:, hq, 0:64], in_=prod.rearrange("p k c -> p c k"), axis=AX.X),
                 reads=["prod"], writes=["part"])
            S.op("dve", lambda e, qc=qc, kcol=kcol, hq=hq: e.tensor_tensor(
                out=pr16[0:16, :], in0=qkv[0:16, (qc if gi == 0 else 512 + qc - 256):(qc if gi == 0 else 512 + qc - 256) + 64],
                in1=qkv[0:16, kcol:kcol + 64], op=ALU.mult), reads=["qkv"], writes=["pr16"])
            S.op("dve", lambda e, hq=hq: e.reduce_sum(out=snew[0:16, hq:hq + 1], in_=pr16[0:16, :], axis=AX.X), reads=["pr16"], writes=["snew"])
            S.op("act", lambda e, hq=hq: e.activation(out=enew[0:16, hq:hq + 1], in_=snew[0:16, hq:hq + 1], func=AF.Exp, scale=0.125),
                 reads=["snew"], writes=["enew"])
            S.op("dve", lambda e, hq=hq, c=c: e.tensor_tensor(out=enew[0:16, hq:hq + 1], in0=enew[0:16, hq:hq + 1],
                                                               in1=f0[0:16, c:c + 1], op=ALU.mult), reads=["enew", "f0"], writes=["enew"])
        S.op("pe", lambda e: e.matmul(PS(3)[0:16, 0:260], lhsT=C["repT"], rhs=part.rearrange("p a b -> p (a b)"),
                                      start=True, stop=True), reads=["part", "c128"], writes=[("ps", 3)])
        S.op("dve", lambda e: e.tensor_copy(out=tot[0:16].rearrange("p a b -> p (a b)"), in_=PS(3)[0:16, 0:260]),
             reads=[("ps", 3)], writes=["tot"])
        for hq in range(4):
            kvh = hq // 2 if gi == 0 else hq
            vcol = (384 + kvh * 64) if gi == 0 else (2048 + ((gi - 1) * 4 + hq) * 64)
            S.op("dve", lambda e, hq=hq, vcol=vcol: e.scalar_tensor_tensor(
                out=tot[0:16, hq, 0:64], in0=qkv[0:16, vcol:vcol + 64], scalar=enew[0:16, hq:hq + 1], in1=tot[0:16, hq, 0:64],
                op0=ALU.mult, op1=ALU.add), reads=["qkv", "enew", "tot"], writes=["tot"])
            S.op("dve", lambda e, hq=hq: e.tensor_tensor(out=tot[0:16, hq, 64:65], in0=tot[0:16, hq, 64:65],
                                                          in1=enew[0:16, hq:hq + 1], op=ALU.add), reads=["enew", "tot"], writes=["tot"])
            if gi == 0:
                S.op("dve", lambda e, hq=hq: e.tensor_tensor(out=tot[0:16, hq, 64:65], in0=tot[0:16, hq, 64:65],
                                                              in1=C["expsink"][0:16, hq:hq + 1], op=ALU.add),
                     reads=["tot", "expsink"], writes=["tot"])
        if gi == 0:
            fin, base = tot, 0
        elif gi == 1:
            S.op("dve", lambda e: e.tensor_copy(out=totd[0:16], in_=tot[0:16]), reads=["tot"], writes=["totd"])
            fin = None
        else:
            S.op("dve", lambda e: e.tensor_tensor(out=totd[0:16], in0=totd[0:16], in1=tot[0:16], op=ALU.add),
                 reads=["tot", "totd"], writes=["totd"])
            fin, base = (totd, 4) if gi == 3 else (None, 0)
        if fin is not None:
            key = "tot" if gi == 0 else "totd"
            for hq in range(4):
                S.op("dve", lambda e, fin=fin, hq=hq: e.reciprocal(out=fin[0:16, hq, 64:65], in_=fin[0:16, hq, 64:65]),
                     reads=[key], writes=[key])
                S.op("dve", lambda e, fin=fin, hq=hq, base=base: e.tensor_scalar(
                    out=cat_s[0:16, base + hq, :], in0=fin[0:16, hq, 0:64], scalar1=fin[0:16, hq, 64:65], scalar2=None, op0=ALU.mult),
                    reads=[key], writes=["cat_s"])
    S.barrier()
    A.off = m0


_PROG = None


def kernel(x_prompt, x_sample, cache_swa_kv, cache_dil1_kv, cache_dil2_kv, cache_dil3_kv,
           rel_bias_table, w_in, b_in, attn_sinks, w_o, b_o, ln1_g, ln1_b,
           w_router, b_router, w_up, b_up, w_down, b_down, ln2_g, ln2_b):
    global _PROG
    f = lambda a: np.ascontiguousarray(np.asarray(a, dtype=np.float32))
    xp = f(x_prompt)
    B, SEQ, _ = xp.shape
    c128, coh, cval, crep = host_consts()
    shared = dict(
        table=f(rel_bias_table), w_in=f(w_in)[0], b_in=f(b_in)[0], sinks=f(attn_sinks)[0], w_o=f(w_o)[0],
        b_o=f(b_o)[0], ln1_g=f(ln1_g)[0], ln1_b=f(ln1_b)[0], ln2_g=f(ln2_g)[0], ln2_b=f(ln2_b)[0],
        w_r=f(w_router)[0], b_r=f(b_router)[0], w_up=f(w_up)[0][:NE_DECL], b_up=f(b_up)[0].reshape(NE * 16, 128),
        w_dn=f(w_down)[0][:NE_DECL], b_dn=f(b_down)[0], c128=c128, coh=coh, cval=cval, crep=crep)
    caches = [f(cache_swa_kv)[0], f(cache_dil1_kv)[0], f(cache_dil2_kv)[0], f(cache_dil3_kv)[0]]
    xs = f(x_sample)[:, 0, :]
    in_maps = []
    for c in range(NCORES):
        n, h = c // 2, c % 2
        xwin = np.zeros((HALO + SOWN, D), np.float32)
        xwin[HALO:] = xp[n, h * SOWN:(h + 1) * SOWN]
        fl = np.zeros((128, NUNIT), np.float32)
        fl[:, 1:] = 1.0
        if h == 1:
            xwin[:HALO] = xp[n, SOWN - HALO:SOWN]
            fl[:, 0] = 1.0
        m = dict(shared)
        m["xw"] = xwin
        m["flag"] = fl
        m["xs"] = np.ascontiguousarray(xs[c * NSMP:(c + 1) * NSMP])
        for nm, ca in zip(("c_swa", "c_d1", "c_d2", "c_d3"), caches):
            sl = ca[c * NSMP:(c + 1) * NSMP]
            m[nm] = np.ascontiguousarray(sl.reshape(NSMP, sl.shape[1], -1))
        in_maps.append(m)
    if _PROG is None:
        _PROG = build_program()
    res = run_bass_kernel_spmd(_PROG, in_maps, core_ids=list(range(NCORES)))
    R = res.results
    y_prompt = np.stack([np.concatenate([R[2 * n]["yp"], R[2 * n + 1]["yp"]], axis=0) for n in range(B)]).astype(np.float32)
    y_sample = np.concatenate([R[c]["ys"] for c in range(NCORES)], axis=0)[:, None, :].astype(np.float32)
    pst = []
    for nm, H in (("ps_swa", 2), ("ps_d1", 4), ("ps_d2", 4), ("ps_d3", 4)):
        pst.append(np.stack([R[2 * n + 1][nm] for n in range(B)])[None].astype(np.float32))
    sst = []
    for nm, H in (("ss_swa", 2), ("ss_d1", 4), ("ss_d2", 4), ("ss_d3", 4)):
        a = np.concatenate([R[c][nm] for c in range(NCORES)], axis=0)
        sst.append(a.reshape(1, a.shape[0], a.shape[1], 2, H, 64).astype(np.float32))
    return (y_prompt, y_sample, pst[0], pst[1], pst[2], pst[3], sst[0], sst[1], sst[2], sst[3])
```

```python
import os
import numpy as np
import concourse.bass as bass
import concourse.mybir as mybir
from concourse.bass_utils import run_bass_kernel_spmd

F32 = mybir.dt.float32
BF16 = mybir.dt.bfloat16
I32 = mybir.dt.int32
U8 = mybir.dt.uint8
ALU = mybir.AluOpType
AF = mybir.ActivationFunctionType
AX = mybir.AxisListType

NCORES = 8
D = 1024
QKV = 2816
NE = 32
CAP = 640
UNIT = 2048
HALO = 2048
WIN = HALO + UNIT
NUNIT = 2
SOWN = UNIT * NUNIT
NSMP = 16
NT = SOWN // 128 + 1
ALPHA = float(2.0 ** 0.25)
EPS = 1e-5
BIG = 1.0e6
XGROWS = NE * CAP
FW = 512
ARENA = 186 * 1024
STAGE = int(os.environ.get("K_STAGE", "99"))
NE_DECL = NE if STAGE >= 5 else 1

GROUPS = [
    (1, 127, 128), (1, 128, 128), (4, 128, 512), (16, 128, 2048)]


def t5_bucket_np(n):
    n = np.maximum(np.asarray(n, np.int64), 0)
    ratio = np.log(np.maximum(n, 16).astype(np.float32) / np.float32(16)) / np.float32(np.log(2048 / 16))
    large = 16 + (ratio.astype(np.float32) * np.float32(16)).astype(np.int32)
    return np.where(n < 16, n, np.minimum(large, 31)).astype(np.int64)


def host_consts():
    c128 = np.zeros((128, 3 * 128 + 32 + 2 + 16), np.float32)
    c128[:, 0:128] = np.eye(128, dtype=np.float32)
    c128[:, 128:256] = np.triu(np.ones((128, 128), np.float32), 1)
    c128[:, 256:384] = 1.0
    c128[:, 384:416] = (np.arange(NE, dtype=np.float32) * CAP)[None, :]
    c128[:NSMP, 416] = 1.0
    c128[NSMP:, 417] = BIG
    for p in range(128):
        c128[p, 418 + p // 8] = 1.0
    oh = np.zeros((32, 4, FW), np.float32)
    valid = np.zeros((4, 4, FW), np.float32)
    for gi, (d, maxd, L) in enumerate(GROUPS):
        for u in range(383):
            dist = u - 127
            if 0 <= dist <= maxd:
                oh[t5_bucket_np(dist * d), gi, u] = 1.0
                valid[:, gi, u] = 1.0
        for j in range(129):
            o = 128 - j
            if o <= maxd:
                oh[t5_bucket_np(o * d), gi, 383 + j] = 1.0
                valid[:, gi, 383 + j] = 1.0
    rep = np.zeros((16, 128), np.float32)
    for p in range(128):
        rep[p // 8, p] = 1.0
    return c128, oh.reshape(32, 4 * FW), valid.reshape(4, 4 * FW), rep


class Lazy:
    def __init__(self, f):
        self.f = f


class _Rec:
    def __init__(self):
        self.call = None

    def __getattr__(self, name):
        def m(*args, **kwargs):
            assert self.call is None
            self.call = (name, args, kwargs)
            return self
        return m


def _bind(fn):
    r = _Rec()
    fn(r)
    name, args, kwargs = r.call

    def run(eng):
        kw = {k: (v.f() if isinstance(v, Lazy) else v) for k, v in kwargs.items()}
        return getattr(eng, name)(*args, **kw)
    return run


class Sched:
    ENG = ("pe", "act", "dve", "pool", "sp")

    def __init__(self, nc, esems, dsems, bgsems=()):
        self.nc = nc
        self.bgsem = list(bgsems)
        self.bgcnt = [0] * len(self.bgsem)
        self.bgnext = 0
        self.bgnext_pool = 0
        self.q = {e: [] for e in self.ENG}
        self.cnt = {e: 0 for e in self.ENG}
        self.esem = esems
        self.dsem = dsems
        self.dcnt = [0] * len(dsems)
        self.dnext = 0
        self.dnext_pool = 0
        self.seen = {e: {} for e in self.ENG}
        self.lastw = {}
        self.readers = {}
        self.pending_pe = False

    def _need(self, e, tok):
        k, v = tok
        if k == e and e == "pe":
            return
        if self.seen[e].get(k, 0) >= v:
            return
        self.seen[e][k] = v
        if isinstance(k, str):
            sem = self.esem[k]
        elif k[0] == "bg":
            sem = self.bgsem[k[1]]
        else:
            sem = self.dsem[k[1]]
        self.q[e].append(lambda eng, s=sem, vv=v: eng.wait_ge(s, vv))

    def _deps(self, e, reads, writes):
        toks = []
        for k in reads:
            if k in self.lastw:
                toks.append(self.lastw[k])
        for k in writes:
            if k in self.lastw:
                toks.append(self.lastw[k])
            toks.extend(self.readers.get(k, {}).values())
        for t in toks:
            self._need(e, t)

    def _record(self, tok, reads, writes):
        for k in reads:
            self.readers.setdefault(k, {})[tok[0]] = tok
        for k in writes:
            self.lastw[k] = tok
            self.readers[k] = {}

    def op(self, e, fn, reads=(), writes=(), signal=True):
        fn = _bind(fn)
        self._deps(e, reads, writes)
        tok = (e, self.cnt[e] + 1)
        if signal:
            self.cnt[e] += 1
            sem = self.esem[e]
            self.q[e].append(lambda eng, f=fn, s=sem: f(eng).then_inc(s, 1))
        else:
            assert e == "pe"
            self.q[e].append(lambda eng, f=fn: f(eng))
        self._record(tok, reads, writes)

    def dma(self, e, fn, reads=(), writes=()):
        fn = _bind(fn)
        self._deps(e, reads, writes)
        npool = 16
        if e == "pool":
            idx = self.dnext_pool
            self.dnext_pool = (self.dnext_pool + 1) % npool
        else:
            idx = npool + self.dnext
            self.dnext = (self.dnext + 1) % (len(self.dsem) - npool)
        if self.dcnt[idx] > 0:
            self._need(e, (("dma", idx), self.dcnt[idx] * 16))
        self.dcnt[idx] += 1
        tok = (("dma", idx), self.dcnt[idx] * 16)
        sem = self.dsem[idx]
        self.q[e].append(lambda eng, f=fn, s=sem: f(eng).then_inc(s, 16))
        self._record(tok, reads, writes)

    def dma_bg(self, e, fn, reads=()):
        fn = _bind(fn)
        self._deps(e, reads, ())
        half = len(self.bgsem) // 2
        if e == "pool":
            idx = half + self.bgnext_pool
            self.bgnext_pool = (self.bgnext_pool + 1) % half
        else:
            idx = self.bgnext
            self.bgnext = (self.bgnext + 1) % half
        self.bgcnt[idx] += 1
        sem = self.bgsem[idx]
        self.q[e].append(lambda eng, f=fn, s=sem: f(eng).then_inc(s, 16))

    def wait_bg(self, engines=None, pool_only=False):
        half = len(self.bgsem) // 2
        for e in (engines or self.ENG):
            for i in range(half if pool_only else 0, len(self.bgsem)):
                if self.bgcnt[i] > 0:
                    self._need(e, (("bg", i), self.bgcnt[i] * 16))

    def barrier(self):
        for e in self.ENG:
            for o in ("pe", "act", "dve", "pool"):
                if o != e and self.cnt[o] > 0:
                    self._need(e, (o, self.cnt[o]))
            for i in range(len(self.dsem)):
                if self.dcnt[i] > 0:
                    self._need(e, (("dma", i), self.dcnt[i] * 16))
        self.lastw = {}
        self.readers = {}

    def replay(self, block):
        q = self.q

        @block.tensor
        def _(eng):
            for t in q["pe"]:
                t(eng)

        @block.scalar
        def _(eng):
            for t in q["act"]:
                t(eng)

        @block.vector
        def _(eng):
            for t in q["dve"]:
                t(eng)

        @block.gpsimd
        def _(eng):
            for t in q["pool"]:
                t(eng)

        @block.sync
        def _(eng):
            for t in q["sp"]:
                t(eng)


class Arena:
    def __init__(self, t, nbytes):
        self.t = t
        self.n = nbytes
        self.off = 0

    def alloc(self, shape, dt, nbytes_el):
        free = int(np.prod(shape[1:]))
        nb = free * nbytes_el
        nb = (nb + 63) // 64 * 64
        assert self.off + nb <= self.n, ("arena overflow", self.off, nb, self.n)
        v = self.t[:, self.off:self.off + free * nbytes_el].bitcast(dt)
        self.off += nb
        if len(shape) == 3:
            v = v.rearrange("p (a b) -> p a b", a=shape[1])
        elif len(shape) == 4:
            v = v.rearrange("p (a b c) -> p a b c", a=shape[1], b=shape[2])
        return v

    def f32(self, *shape):
        return self.alloc((128,) + shape, F32, 4)

    def bf(self, *shape):
        return self.alloc((128,) + shape, BF16, 2)

    def i32(self, *shape):
        return self.alloc((128,) + shape, I32, 4)


def build_program():
    nc = bass.Bass("TRN2", target_bir_lowering=False)

    def din(name, shape, dt=F32):
        return nc.dram_tensor(name, list(shape), dt, kind="ExternalInput")

    def dout(name, shape, dt=F32):
        return nc.dram_tensor(name, list(shape), dt, kind="ExternalOutput")

    def dint(name, shape, dt=F32):
        return nc.dram_tensor(name, list(shape), dt, kind="Internal")

    xw = din("xw", [HALO + SOWN, D])
    flag_d = din("flag", [128, NUNIT])
    xs_d = din("xs", [NSMP, D])
    cache_d = [din("c_swa", [NSMP, 128, 256]), din("c_d1", [NSMP, 128, 512]),
               din("c_d2", [NSMP, 512, 512]), din("c_d3", [NSMP, 2048, 512])]
    table_d = din("table", [32, 16])
    w_in_d = din("w_in", [D, QKV])
    b_in_d = din("b_in", [QKV])
    sinks_d = din("sinks", [4])
    w_o_d = din("w_o", [512, D])
    b_o_d = din("b_o", [D])
    ln1g_d = din("ln1_g", [D]); ln1b_d = din("ln1_b", [D])
    ln2g_d = din("ln2_g", [D]); ln2b_d = din("ln2_b", [D])
    w_r_d = din("w_r", [D, NE]); b_r_d = din("b_r", [NE])
    w_up_d = din("w_up", [NE_DECL, D, 2 * D]); b_up_d = din("b_up", [NE * 16, 128])
    w_dn_d = din("w_dn", [NE_DECL, D, D]); b_dn_d = din("b_dn", [NE, D])
    c128_d = din("c128", [128, 434]); coh_d = din("coh", [32, 4 * FW])
    cval_d = din("cval", [4, 4 * FW]); crep_d = din("crep", [16, 128])

    yp_d = dout("yp", [SOWN, D]); ys_d = dout("ys", [NSMP, D])
    ps_d = [dout("ps_swa", [128, 2, 2, 64]), dout("ps_d1", [128, 2, 4, 64]),
            dout("ps_d2", [512, 2, 4, 64]), dout("ps_d3", [2048, 2, 4, 64])]
    ss_d = [dout("ss_swa", [NSMP, 128, 256]), dout("ss_d1", [NSMP, 128, 512]),
            dout("ss_d2", [NSMP, 512, 512]), dout("ss_d3", [NSMP, 2048, 512])]

    X1 = dint("X1", [NT * 128, D])
    XG = dint("XG", [XGROWS, D], BF16)
    YS = dint("YS", [XGROWS + 1, D])
    FD = dint("FD", [16, FW])
    FDR = dint("FDR", [16, 128, FW])
    FDS = dint("FDS", [16, 16, 128])

    import contextlib
    with contextlib.ExitStack() as es:
        arena_t = es.enter_context(nc.sbuf_tensor("arena", [128, ARENA], U8))
        psb = [es.enter_context(nc.psum_tensor("psb%d" % i, [128, 512], F32)) for i in range(8)]
        esems = {e: es.enter_context(nc.semaphore("s_" + e)) for e in ("pe", "act", "dve", "pool")}
        dsems = [es.enter_context(nc.semaphore("d%d" % i)) for i in range(56)]
        bgsems = [es.enter_context(nc.semaphore("g%d" % i)) for i in range(8)]
        es.enter_context(nc.allow_non_contiguous_dma(reason="small strided constant loads"))
        S = Sched(nc, esems, dsems, bgsems)
        A = Arena(arena_t, ARENA)
        emit(nc, S, A, psb, locals())
        block = es.enter_context(nc.Block())
        S.replay(block)
    return nc


def emit(nc, S, A, psb, T):
    xw, flag_d, xs_d, cache_d, table_d = T["xw"], T["flag_d"], T["xs_d"], T["cache_d"], T["table_d"]
    w_in_d, b_in_d, sinks_d, w_o_d, b_o_d = T["w_in_d"], T["b_in_d"], T["sinks_d"], T["w_o_d"], T["b_o_d"]
    X1, XG, YS, FD = T["X1"], T["XG"], T["YS"], T["FD"]
    FDR, FDS = T["FDR"], T["FDS"]
    ps_d, ss_d = T["ps_d"], T["ss_d"]

    def PS(i):
        return psb[i][:, :]

    def PSB(i):
        return psb[i][:, :].bitcast(BF16)

    def bc_row(dram_ap_1d, n):
        return dram_ap_1d.unsqueeze(0).broadcast_to([128, n])

    c128 = A.f32(434)
    ident_f = c128[:, 0:128]
    ones_f = c128[:, 256:384]
    ecap = c128[:, 384:416]
    rowvalid = c128[:, 416:417]
    rowbig = c128[:, 417:418]
    repT = c128[:, 418:434]
    cbf = A.bf(384)
    ident_b = cbf[:, 0:128]
    triu_b = cbf[:, 128:256]
    ones_b = cbf[:, 256:384]
    crep = A.f32(128)
    flag = A.f32(NUNIT)
    bqk = A.f32(22)
    bkdup = A.f32(2)
    expsink = A.f32(4)
    G_all = A.f32(NT, NE)
    g4_all = A.f32(NT, 4)
    dsc_all = A.i32(NT, 4)
    dga_all = A.i32(NT, 4)
    cnt = A.f32(NE)
    wr = A.f32(8, NE)
    brb = A.f32(NE)
    zero_bf = A.bf(2048)
    persist_mark = A.off

    REGS = T["_regs"] = {}

    def _mkregs(eng):
        REGS["sc"] = eng.alloc_register("bc_sc")
        eng.reg_mov(REGS["sc"], XGROWS - 1)
        REGS["ga"] = eng.alloc_register("bc_ga")
        eng.reg_mov(REGS["ga"], XGROWS)
    S.q["pool"].append(_mkregs)
    S.dma("sp", lambda e: e.dma_start(out=c128, in_=T["c128_d"].ap()), writes=["c128"])
    S.dma("sp", lambda e: e.dma_start(out=crep[0:16, :], in_=T["crep_d"].ap()), writes=["crep"])
    S.dma("sp", lambda e: e.dma_start(out=flag, in_=flag_d.ap()), writes=["flag"])
    S.dma("sp", lambda e: e.dma_start(out=bqk, in_=b_in_d.ap().rearrange("(j p) -> p j", p=128)), writes=["bqk"])
    for p in range(2):
        for hh in range(2):
            S.dma("sp", lambda e, p=p, hh=hh: e.dma_start(
                out=bkdup[hh * 64:(hh + 1) * 64, p:p + 1],
                in_=b_in_d.ap()[256 + p * 64:256 + (p + 1) * 64].unsqueeze(1)), writes=["bkdup"])
    S.dma("sp", lambda e: e.dma_start(out=expsink, in_=bc_row(sinks_d.ap(), 4)), writes=["expsink"])
    S.dma("sp", lambda e: e.dma_start(out=wr, in_=T["w_r_d"].ap().rearrange("(k p) n -> p k n", p=128)), writes=["wr"])
    S.dma("sp", lambda e: e.dma_start(out=brb, in_=bc_row(T["b_r_d"].ap(), NE)), writes=["brb"])
    S.op("act", lambda e: e.activation(out=expsink, in_=expsink, func=AF.Exp), reads=["expsink"], writes=["expsink"])
    S.op("dve", lambda e: e.tensor_copy(out=cbf, in_=c128[:, 0:384]), reads=["c128"], writes=["cbf"])
    S.op("pool", lambda e: e.memset(cnt, 0.0), writes=["cnt"])
    S.op("pool", lambda e: e.memset(zero_bf, 0.0), writes=["zero_bf"])

    xg_v = XG.ap().rearrange("(a p r) d -> a p (r d)", p=128, r=2)
    for a in range(XGROWS // 256):
        S.dma_bg("pool", lambda e, a=a: e.dma_start(out=xg_v[a], in_=zero_bf), reads=["zero_bf"])
    S.dma_bg("pool", lambda e: e.dma_start(out=YS.ap()[XGROWS:XGROWS + 1, :].bitcast(BF16),
                                         in_=zero_bf[0:1, :]), reads=["zero_bf"])

    for gi, (d, maxd, L) in enumerate(GROUPS):
        nch = max(1, L // 256)
        rows = (L - 1)
        per = (rows + nch - 1) // nch
        for ch in range(nch):
            r0, r1 = ch * per, min(rows, (ch + 1) * per)
            if r0 >= r1:
                continue
            S.dma_bg("sp", lambda e, gi=gi, r0=r0, r1=r1: e.dma_start(
                out=ss_d[gi].ap()[:, r0:r1, :].rearrange("b r c -> b (r c)"),
                in_=cache_d[gi].ap()[:, r0 + 1:r1 + 1, :].rearrange("b r c -> b (r c)")))

    m0 = A.off
    tab = A.f32(16)
    coh = A.f32(4 * FW)
    cval = A.f32(4 * FW)
    ft = A.f32(4, FW)
    S.dma("sp", lambda e: e.dma_start(out=tab[0:32, :], in_=table_d.ap()), writes=["tab"])
    S.dma("sp", lambda e: e.dma_start(out=coh[0:32, :], in_=T["coh_d"].ap()), writes=["coh"])
    S.dma("sp", lambda e: e.dma_start(out=cval[0:4, :], in_=T["cval_d"].ap()), writes=["cval"])
    for gi in range(4):
        S.op("pe", lambda e, gi=gi: e.matmul(PS(gi)[0:4, :], lhsT=tab[0:32, gi * 4:gi * 4 + 4],
                                             rhs=coh[0:32, gi * FW:(gi + 1) * FW], start=True, stop=True),
             reads=["tab", "coh"], writes=[("ps", gi)])
        S.op("act", lambda e, gi=gi: e.activation(out=ft[0:4, gi, :], in_=PS(gi)[0:4, :], func=AF.Exp),
             reads=[("ps", gi)], writes=[("ft", gi)])
        S.op("dve", lambda e, gi=gi: e.tensor_tensor(out=ft[0:4, gi, :], in0=ft[0:4, gi, :],
                                                      in1=cval[0:4, gi * FW:(gi + 1) * FW], op=ALU.mult),
             reads=[("ft", gi), "cval"], writes=[("ft", gi)])
        S.dma("sp", lambda e, gi=gi: e.dma_start(out=FD.ap()[gi * 4:gi * 4 + 4, :], in_=ft[0:4, gi, :]),
              reads=[("ft", gi)], writes=[("FDw", gi)])
        S.dma("sp", lambda e, gi=gi: e.dma_start(out=FDR.ap()[gi * 4:gi * 4 + 4, :, :],
                                                 in_=ft[0:4, gi, :].unsqueeze(1).broadcast_to([4, 128, FW])),
              reads=[("ft", gi)], writes=[("FDRw", gi)])
        S.dma("sp", lambda e, gi=gi: e.dma_start(out=FDS.ap()[gi * 4:gi * 4 + 4, :, :],
                                                 in_=ft[0:4, gi, 383:383 + 128].unsqueeze(1).broadcast_to([4, 16, 128])),
              reads=[("ft", gi)], writes=[("FDSw", gi)])
    S.barrier()
    A.off = m0
    if STAGE == 1:
        S.barrier()
        S.wait_bg()
        return

    sample_phase(nc, S, A, PS, T, dict(c128=c128, ident_f=ident_f, ident_b=ident_b, repT=repT, crep=crep,
                                        expsink=expsink, bc_row=bc_row, PSB=PSB))
    cat_s = T["_cat_s"]
    xs_t = T["_xs_t"]
    S.barrier()
    if STAGE == 2:
        S.barrier()
        S.wait_bg()
        return

    jobs = []
    for p in range(2):
        jobs.append(dict(gi=0, d=1, maxd=127, qcol=p * 128, kcol=256 + p * 64, vcol=384 + p * 64, dup=True,
                         tcol=[2 * p, 2 * p + 1], slot=[2 * p, 2 * p + 1], first=True, last=True,
                         sink=[2 * p, 2 * p + 1], bq=p, bk=None, st_h=p, pair=p))
    for pr in range(2):
        for g in range(3):
            base = (g * 4 + 2 * pr) * 64
            jobs.append(dict(gi=g + 1, d=GROUPS[g + 1][0], maxd=128, qcol=512 + base, kcol=1280 + base,
                             vcol=2048 + base, dup=False, tcol=[4 + g * 4 + 2 * pr, 4 + g * 4 + 2 * pr + 1],
                             slot=[4 + 2 * pr, 4 + 2 * pr + 1], first=(g == 0), last=(g == 2), sink=None,
                             bq=(512 + base) // 128, bk=(1280 + base) // 128, st_h=2 * pr, pair=pr))

    pm = A.off
    xT = A.bf(8, WIN)
    catT = A.bf(8, UNIT)
    xst = A.f32(D)
    xbf = A.bf(D)
    sfull = [A.f32(256), A.f32(256)]
    wq = A.bf(8, 128)
    wkv = A.bf(8, 256)
    jm = A.off

    tile_ctx = dict(cnt=cnt, G_all=G_all, g4_all=g4_all, dsc_all=dsc_all, dga_all=dga_all, wr=wr, brb=brb,
                    ecap=ecap, rowvalid=rowvalid, rowbig=rowbig, ident_f=ident_f, ident_b=ident_b,
                    triu_b=triu_b, ones_b=ones_b, bc_row=bc_row, PSB=PSB)

    S.wait_bg(pool_only=True)
    A.off = jm
    post_tiles(nc, S, A, PS, T, tile_ctx, None, None, [None], xst, sample=(cat_s, xs_t, catT))
    S.barrier()
    A.off = jm

    for u in range(NUNIT):
        for t in range(WIN // 128):
            S.dma("act", lambda e, t=t, u=u: e.dma_start(out=xst, in_=xw.ap()[u * UNIT + t * 128:u * UNIT + (t + 1) * 128, :]),
                  writes=["xst"])
            S.op("act", lambda e: e.activation(out=xbf, in_=xst, func=AF.Copy), reads=["xst"], writes=["xbf"])
            pb = 6 + (t % 2)
            for kc in range(8):
                S.op("pe", lambda e, kc=kc, pb=pb: e.transpose(PSB(pb)[:, kc * 128:(kc + 1) * 128],
                                                              xbf[:, kc * 128:(kc + 1) * 128], ident_b),
                     reads=["xbf", "cbf"], writes=[("ps", pb)], signal=(kc == 7))
            S.op("dve", lambda e, t=t, pb=pb: e.tensor_copy(
                out=xT[:, :, t * 128:(t + 1) * 128], in_=PSB(pb).rearrange("p (a b) -> p a b", a=8)),
                reads=[("ps", pb)], writes=[("xT", t)])
        xT_keys = [("xT", t) for t in range(WIN // 128)]

        for ji, jb in enumerate(jobs):
            A.off = jm
            d, gi = jb["d"], jb["gi"]
            halo_len = 128 * d
            T0 = HALO - halo_len
            nbo = UNIT // (128 * d)
            nb = nbo + 1
            QT = A.bf(UNIT)
            KT = A.bf(WIN)
            Vaug = A.bf(32, 2, 65)
            acc = A.f32(2, UNIT)
            EB = A.f32(2, 256)
            EBf = A.f32(2, 256)
            Et = [A.bf(512), A.bf(512)]
            Pt = [A.bf(512), A.bf(512)]
            kvb = A.f32(256)
            K = lambda name: (name, 0)
            S.dma("pool", lambda e, jb=jb: e.dma_start(
                out=wq, in_=w_in_d.ap()[:, jb["qcol"]:jb["qcol"] + 128].rearrange("(k p) n -> p k n", p=128)),
                writes=["wq"])
            if jb["dup"]:
                for hh in range(2):
                    S.dma("pool", lambda e, jb=jb, hh=hh: e.dma_start(
                        out=wkv[:, :, hh * 64:(hh + 1) * 64],
                        in_=w_in_d.ap()[:, jb["kcol"]:jb["kcol"] + 64].rearrange("(k p) n -> p k n", p=128)),
                        writes=["wkv"])
                    S.dma("pool", lambda e, jb=jb, hh=hh: e.dma_start(
                        out=wkv[:, :, 128 + hh * 64:128 + (hh + 1) * 64],
                        in_=w_in_d.ap()[:, jb["vcol"]:jb["vcol"] + 64].rearrange("(k p) n -> p k n", p=128)),
                        writes=["wkv"])
                    S.dma("sp", lambda e, jb=jb, hh=hh: e.dma_start(
                        out=kvb[:, hh * 64:(hh + 1) * 64], in_=bc_row(b_in_d.ap()[jb["kcol"]:jb["kcol"] + 64], 64)),
                        writes=["kvb"])
                    S.dma("sp", lambda e, jb=jb, hh=hh: e.dma_start(
                        out=kvb[:, 128 + hh * 64:128 + (hh + 1) * 64],
                        in_=bc_row(b_in_d.ap()[jb["vcol"]:jb["vcol"] + 64], 64)), writes=["kvb"])
            else:
                S.dma("pool", lambda e, jb=jb: e.dma_start(
                    out=wkv[:, :, 0:128],
                    in_=w_in_d.ap()[:, jb["kcol"]:jb["kcol"] + 128].rearrange("(k p) n -> p k n", p=128)),
                    writes=["wkv"])
                S.dma("pool", lambda e, jb=jb: e.dma_start(
                    out=wkv[:, :, 128:256],
                    in_=w_in_d.ap()[:, jb["vcol"]:jb["vcol"] + 128].rearrange("(k p) n -> p k n", p=128)),
                    writes=["wkv"])
                S.dma("sp", lambda e, jb=jb: e.dma_start(
                    out=kvb[:, 0:128], in_=bc_row(b_in_d.ap()[jb["kcol"]:jb["kcol"] + 128], 128)), writes=["kvb"])
                S.dma("sp", lambda e, jb=jb: e.dma_start(
                    out=kvb[:, 128:256], in_=bc_row(b_in_d.ap()[jb["vcol"]:jb["vcol"] + 128], 128)), writes=["kvb"])
            for hh in range(2):
                c = jb["tcol"][hh]
                S.dma("act", lambda e, c=c, hh=hh: e.dma_start(
                    out=EB[:, hh, 0:128], in_=bass.AP(FDR, c * 128 * FW + 255, [[FW - 1, 128], [1, 128]])),
                    reads=["FD"], writes=["EB"])
                S.dma("act", lambda e, c=c, hh=hh: e.dma_start(
                    out=EB[:, hh, 128:256], in_=bass.AP(FDR, c * 128 * FW + 127, [[FW - 1, 128], [1, 128]])),
                    reads=["FD"], writes=["EB"])
            S.op("pool", lambda e, u=u: e.tensor_scalar(out=EBf[:, :, 0:128], in0=EB[:, :, 0:128],
                                                         scalar1=flag[:, u:u + 1], scalar2=None, op0=ALU.mult),
                 reads=["EB", "flag"], writes=["EBf"])
            S.op("pool", lambda e: e.tensor_copy(out=EBf[:, :, 128:256], in_=EB[:, :, 128:256]),
                 reads=["EB"], writes=["EBf"])
            S.op("pool", lambda e: e.memset(Vaug[:, :, :, 64:65], 1.0), writes=["Vones"])
            bqc = bqk[:, jb["bq"]:jb["bq"] + 1]
            for c4 in range(UNIT // 512):
                pb = c4 % 2
                for kc in range(8):
                    S.op("pe", lambda e, kc=kc, pb=pb, c4=c4: e.matmul(
                        PS(pb), lhsT=wq[:, kc, :], rhs=xT[:, kc, HALO + c4 * 512:HALO + (c4 + 1) * 512],
                        start=(kc == 0), stop=(kc == 7)),
                        reads=["wq"] + xT_keys[16 + c4 * 4:16 + c4 * 4 + 4], writes=[("ps", pb)], signal=(kc == 7))
                S.op("dve", lambda e, pb=pb, c4=c4, bqc=bqc: e.tensor_scalar(
                    out=QT[:, c4 * 512:(c4 + 1) * 512], in0=PS(pb), scalar1=bqc, scalar2=0.125,
                    op0=ALU.add, op1=ALU.mult), reads=[("ps", pb), "bqk"], writes=["QT"])
            bkc = bkdup[:, jb["pair"]:jb["pair"] + 1] if jb["dup"] else bqk[:, jb["bk"]:jb["bk"] + 1]
            pos = T0
            ci = 0
            while pos < WIN:
                n = min(512, WIN - pos)
                pb = ci % 2
                for kc in range(8):
                    S.op("pe", lambda e, kc=kc, pb=pb, pos=pos, n=n: e.matmul(
                        PS(pb)[:, 0:n], lhsT=wkv[:, kc, 0:128], rhs=xT[:, kc, pos:pos + n],
                        start=(kc == 0), stop=(kc == 7)),
                        reads=["wkv"] + xT_keys[pos // 128:(pos + n + 127) // 128], writes=[("ps", pb)],
                        signal=(kc == 7))
                S.op("act", lambda e, pb=pb, pos=pos, n=n, bkc=bkc: e.activation(
                    out=KT[:, pos:pos + n], in_=PS(pb)[:, 0:n], func=AF.Identity, bias=bkc, scale=1.0),
                    reads=[("ps", pb), "bqk", "bkdup"], writes=["KT"])
                pos += n
                ci += 1
            for r in range(d):
                for ib in range(nb):
                    blk = r * nb + ib
                    pb = 2 + (blk % 2)
                    start = T0 + r + d * 128 * ib
                    for kc in range(8):
                        S.op("pe", lambda e, kc=kc, pb=pb, start=start, d=d: e.matmul(
                            PS(pb)[:, 0:256], lhsT=xT[:, kc, start:start + 127 * d + 1:d], rhs=wkv[:, kc, :],
                            start=(kc == 0), stop=(kc == 7)),
                            reads=["wkv"] + xT_keys[start // 128:(start + 128 * d + 127) // 128],
                            writes=[("ps", pb)], signal=(kc == 7))
                    st_ib = nbo
                    is_state = (u == NUNIT - 1 and ib == st_ib and not os.environ.get("K_NOSTATE"))
                    if not is_state:
                        S.op("dve", lambda e, pb=pb, blk=blk: e.tensor_tensor(
                            out=Vaug[:, blk, :, 0:64], in0=PS(pb)[:, 128:256].rearrange("p (a b) -> p a b", a=2),
                            in1=kvb[:, 128:256].rearrange("p (a b) -> p a b", a=2), op=ALU.add),
                            reads=[("ps", pb), "kvb"], writes=[("V", blk)])
                    else:
                        sf = sfull[blk % 2]
                        S.op("dve", lambda e, pb=pb, sf=sf: e.tensor_tensor(out=sf, in0=PS(pb)[:, 0:256], in1=kvb, op=ALU.add),
                             reads=[("ps", pb), "kvb"], writes=[("sf", blk % 2)])
                        S.op("pool", lambda e, sf=sf, blk=blk: e.tensor_copy(
                            out=Vaug[:, blk, :, 0:64], in_=sf[:, 128:256].rearrange("p (a b) -> p a b", a=2)),
                            reads=[("sf", blk % 2)], writes=[("V", blk)])
                        for kv in range(2):
                            if jb["dup"]:
                                dst = ps_d[0].ap()[:, kv, jb["st_h"], :]
                                src = sf[:, kv * 128:kv * 128 + 64]
                            else:
                                dst = ps_d[gi].ap()[r::d, kv, jb["st_h"]:jb["st_h"] + 2, :].rearrange("p b c -> p (b c)")
                                src = sf[:, kv * 128:(kv + 1) * 128]
                            S.dma("pool", lambda e, dst=dst, src=src: e.dma_start(out=dst, in_=src),
                                  reads=[("sf", blk % 2)], writes=[("psd", gi, jb["pair"], r, kv)])
            qbs = [(r, ib) for r in range(d) for ib in range(1, nb)]
            for hh in range(2):
                hp = slice(hh * 64, (hh + 1) * 64)
                for bi in range(0, len(qbs), 2):
                    bsel = (bi // 2) % 2
                    stb = 4 + bsel
                    ob = 6 + bsel
                    ST = PS(stb).rearrange("p (a b) -> p a b", a=2)
                    for qi in range(2):
                        r, ib = qbs[bi + qi]
                        qs = r + d * 128 * (ib - 1)
                        qap = QT[hp, qs:qs + 127 * d + 1:d]
                        for side in range(2):
                            ks = T0 + r + d * 128 * (ib - 1 + side)
                            S.op("pe", lambda e, ST=ST, qi=qi, side=side, ks=ks, qap=qap, hp=hp, d=d: e.matmul(
                                ST[:, qi, side * 128:(side + 1) * 128], lhsT=KT[hp, ks:ks + 127 * d + 1:d], rhs=qap,
                                start=True, stop=True),
                                reads=["QT", "KT"], writes=[("ps", stb)], signal=(qi == 1 and side == 1))
                    E = Et[bsel]
                    S.op("act", lambda e, E=E, stb=stb: e.activation(out=E, in_=PS(stb), func=AF.Exp),
                         reads=[("ps", stb)], writes=[("E", bsel)])
                    Pq = Pt[bsel].rearrange("p (a b) -> p a b", a=2)
                    Ev = E.rearrange("p (a b) -> p a b", a=2)
                    for qi in range(2):
                        r, ib = qbs[bi + qi]
                        ebt = EBf if ib == 1 else EB
                        S.op("pool" if qi == 0 else "dve", lambda e, Pq=Pq, Ev=Ev, qi=qi, ebt=ebt, hh=hh: e.tensor_tensor(
                            out=Pq[:, qi, :], in0=Ev[:, qi, :], in1=ebt[:, hh, :], op=ALU.mult),
                            reads=[("E", bsel), "EB", "EBf"], writes=[("P", bsel, qi)])
                    OT = PS(ob).rearrange("p (a b) -> p a b", a=4)
                    for qi in range(2):
                        r, ib = qbs[bi + qi]
                        for side in range(2):
                            blk = r * nb + ib - 1 + side
                            S.op("pe", lambda e, OT=OT, qi=qi, side=side, blk=blk, Pq=Pq, hh=hh: e.matmul(
                                OT[0:65, qi, :], lhsT=Vaug[:, blk, hh, :], rhs=Pq[:, qi, side * 128:(side + 1) * 128],
                                start=(side == 0), stop=(side == 1)),
                                reads=[("V", blk), "Vones", ("P", bsel, qi)], writes=[("ps", ob)],
                                signal=(qi == 1 and side == 1))
                    for qi in range(2):
                        r, ib = qbs[bi + qi]
                        qs = r + d * 128 * (ib - 1)
                        dst = acc[0:65, hh, qs:qs + 127 * d + 1:d]
                        if jb["first"]:
                            S.op("act", lambda e, dst=dst, OT=OT, qi=qi: e.activation(out=dst, in_=OT[0:65, qi, :], func=AF.Identity),
                                 reads=[("ps", ob)], writes=[("acc", hh)])
                        else:
                            S.op("dve", lambda e, dst=dst, OT=OT, qi=qi: e.tensor_tensor(
                                out=dst, in0=OT[0:65, qi, :], in1=dst, op=ALU.add),
                                reads=[("ps", ob)], writes=[("acc", hh)])
            if jb["last"]:
                for hh in range(2):
                    slot = jb["slot"][hh]
                    den = acc[64:65, hh, :]
                    if jb["sink"] is not None:
                        si = jb["sink"][hh]
                        S.op("dve", lambda e, den=den, si=si: e.tensor_scalar(
                            out=den, in0=den, scalar1=expsink[64:65, si:si + 1], scalar2=None, op0=ALU.add),
                            reads=[("acc", hh), "expsink"], writes=[("acc", hh)])
                    S.op("dve", lambda e, den=den: e.reciprocal(out=den, in_=den), reads=[("acc", hh)], writes=[("acc", hh)])
                    for c4 in range(UNIT // 512):
                        pb = c4 % 2
                        S.op("pe", lambda e, pb=pb, c4=c4, hh=hh: e.matmul(
                            PS(pb)[0:64, :], lhsT=ones_f[64:65, 0:64], rhs=acc[64:65, hh, c4 * 512:(c4 + 1) * 512],
                            start=True, stop=True), reads=[("acc", hh), "c128"], writes=[("ps", pb)])
                        S.op("dve", lambda e, pb=pb, c4=c4, hh=hh, slot=slot: e.tensor_tensor(
                            out=catT[0:64, slot, c4 * 512:(c4 + 1) * 512], in0=acc[0:64, hh, c4 * 512:(c4 + 1) * 512],
                            in1=PS(pb)[0:64, :], op=ALU.mult), reads=[("ps", pb), ("acc", hh)], writes=[("catT", slot)])
        if STAGE == 3:
            S.barrier()
            S.wait_bg()
            return
        S.barrier()
        S.wait_bg()
        A.off = jm
        if STAGE == 32 and u == 1:
            return
        post_tiles(nc, S, A, PS, T, tile_ctx, catT, u, list(range(UNIT // 128)), xst)
        S.barrier()
        if STAGE == 31 or (STAGE == 33 and u == 1):
            S.wait_bg()
            return
    A.off = persist_mark
    if STAGE == 4:
        S.barrier()
        S.wait_bg()
        return

    moe_phase(nc, S, A, PS, PSB, T, ident_b)
    S.barrier()
    A.off = persist_mark
    if STAGE == 5:
        S.barrier()
        S.wait_bg()
        return
    combine_phase(nc, S, A, PS, T, tile_ctx)
    S.barrier()
    S.wait_bg()


def layer_norm_tile(S, A_tiles, z, g_b, b_b, out, key_in, key_out):
    junk, st = A_tiles["junk"], A_tiles["st"]
    S.op("act", lambda e: e.activation(out=junk, in_=z, func=AF.Identity, accum_out=st[:, 0:1]),
         reads=[key_in], writes=["ln_junk", "ln_st0"])
    S.op("act", lambda e: e.activation(out=junk, in_=z, func=AF.Square, accum_out=st[:, 1:2]),
         reads=[key_in], writes=["ln_junk", "ln_st1"])
    S.op("dve", lambda e: e.tensor_scalar(out=st[:, 2:3], in0=st[:, 0:1], scalar1=1.0 / D, scalar2=None, op0=ALU.mult),
         reads=["ln_st0"], writes=["ln_st2"])
    S.op("dve", lambda e: e.tensor_tensor(out=st[:, 3:4], in0=st[:, 2:3], in1=st[:, 2:3], op=ALU.mult),
         reads=["ln_st2"], writes=["ln_st3"])
    S.op("dve", lambda e: e.scalar_tensor_tensor(out=st[:, 4:5], in0=st[:, 1:2], scalar=1.0 / D, in1=st[:, 3:4],
                                                  op0=ALU.mult, op1=ALU.subtract),
         reads=["ln_st1", "ln_st3"], writes=["ln_st4"])
    S.op("dve", lambda e: e.tensor_scalar(out=st[:, 4:5], in0=st[:, 4:5], scalar1=EPS, scalar2=None, op0=ALU.add),
         reads=["ln_st4"], writes=["ln_st4"])
    S.op("act", lambda e: e.activation(out=st[:, 5:6], in_=st[:, 4:5], func=AF.Ln), reads=["ln_st4"], writes=["ln_st5"])
    S.op("act", lambda e: e.activation(out=st[:, 5:6], in_=st[:, 5:6], func=AF.Exp, scale=-0.5), reads=["ln_st5"], writes=["ln_st5"])
    S.op("dve", lambda e: e.scalar_tensor_tensor(out=st[:, 6:7], in0=st[:, 2:3], scalar=-1.0, in1=st[:, 5:6],
                                                  op0=ALU.mult, op1=ALU.mult),
         reads=["ln_st2", "ln_st5"], writes=["ln_st6"])
    S.op("act", lambda e: e.activation(out=junk, in_=z, func=AF.Identity, bias=st[:, 6:7], scale=st[:, 5:6]),
         reads=[key_in, "ln_st5", "ln_st6"], writes=["ln_junk"])
    S.op("pool", lambda e: e.tensor_tensor(out=junk, in0=junk, in1=g_b, op=ALU.mult),
         reads=["ln_junk", "lnconst"], writes=["ln_junk"])
    S.op("pool", lambda e: e.tensor_tensor(out=out, in0=junk, in1=b_b, op=ALU.add),
         reads=["ln_junk", "lnconst"], writes=[key_out])


def post_tiles(nc, S, A, PS, T, C, catT, u, tiles, xst, sample=None):
    bc_row = C["bc_row"]
    X1, XG = T["X1"], T["XG"]
    w_o = A.bf(8, D)
    bo_b = A.f32(D)
    g_b = A.f32(D)
    b_b = A.f32(D)
    z = A.f32(D)
    junk = A.f32(D)
    x1 = A.f32(D)
    x1bf = A.bf(D)
    x1T = A.f32(8, 128)
    st = A.f32(8)
    lg = A.f32(NE)
    m8 = A.f32(8)
    mask = A.f32(NE)
    mask_b = A.bf(NE)
    ex = A.f32(NE)
    ssum = A.f32(2)
    posc = A.f32(NE)
    big = A.f32(NE)
    dst_f = A.f32(8)
    scr = A.f32(NE)
    S.dma("pool", lambda e: e.dma_start(out=w_o[0:64, :, :], in_=T["w_o_d"].ap().rearrange("(s p) n -> p s n", p=64)),
          writes=["w_o"])
    S.dma("sp", lambda e: e.dma_start(out=bo_b, in_=bc_row(T["b_o_d"].ap(), D)), writes=["lnconst"])
    S.dma("sp", lambda e: e.dma_start(out=g_b, in_=bc_row(T["ln1g_d"].ap(), D)), writes=["lnconst"])
    S.dma("sp", lambda e: e.dma_start(out=b_b, in_=bc_row(T["ln1b_d"].ap(), D)), writes=["lnconst"])
    if sample is not None:
        cat_s, xs_t, catT_buf = sample
        catT = catT_buf
        for slot in range(8):
            S.op("pe", lambda e, slot=slot: e.transpose(PS(4 + slot // 4)[0:64, (slot % 4) * 128:(slot % 4) * 128 + 128],
                                                        cat_s[:, slot, :], C["ident_f"]),
                 reads=["cat_s", "c128"], writes=[("ps", 4 + slot // 4)], signal=(slot % 4 == 3))
        for half in range(2):
            S.op("dve", lambda e, half=half: e.tensor_copy(
                out=catT[0:64, half * 4:half * 4 + 4, 0:128], in_=PS(4 + half)[0:64, :].rearrange("p (a b) -> p a b", a=4)),
                reads=[("ps", 4 + half)], writes=[("catT", "s")])
    for tl in tiles:
        if sample is None:
            ti = u * (UNIT // 128) + tl
            tok0 = tl * 128
            S.dma("sp", lambda e, ti=ti: e.dma_start(out=xst, in_=T["xw"].ap()[HALO + ti * 128:HALO + (ti + 1) * 128, :]),
                  writes=["xst"])
            xin = xst
        else:
            ti = NT - 1
            tok0 = 0
            xin = sample[1]
        for half in range(2):
            for slot in range(8):
                S.op("pe", lambda e, half=half, slot=slot, tok0=tok0: e.matmul(
                    PS(half), lhsT=catT[0:64, slot, tok0:tok0 + 128], rhs=w_o[0:64, slot, half * 512:(half + 1) * 512],
                    start=(slot == 0), stop=(slot == 7)),
                    reads=["w_o"] + [("catT", s) for s in list(range(8)) + ["s"]], writes=[("ps", half)], signal=(slot == 7))
        S.op("pool", lambda e, xin=xin: e.tensor_scalar(out=z, in0=xin, scalar1=ALPHA, scalar2=None, op0=ALU.mult),
             reads=["xst", "xs_t"], writes=["z"])
        S.op("pool", lambda e: e.tensor_tensor(out=z, in0=z, in1=bo_b, op=ALU.add), reads=["z", "lnconst"], writes=["z"])
        for half in range(2):
            S.op("dve", lambda e, half=half: e.tensor_tensor(out=z[:, half * 512:(half + 1) * 512],
                                                              in0=z[:, half * 512:(half + 1) * 512], in1=PS(half), op=ALU.add),
                 reads=["z", ("ps", half)], writes=["z"])
        layer_norm_tile(S, dict(junk=junk, st=st), z, g_b, b_b, x1, "z", "x1")
        S.dma("pool", lambda e, ti=ti: e.dma_start(out=X1.ap()[ti * 128:(ti + 1) * 128, :], in_=x1), reads=["x1"], writes=[("X1", ti)])
        S.op("act", lambda e: e.activation(out=x1bf, in_=x1, func=AF.Copy), reads=["x1"], writes=["x1bf"])
        for kc in range(8):
            S.op("pe", lambda e, kc=kc: e.transpose(PS(2 + kc // 4)[:, (kc % 4) * 128:(kc % 4) * 128 + 128],
                                                    x1[:, kc * 128:(kc + 1) * 128], C["ident_f"]),
                 reads=["x1", "c128"], writes=[("ps", 2 + kc // 4)], signal=(kc % 4 == 3))
        for half in range(2):
            S.op("act", lambda e, half=half: e.activation(
                out=x1T[:, half * 4:half * 4 + 4, :], in_=PS(2 + half).rearrange("p (a b) -> p a b", a=4), func=AF.Identity),
                reads=[("ps", 2 + half)], writes=[("x1T", half)])
        for kc in range(8):
            S.op("pe", lambda e, kc=kc: e.matmul(PS(4)[:, 0:NE], lhsT=x1T[:, kc, :], rhs=C["wr"][:, kc, :],
                                                 start=(kc == 0), stop=(kc == 7)),
                 reads=[("x1T", kc // 4), "wr"], writes=[("ps", 4)], signal=(kc == 7))
        S.op("dve", lambda e: e.tensor_tensor(out=lg, in0=PS(4)[:, 0:NE], in1=C["brb"], op=ALU.add),
             reads=[("ps", 4), "brb"], writes=["lg"])
        S.op("dve", lambda e: e.max(out=m8, in_=lg), reads=["lg"], writes=["m8"])
        S.op("dve", lambda e: e.tensor_scalar(out=mask, in0=lg, scalar1=m8[:, 3:4], scalar2=None, op0=ALU.is_ge),
             reads=["lg", "m8"], writes=["mask"])
        if sample is not None:
            S.op("dve", lambda e: e.tensor_scalar(out=mask, in0=mask, scalar1=C["rowvalid"], scalar2=None, op0=ALU.mult),
                 reads=["mask", "c128"], writes=["mask"])
        S.op("dve", lambda e: e.tensor_scalar(out=ssum[:, 0:1], in0=m8[:, 0:1], scalar1=-1.0, scalar2=None, op0=ALU.mult),
             reads=["m8"], writes=["nmax"])
        S.op("act", lambda e: e.activation(out=ex, in_=lg, func=AF.Exp, bias=ssum[:, 0:1], scale=1.0),
             reads=["lg", "nmax"], writes=["ex"])
        S.op("dve", lambda e: e.tensor_tensor(out=ex, in0=ex, in1=mask, op=ALU.mult), reads=["ex", "mask"], writes=["ex"])
        S.op("dve", lambda e: e.reduce_sum(out=ssum[:, 1:2], in_=ex, axis=AX.X), reads=["ex"], writes=["esum"])
        if sample is not None:
            S.op("dve", lambda e: e.tensor_tensor(out=ssum[:, 1:2], in0=ssum[:, 1:2], in1=C["rowbig"], op=ALU.add),
                 reads=["esum", "c128"], writes=["esum"])
        S.op("dve", lambda e: e.reciprocal(out=ssum[:, 1:2], in_=ssum[:, 1:2]), reads=["esum"], writes=["esum"])
        Gt = C["G_all"][:, ti, :]
        S.op("dve", lambda e, Gt=Gt: e.tensor_scalar(out=Gt, in0=ex, scalar1=ssum[:, 1:2], scalar2=None, op0=ALU.mult),
             reads=["ex", "esum"], writes=["G"])
        S.op("act", lambda e: e.activation(out=mask_b, in_=mask, func=AF.Copy), reads=["mask"], writes=["mask_b"])
        S.op("pe", lambda e: e.matmul(PS(5)[:, 0:NE], lhsT=C["triu_b"], rhs=mask_b, start=True, stop=True),
             reads=["mask_b", "cbf"], writes=[("ps", 5)])
        S.op("pe", lambda e: e.matmul(PS(5)[:, 64:64 + NE], lhsT=C["ones_b"], rhs=mask_b, start=True, stop=True),
             reads=["mask_b", "cbf"], writes=[("ps", 5)])
        S.op("dve", lambda e: e.tensor_tensor(out=posc, in0=PS(5)[:, 0:NE], in1=C["cnt"], op=ALU.add),
             reads=[("ps", 5), "cnt"], writes=["posc"])
        S.op("dve", lambda e: e.tensor_tensor(out=C["cnt"], in0=PS(5)[:, 64:64 + NE], in1=C["cnt"], op=ALU.add),
             reads=[("ps", 5), "posc"], writes=["cnt"])
        S.op("dve", lambda e: e.tensor_scalar(out=big, in0=posc, scalar1=float(CAP) - 0.5, scalar2=BIG, op0=ALU.is_gt, op1=ALU.mult),
             reads=["posc"], writes=["big"])
        S.op("dve", lambda e: e.tensor_tensor(out=posc, in0=posc, in1=C["ecap"], op=ALU.add), reads=["posc", "c128"], writes=["posc"])
        S.op("dve", lambda e: e.tensor_tensor(out=posc, in0=posc, in1=big, op=ALU.add), reads=["posc", "big"], writes=["posc"])
        if sample is not None:
            S.op("dve", lambda e: e.tensor_scalar(out=posc, in0=posc, scalar1=C["rowbig"], scalar2=None, op0=ALU.add),
                 reads=["posc", "c128"], writes=["posc"])
        for k in range(4):
            S.op("dve", lambda e, k=k: e.scalar_tensor_tensor(
                out=scr, in0=lg, scalar=m8[:, k:k + 1], in1=posc, op0=ALU.is_equal, op1=ALU.mult, accum_out=dst_f[:, k:k + 1]),
                reads=["lg", "m8", "posc"], writes=["scr", ("dstf", k)])
            S.op("dve", lambda e, k=k, Gt=Gt, ti=ti: e.scalar_tensor_tensor(
                out=scr, in0=lg, scalar=m8[:, k:k + 1], in1=Gt, op0=ALU.is_equal, op1=ALU.mult,
                accum_out=C["g4_all"][:, ti, k:k + 1]),
                reads=["lg", "m8", "G"], writes=["scr", "g4"])
        S.op("dve", lambda e, ti=ti: e.tensor_copy(out=C["dsc_all"][:, ti, :], in_=dst_f[:, 0:4]),
             reads=[("dstf", k) for k in range(4)], writes=["dsc"])
        S.op("dve", lambda e: e.tensor_scalar(out=dst_f[:, 4:8], in0=dst_f[:, 0:4], scalar1=float(XGROWS), scalar2=None, op0=ALU.min),
             reads=[("dstf", k) for k in range(4)], writes=["dstf2"])
        S.op("dve", lambda e, ti=ti: e.tensor_copy(out=C["dga_all"][:, ti, :], in_=dst_f[:, 4:8]),
             reads=["dstf2"], writes=["dga"])
        for k in range(4):
            S.dma("pool", lambda e, k=k, ti=ti: e.indirect_dma_start(
                out=XG.ap(), out_offset=bass.IndirectOffsetOnAxis(ap=C["dsc_all"][:, ti, k:k + 1], axis=0),
                in_=x1bf, in_offset=None, bounds_check=Lazy(lambda: T["_regs"]["sc"]), oob_is_err=False),
                reads=["x1bf", "dsc", "XG"], writes=[("XGs", ti, k)])


def moe_phase(nc, S, A, PS, PSB, T, ident_b):
    XG, YS = T["XG"], T["YS"]
    w_up_d, w_dn_d = T["w_up_d"], T["w_dn_d"]
    wu = [A.bf(8, 2 * D), A.bf(8, 2 * D)]
    wd = [A.bf(8, D), A.bf(8, D)]
    xg = [A.bf(CAP // 128, D), A.bf(CAP // 128, D)]
    xgT = [A.bf(8, CAP), A.bf(8, CAP)]
    aT = A.bf(8, CAP)
    gt = [A.f32(512), A.f32(512)]
    sg = [A.f32(512), A.f32(512)]
    t2 = [A.f32(512), A.f32(512)]
    yst = [A.f32(D), A.f32(D)]
    bupT = A.f32(NE * 16)
    bst = A.f32(128)
    for a in range(4):
        S.dma("sp", lambda e, a=a: e.dma_start(out=bst, in_=T["b_up_d"].ap()[a * 128:(a + 1) * 128, :]), writes=["bst"])
        S.op("pe", lambda e, a=a: e.transpose(PS(0)[:, a * 128:(a + 1) * 128], bst, T["_ident_f"]),
             reads=["bst"], writes=[("ps", 0)])
        S.op("dve", lambda e, a=a: e.tensor_copy(out=bupT[:, a * 128:(a + 1) * 128], in_=PS(0)[:, a * 128:(a + 1) * 128]),
             reads=[("ps", 0)], writes=["bupT"])
    bup1 = A.f32(NE * 16)
    S.op("dve", lambda e: e.tensor_scalar(out=bup1, in0=bupT, scalar1=1.0, scalar2=None, op0=ALU.add),
         reads=["bupT"], writes=["bup1"])
    pieces = [(0, 512), (512, CAP - 512)]

    def load_w(ex):
        b = ex % 2
        for kc2 in range(4):
            S.dma("pool", lambda e, ex=ex, b=b, kc2=kc2: e.dma_start(
                out=wu[b][:, 2 * kc2:2 * kc2 + 2, :],
                in_=w_up_d.ap()[ex, kc2 * 256:(kc2 + 1) * 256, :].rearrange("(k p) n -> p k n", p=128)),
                writes=[("wu", b, kc2)])
        for kc2 in range(2):
            S.dma("pool", lambda e, ex=ex, b=b, kc2=kc2: e.dma_start(
                out=wd[b][:, 4 * kc2:4 * kc2 + 4, :],
                in_=w_dn_d.ap()[ex, kc2 * 512:(kc2 + 1) * 512, :].rearrange("(k p) n -> p k n", p=128)),
                writes=[("wd", b, kc2)])

    def load_xg(ex):
        S.dma("pool", lambda e, ex=ex: e.dma_start(
            out=xg[ex % 2], in_=XG.ap()[ex * CAP:(ex + 1) * CAP, :].rearrange("(a p) d -> p a d", p=128)),
            reads=["XGall"], writes=[("xg", ex % 2)])

    def transposes(ex):
        xb = ex % 2
        for a in range(CAP // 128):
            pb = 6 + (a % 2)
            for kc in range(8):
                S.op("pe", lambda e, a=a, kc=kc, pb=pb, xb=xb: e.transpose(PSB(pb)[:, kc * 128:(kc + 1) * 128],
                                                                           xg[xb][:, a, kc * 128:(kc + 1) * 128], ident_b),
                     reads=[("xg", xb)], writes=[("ps", pb)], signal=(kc == 7))
            S.op("act", lambda e, a=a, pb=pb, xb=xb: e.activation(
                out=xgT[xb][:, :, a * 128:(a + 1) * 128], in_=PSB(pb).rearrange("p (a b) -> p a b", a=8), func=AF.Copy),
                reads=[("ps", pb)], writes=[("xgT", xb, a)])

    load_w(0)
    load_xg(0)
    transposes(0)
    load_xg(1)
    for ex in range(NE):
        b = ex % 2
        if ex + 1 < NE:
            load_w(ex + 1)
        xgT_keys = [("xgT", b, a) for a in range(CAP // 128)]
        it = 0
        for (s0, sn) in pieces:
            for j in range(8):
                pg = (it % 2) * 2
                pl = pg + 1
                for (pb, fo) in ((pg, j), (pl, 8 + j)):
                    for kc in range(8):
                        S.op("pe", lambda e, pb=pb, fo=fo, kc=kc, s0=s0, sn=sn, b=b: e.matmul(
                            PS(pb)[:, 0:sn], lhsT=wu[b][:, kc, fo * 128:(fo + 1) * 128], rhs=xgT[b][:, kc, s0:s0 + sn],
                            start=(kc == 0), stop=(kc == 7)),
                            reads=[("wu", b, kc // 2)] + xgT_keys, writes=[("ps", pb)], signal=(kc == 7))
                bi = it % 2
                bg = bupT[:, ex * 16 + j:ex * 16 + j + 1]
                bl = bupT[:, ex * 16 + 8 + j:ex * 16 + 8 + j + 1]
                g_, s_, t_ = gt[bi][:, 0:sn], sg[bi][:, 0:sn], t2[bi][:, 0:sn]
                S.op("dve", lambda e, pg=pg, g_=g_, bg=bg, sn=sn: e.tensor_scalar(
                    out=g_, in0=PS(pg)[:, 0:sn], scalar1=bg, scalar2=7.0, op0=ALU.add, op1=ALU.min),
                    reads=[("ps", pg), "bupT"], writes=[("g", bi)])
                S.op("act", lambda e, g_=g_, s_=s_: e.activation(out=s_, in_=g_, func=AF.Sigmoid, scale=1.702),
                     reads=[("g", bi)], writes=[("sg", bi)])
                bl1 = bup1[:, ex * 16 + 8 + j:ex * 16 + 8 + j + 1]
                S.op("dve", lambda e, pl=pl, t_=t_, bl1=bl1, sn=sn: e.tensor_scalar(
                    out=t_, in0=PS(pl)[:, 0:sn], scalar1=bl1, scalar2=8.0, op0=ALU.add, op1=ALU.min),
                    reads=[("ps", pl), "bup1"], writes=[("t2", bi)])
                S.op("dve", lambda e, t_=t_, g_=g_: e.scalar_tensor_tensor(
                    out=t_, in0=t_, scalar=-6.0, in1=g_, op0=ALU.max, op1=ALU.mult),
                    reads=[("t2", bi), ("g", bi)], writes=[("t2", bi)])
                S.op("pool", lambda e, s_=s_, t_=t_, j=j, s0=s0, sn=sn: e.tensor_tensor(
                    out=aT[:, j, s0:s0 + sn], in0=t_, in1=s_, op=ALU.mult),
                    reads=[("sg", bi), ("t2", bi)], writes=[("aT", j, s0)])
                it += 1
        if ex + 1 < NE:
            transposes(ex + 1)
            if ex + 2 < NE:
                load_xg(ex + 2)
        aT_keys = [("aT", j, s0) for j in range(8) for (s0, sn) in pieces]
        for a in range(CAP // 128):
            yt = yst[a % 2]
            for half in range(2):
                pb = 4 + half
                for fc in range(8):
                    S.op("pe", lambda e, a=a, half=half, fc=fc, pb=pb, b=b: e.matmul(
                        PS(pb), lhsT=aT[:, fc, a * 128:(a + 1) * 128], rhs=wd[b][:, fc, half * 512:(half + 1) * 512],
                        start=(fc == 0), stop=(fc == 7)),
                        reads=aT_keys + [("wd", b, fc // 4)], writes=[("ps", pb)], signal=(fc == 7))
                S.op("act" if half == 0 else "dve",
                     (lambda e, yt=yt, pb=pb, half=half: e.activation(out=yt[:, half * 512:(half + 1) * 512], in_=PS(pb), func=AF.Identity))
                     if half == 0 else
                     (lambda e, yt=yt, pb=pb, half=half: e.tensor_copy(out=yt[:, half * 512:(half + 1) * 512], in_=PS(pb))),
                     reads=[("ps", pb)], writes=[("yst", a % 2, half)])
            S.dma("act", lambda e, ex=ex, a=a, yt=yt: e.dma_start(
                out=YS.ap()[ex * CAP + a * 128:ex * CAP + (a + 1) * 128, :], in_=yt),
                reads=[("yst", a % 2, 0), ("yst", a % 2, 1)], writes=[("YS", ex, a)])


def combine_phase(nc, S, A, PS, T, C):
    bc_row = C["bc_row"]
    X1, YS = T["X1"], T["YS"]
    g_b = A.f32(D)
    b_b = A.f32(D)
    bdn = A.f32(D)
    yk = [A.f32(D) for _ in range(4)]
    x1 = A.f32(D)
    z = A.f32(D)
    junk = A.f32(D)
    out = A.f32(D)
    st = A.f32(8)
    GT = A.f32(128)
    S.dma("sp", lambda e: e.dma_start(out=g_b, in_=bc_row(T["ln2g_d"].ap(), D)), writes=["lnconst"])
    S.dma("sp", lambda e: e.dma_start(out=b_b, in_=bc_row(T["ln2b_d"].ap(), D)), writes=["lnconst"])
    S.dma("sp", lambda e: e.dma_start(out=bdn[0:NE, :], in_=T["b_dn_d"].ap()), writes=["bdn"])
    for ti in range(NT):
        S.dma("pool", lambda e, ti=ti: e.dma_start(out=x1, in_=X1.ap()[ti * 128:(ti + 1) * 128, :]), writes=["x1"])
        for k in range(4):
            S.dma("pool", lambda e, k=k, ti=ti: e.indirect_dma_start(
                out=yk[k], out_offset=None, in_=YS.ap(),
                in_offset=bass.IndirectOffsetOnAxis(ap=C["dga_all"][:, ti, k:k + 1], axis=0),
                bounds_check=Lazy(lambda: T["_regs"]["ga"]), oob_is_err=False), writes=[("yk", k)])
        S.op("pe", lambda e, ti=ti: e.transpose(PS(2)[0:NE, 0:128], C["G_all"][:, ti, :], C["ident_f"]),
             writes=[("ps", 2)])
        S.op("act", lambda e: e.activation(out=GT[0:NE, :], in_=PS(2)[0:NE, 0:128], func=AF.Identity),
             reads=[("ps", 2)], writes=["GT"])
        for half in range(2):
            S.op("pe", lambda e, half=half: e.matmul(PS(half), lhsT=GT[0:NE, :], rhs=bdn[0:NE, half * 512:(half + 1) * 512],
                                                     start=True, stop=True), reads=["GT", "bdn"], writes=[("ps", half)])
        S.op("pool", lambda e: e.tensor_scalar(out=z, in0=x1, scalar1=ALPHA, scalar2=None, op0=ALU.mult),
             reads=["x1"], writes=["z"])
        for k in range(4):
            S.op("dve", lambda e, k=k, ti=ti: e.scalar_tensor_tensor(
                out=z, in0=yk[k], scalar=C["g4_all"][:, ti, k:k + 1], in1=z, op0=ALU.mult, op1=ALU.add),
                reads=[("yk", k), "z"], writes=["z"])
        for half in range(2):
            S.op("dve", lambda e, half=half: e.tensor_tensor(out=z[:, half * 512:(half + 1) * 512],
                                                              in0=z[:, half * 512:(half + 1) * 512], in1=PS(half), op=ALU.add),
                 reads=["z", ("ps", half)], writes=["z"])
        layer_norm_tile(S, dict(junk=junk, st=st), z, g_b, b_b, out, "z", "out")
        if ti < NT - 1:
            S.dma("pool", lambda e, ti=ti: e.dma_start(out=T["yp_d"].ap()[ti * 128:(ti + 1) * 128, :], in_=out),
                  reads=["out"], writes=[("yp", ti)])
        else:
            S.dma("sp", lambda e: e.dma_start(out=T["ys_d"].ap(), in_=out[0:NSMP, :]), reads=["out"], writes=["ys"])


def sample_phase(nc, S, A, PS, T, C):
    bc_row = C["bc_row"]
    cache_d, ss_d = T["cache_d"], T["ss_d"]
    FD = T["FD"]
    cat_s = A.f32(8, 64)
    xs_t = A.f32(D)
    T["_cat_s"] = cat_s
    T["_xs_t"] = xs_t
    T["_ident_f"] = C["ident_f"]
    m0 = A.off
    w_in = A.bf(8, QKV)
    xsb = A.bf(D)
    xsT = A.bf(8, 128)
    binb = A.f32(QKV)
    qkv = A.f32(QKV)
    qrep = A.f32(1024)
    ck = A.f32(16, 512)
    ebs = A.f32(4, 16)
    prod = A.f32(16, 64)
    sc = A.f32(4, 16)
    part = A.f32(4, 65)
    tot = A.f32(4, 65)
    totd = A.f32(4, 65)
    snew = A.f32(16)
    enew = A.f32(16)
    f0 = A.f32(16)
    pr16 = A.f32(64)
    S.op("pool", lambda e: e.memset(xs_t, 0.0), writes=["xs_t"])
    S.op("pool", lambda e: e.memset(cat_s, 0.0), writes=["cat_s"])
    S.dma("sp", lambda e: e.dma_start(out=xs_t[0:NSMP, :], in_=T["xs_d"].ap()), writes=["xs_t"])
    for kc2 in range(4):
        S.dma("pool", lambda e, kc2=kc2: e.dma_start(
            out=w_in[:, 2 * kc2:2 * kc2 + 2, :],
            in_=T["w_in_d"].ap()[kc2 * 256:(kc2 + 1) * 256, :].rearrange("(k p) n -> p k n", p=128)), writes=["w_in"])
    S.dma("sp", lambda e: e.dma_start(out=binb, in_=bc_row(T["b_in_d"].ap(), QKV)), writes=["binb"])
    S.dma("sp", lambda e: e.dma_start(out=f0, in_=bass.AP(FD, 383 + 128, [[0, 128], [FW, 16]])), reads=["FD"], writes=["f0"])
    S.op("act", lambda e: e.activation(out=xsb, in_=xs_t, func=AF.Copy), reads=["xs_t"], writes=["xsb"])
    for kc in range(8):
        S.op("pe", lambda e, kc=kc: e.transpose(C["PSB"](6)[:, kc * 128:(kc + 1) * 128], xsb[:, kc * 128:(kc + 1) * 128], C["ident_b"]),
             reads=["xsb", "cbf"], writes=[("ps", 6)], signal=(kc == 7))
    S.op("dve", lambda e: e.tensor_copy(out=xsT, in_=C["PSB"](6).rearrange("p (a b) -> p a b", a=8)),
         reads=[("ps", 6)], writes=["xsT"])
    c0 = 0
    ci = 0
    while c0 < QKV:
        n = min(512, QKV - c0)
        pb = ci % 2
        for kc in range(8):
            S.op("pe", lambda e, kc=kc, pb=pb, c0=c0, n=n: e.matmul(
                PS(pb)[:, 0:n], lhsT=xsT[:, kc, :], rhs=w_in[:, kc, c0:c0 + n], start=(kc == 0), stop=(kc == 7)),
                reads=["xsT", "w_in"], writes=[("ps", pb)], signal=(kc == 7))
        S.op("dve", lambda e, pb=pb, c0=c0, n=n: e.tensor_tensor(out=qkv[:, c0:c0 + n], in0=PS(pb)[:, 0:n],
                                                                  in1=binb[:, c0:c0 + n], op=ALU.add),
             reads=[("ps", pb), "binb"], writes=["qkv"])
        c0 += n
        ci += 1
    S.dma("sp", lambda e: e.dma_start(out=ss_d[0].ap()[:, 127, :], in_=qkv[0:NSMP, 256:512]), reads=["qkv"], writes=["ssn0"])
    for g in range(3):
        L = GROUPS[g + 1][2]
        S.dma("sp", lambda e, g=g, L=L: e.dma_start(out=ss_d[g + 1].ap()[:, L - 1, 0:256],
                                                    in_=qkv[0:NSMP, 1280 + g * 256:1280 + (g + 1) * 256]),
              reads=["qkv"], writes=[("ssn", g, 0)])
        S.dma("sp", lambda e, g=g, L=L: e.dma_start(out=ss_d[g + 1].ap()[:, L - 1, 256:512],
                                                    in_=qkv[0:NSMP, 2048 + g * 256:2048 + (g + 1) * 256]),
              reads=["qkv"], writes=[("ssn", g, 1)])
    for (dst0, src0, n) in ((0, 0, 256), (256, 512, 512), (768, 1024, 256)):
        S.op("pe", lambda e, src0=src0, n=n: e.matmul(PS(2)[:, 0:n], lhsT=C["crep"][0:16, :], rhs=qkv[0:16, src0:src0 + n],
                                                      start=True, stop=True), reads=["qkv", "crep"], writes=[("ps", 2)])
        S.op("act", lambda e, dst0=dst0, n=n: e.activation(out=qrep[:, dst0:dst0 + n], in_=PS(2)[:, 0:n], func=AF.Identity, scale=0.125),
             reads=[("ps", 2)], writes=["qrep"])
    for gi, (d, maxd, L) in enumerate(GROUPS):
        H = 2 if gi == 0 else 4
        rowsz = 2 * H * 64
        ckv = ck[:, :, 0:rowsz]
        src = bass.AP(cache_d[gi], 0, [[16 * d * rowsz, 128], [d * rowsz, 16], [1, rowsz]])
        S.dma("sp", lambda e, ckv=ckv, src=src: e.dma_start(out=ckv, in_=src), writes=["ck"])
        for hq in range(4):
            c = (hq if gi == 0 else 4 + (gi - 1) * 4 + hq)
            S.dma("sp", lambda e, hq=hq, c=c: e.dma_start(
                out=ebs[:, hq, :], in_=bass.AP(T["FDS"], c * 2048, [[16, 128], [1, 16]])), reads=["FD"], writes=["ebs"])
        ck5 = ckv.rearrange("p k (a h c) -> p k a h c", a=2, h=H)
        for hq in range(4):
            kvh = hq // 2 if gi == 0 else hq
            qc = (hq * 64) if gi == 0 else (256 + ((gi - 1) * 4 + hq) * 64)
            kcol = (256 + kvh * 64) if gi == 0 else (1280 + ((gi - 1) * 4 + hq) * 64)
            vcol = (384 + kvh * 64) if gi == 0 else (2048 + ((gi - 1) * 4 + hq) * 64)
            c = (hq if gi == 0 else 4 + (gi - 1) * 4 + hq)
            S.op("dve", lambda e, kvh=kvh, qc=qc: e.tensor_tensor(
                out=prod, in0=ck5[:, :, 0, kvh, :], in1=qrep[:, qc:qc + 64].unsqueeze(1).broadcast_to([128, 16, 64]), op=ALU.mult),
                reads=["ck", "qrep"], writes=["prod"])
            S.op("dve", lambda e, hq=hq: e.reduce_sum(out=sc[:, hq, :], in_=prod, axis=AX.X), reads=["prod"], writes=["sc"])
            S.op("act", lambda e, hq=hq: e.activation(out=sc[:, hq, :], in_=sc[:, hq, :], func=AF.Exp), reads=["sc"], writes=["sc"])
            S.op("dve", lambda e, hq=hq: e.tensor_tensor(out=sc[:, hq, :], in0=sc[:, hq, :], in1=ebs[:, hq, :], op=ALU.mult),
                 reads=["sc", "ebs"], writes=["sc"])
            S.op("dve", lambda e, hq=hq: e.reduce_sum(out=part[:, hq, 64:65], in_=sc[:, hq, :], axis=AX.X), reads=["sc"], writes=["part"])
            S.op("dve", lambda e, kvh=kvh, hq=hq: e.tensor_tensor(
                out=prod.rearrange("p k c -> p c k"), in0=ck5[:, :, 1, kvh, :].rearrange("p k c -> p c k"),
                in1=sc[:, hq, :].unsqueeze(1).broadcast_to([128, 64, 16]), op=ALU.mult),
                reads=["ck", "sc"], writes=["prod"])
            S.op("dve", lambda e, hq=hq: e.reduce_sum(out=part[:, hq, 0:64], in_=prod.rearrange("p k c -> p c k"), axis=AX.X),
                 reads=["prod"], writes=["part"])
            S.op("dve", lambda e, qc=qc, kcol=kcol, hq=hq: e.tensor_tensor(
                out=pr16[0:16, :], in0=qkv[0:16, (qc if gi == 0 else 512 + qc - 256):(qc if gi == 0 else 512 + qc - 256) + 64],
                in1=qkv[0:16, kcol:kcol + 64], op=ALU.mult), reads=["qkv"], writes=["pr16"])
            S.op("dve", lambda e, hq=hq: e.reduce_sum(out=snew[0:16, hq:hq + 1], in_=pr16[0:16, :], axis=AX.X), reads=["pr16"], writes=["snew"])
            S.op("act", lambda e, hq=hq: e.activation(out=enew[0:16, hq:hq + 1], in_=snew[0:16, hq:hq + 1], func=AF.Exp, scale=0.125),
                 reads=["snew"], writes=["enew"])
            S.op("dve", lambda e, hq=hq, c=c: e.tensor_tensor(out=enew[0:16, hq:hq + 1], in0=enew[0:16, hq:hq + 1],
                                                               in1=f0[0:16, c:c + 1], op=ALU.mult), reads=["enew", "f0"], writes=["enew"])
        S.op("pe", lambda e: e.matmul(PS(3)[0:16, 0:260], lhsT=C["repT"], rhs=part.rearrange("p a b -> p (a b)"),
                                      start=True, stop=True), reads=["part", "c128"], writes=[("ps", 3)])
        S.op("dve", lambda e: e.tensor_copy(out=tot[0:16].rearrange("p a b -> p (a b)"), in_=PS(3)[0:16, 0:260]),
             reads=[("ps", 3)], writes=["tot"])
        for hq in range(4):
            kvh = hq // 2 if gi == 0 else hq
            vcol = (384 + kvh * 64) if gi == 0 else (2048 + ((gi - 1) * 4 + hq) * 64)
            S.op("dve", lambda e, hq=hq, vcol=vcol: e.scalar_tensor_tensor(
                out=tot[0:16, hq, 0:64], in0=qkv[0:16, vcol:vcol + 64], scalar=enew[0:16, hq:hq + 1], in1=tot[0:16, hq, 0:64],
                op0=ALU.mult, op1=ALU.add), reads=["qkv", "enew", "tot"], writes=["tot"])
            S.op("dve", lambda e, hq=hq: e.tensor_tensor(out=tot[0:16, hq, 64:65], in0=tot[0:16, hq, 64:65],
                                                          in1=enew[0:16, hq:hq + 1], op=ALU.add), reads=["enew", "tot"], writes=["tot"])
            if gi == 0:
                S.op("dve", lambda e, hq=hq: e.tensor_tensor(out=tot[0:16, hq, 64:65], in0=tot[0:16, hq, 64:65],
                                                              in1=C["expsink"][0:16, hq:hq + 1], op=ALU.add),
                     reads=["tot", "expsink"], writes=["tot"])
        if gi == 0:
            fin, base = tot, 0
        elif gi == 1:
            S.op("dve", lambda e: e.tensor_copy(out=totd[0:16], in_=tot[0:16]), reads=["tot"], writes=["totd"])
            fin = None
        else:
            S.op("dve", lambda e: e.tensor_tensor(out=totd[0:16], in0=totd[0:16], in1=tot[0:16], op=ALU.add),
                 reads=["tot", "totd"], writes=["totd"])
            fin, base = (totd, 4) if gi == 3 else (None, 0)
        if fin is not None:
            key = "tot" if gi == 0 else "totd"
            for hq in range(4):
                S.op("dve", lambda e, fin=fin, hq=hq: e.reciprocal(out=fin[0:16, hq, 64:65], in_=fin[0:16, hq, 64:65]),
                     reads=[key], writes=[key])
                S.op("dve", lambda e, fin=fin, hq=hq, base=base: e.tensor_scalar(
                    out=cat_s[0:16, base + hq, :], in0=fin[0:16, hq, 0:64], scalar1=fin[0:16, hq, 64:65], scalar2=None, op0=ALU.mult),
                    reads=[key], writes=["cat_s"])
    S.barrier()
    A.off = m0


_PROG = None


def kernel(x_prompt, x_sample, cache_swa_kv, cache_dil1_kv, cache_dil2_kv, cache_dil3_kv,
           rel_bias_table, w_in, b_in, attn_sinks, w_o, b_o, ln1_g, ln1_b,
           w_router, b_router, w_up, b_up, w_down, b_down, ln2_g, ln2_b):
    global _PROG
    f = lambda a: np.ascontiguousarray(np.asarray(a, dtype=np.float32))
    xp = f(x_prompt)
    B, SEQ, _ = xp.shape
    c128, coh, cval, crep = host_consts()
    shared = dict(
        table=f(rel_bias_table), w_in=f(w_in)[0], b_in=f(b_in)[0], sinks=f(attn_sinks)[0], w_o=f(w_o)[0],
        b_o=f(b_o)[0], ln1_g=f(ln1_g)[0], ln1_b=f(ln1_b)[0], ln2_g=f(ln2_g)[0], ln2_b=f(ln2_b)[0],
        w_r=f(w_router)[0], b_r=f(b_router)[0], w_up=f(w_up)[0][:NE_DECL], b_up=f(b_up)[0].reshape(NE * 16, 128),
        w_dn=f(w_down)[0][:NE_DECL], b_dn=f(b_down)[0], c128=c128, coh=coh, cval=cval, crep=crep)
    caches = [f(cache_swa_kv)[0], f(cache_dil1_kv)[0], f(cache_dil2_kv)[0], f(cache_dil3_kv)[0]]
    xs = f(x_sample)[:, 0, :]
    in_maps = []
    for c in range(NCORES):
        n, h = c // 2, c % 2
        xwin = np.zeros((HALO + SOWN, D), np.float32)
        xwin[HALO:] = xp[n, h * SOWN:(h + 1) * SOWN]
        fl = np.zeros((128, NUNIT), np.float32)
        fl[:, 1:] = 1.0
        if h == 1:
            xwin[:HALO] = xp[n, SOWN - HALO:SOWN]
            fl[:, 0] = 1.0
        m = dict(shared)
        m["xw"] = xwin
        m["flag"] = fl
        m["xs"] = np.ascontiguousarray(xs[c * NSMP:(c + 1) * NSMP])
        for nm, ca in zip(("c_swa", "c_d1", "c_d2", "c_d3"), caches):
            sl = ca[c * NSMP:(c + 1) * NSMP]
            m[nm] = np.ascontiguousarray(sl.reshape(NSMP, sl.shape[1], -1))
        in_maps.append(m)
    if _PROG is None:
        _PROG = build_program()
    res = run_bass_kernel_spmd(_PROG, in_maps, core_ids=list(range(NCORES)))
    R = res.results
    y_prompt = np.stack([np.concatenate([R[2 * n]["yp"], R[2 * n + 1]["yp"]], axis=0) for n in range(B)]).astype(np.float32)
    y_sample = np.concatenate([R[c]["ys"] for c in range(NCORES)], axis=0)[:, None, :].astype(np.float32)
    pst = []
    for nm, H in (("ps_swa", 2), ("ps_d1", 4), ("ps_d2", 4), ("ps_d3", 4)):
        pst.append(np.stack([R[2 * n + 1][nm] for n in range(B)])[None].astype(np.float32))
    sst = []
    for nm, H in (("ss_swa", 2), ("ss_d1", 4), ("ss_d2", 4), ("ss_d3", 4)):
        a = np.concatenate([R[c][nm] for c in range(NCORES)], axis=0)
        sst.append(a.reshape(1, a.shape[0], a.shape[1], 2, H, 64).astype(np.float32))
    return (y_prompt, y_sample, pst[0], pst[1], pst[2], pst[3], sst[0], sst[1], sst[2], sst[3])
```
